# Optimizing a Trainium2 kernel written in Bass

```python
import math
import jax, jax.numpy as jnp
from jax import lax
import numpy as np

D_MODEL = 1024
BATCH = 32
SEQ = 2048
DEPTH = 2

HEAD_DIM = 64
A_HEADS = D_MODEL // (2 * HEAD_DIM)
A_WIDTH = A_HEADS * HEAD_DIM
A_BRANCHES = ((128, 1), (512, 4), (2048, 16))
A_BLOCK = 128
B_HEADS = D_MODEL // (4 * HEAD_DIM)
B_QK_WIDTH = 2 * B_HEADS * HEAD_DIM
B_V_WIDTH = B_HEADS * 2 * HEAD_DIM
Q_BLOCK = 128
C_INNER = D_MODEL
C_HEAD_DIM = 64
C_HEADS = C_INNER // C_HEAD_DIM
C_GROUPS = 2
C_STATE = 128
C_CONV = 4
C_CHUNK = 128
D_INNER = D_MODEL
D_HEADS = 4
D_HEAD_DIM = D_INNER // D_HEADS
D_QK_BLOCK = 4
D_CONV = 4
D_CHUNK = 128
FFN_HIDDEN = -(-8 * D_MODEL // (3 * 256)) * 256
RMS_EPS = 1e-6

AB_SIZES = (A_WIDTH, A_WIDTH, A_WIDTH, B_QK_WIDTH, B_QK_WIDTH, B_V_WIDTH)
CD_SIZES = (C_INNER, C_INNER + 2 * C_GROUPS * C_STATE, C_HEADS,
            D_INNER, D_INNER, D_INNER, D_HEADS, D_HEADS)

kernel_name = "hybrid_dilated_diffattn_ssd_mlstm_trunk"


def _split(t, sizes):
    return jnp.split(t, np.cumsum(sizes)[:-1].tolist(), axis=-1)


def rmsnorm(x, w):
    xf = x.astype(jnp.float32)
    y = xf * lax.rsqrt(jnp.mean(xf * xf, axis=-1, keepdims=True) + RMS_EPS)
    return (y * w.astype(jnp.float32)).astype(x.dtype)


def causal_depthwise_conv(x, w, b):
    k, c = w.shape
    y = lax.conv_general_dilated(x, w[:, None, :], window_strides=(1,), padding=[(k - 1, 0)],
                                 dimension_numbers=("NWC", "WIO", "NWC"), feature_group_count=c)
    return y + b


def local_window_attn(q, k, v, n_back):
    lead, n, dh = q.shape[:-2], q.shape[-2], q.shape[-1]
    npad = -(-n // A_BLOCK) * A_BLOCK
    nb = npad // A_BLOCK
    pad = [(0, 0)] * len(lead) + [(0, npad - n), (0, 0)]
    qb, kb, vb = (jnp.pad(t, pad).reshape(lead + (nb, A_BLOCK, dh)) for t in (q, k, v))
    def with_prev(t):
        prev = jnp.concatenate([jnp.zeros_like(t[..., :1, :, :]), t[..., :-1, :, :]], axis=-3)
        return jnp.concatenate([prev, t], axis=-2)
    kk, vv = with_prev(kb), with_prev(vb)
    s = jnp.einsum("...bqd,...bkd->...bqk", qb, kk).astype(jnp.float32) * (HEAD_DIM ** -0.5)
    qpos = jnp.arange(nb)[:, None, None] * A_BLOCK + jnp.arange(A_BLOCK)[None, :, None]
    kpos = (jnp.arange(nb)[:, None, None] - 1) * A_BLOCK + jnp.arange(2 * A_BLOCK)[None, None, :]
    dist = qpos - kpos
    valid = (dist >= 0) & (dist <= n_back) & (kpos >= 0)
    s = jnp.where(valid, s, -jnp.inf)
    m = jnp.max(s, axis=-1, keepdims=True)
    p = jnp.exp(s - m)
    den = jnp.sum(p, axis=-1, keepdims=True)
    o = jnp.einsum("...bqk,...bkd->...bqd", (p / den).astype(v.dtype), vv)
    lse = (m + jnp.log(den))[..., 0]
    o = o.reshape(lead + (npad, dh))[..., :n, :]
    lse = lse.reshape(lead + (npad,))[..., :n]
    return o, lse


def dilated_branch(q, k, v, window, dilation):
    bn, h, t, dh = q.shape
    def strided(a):
        return a.reshape(bn, h, t // dilation, dilation, dh).swapaxes(2, 3)
    o, lse = local_window_attn(strided(q), strided(k), strided(v), window // dilation)
    return o.swapaxes(2, 3).reshape(bn, h, t, dh), lse.swapaxes(2, 3).reshape(bn, h, t)


def dilated_mixture_attention(q, k, v):
    outs, lses = [], []
    for window, dilation in A_BRANCHES:
        o, lse = dilated_branch(q, k, v, window, dilation)
        outs.append(o)
        lses.append(lse)
    wts = jax.nn.softmax(jnp.stack(lses, axis=0), axis=0)
    return jnp.einsum("gbht,gbhtd->bhtd", wts.astype(q.dtype), jnp.stack(outs, axis=0))


def diff_attention(q, k, v, lam):
    bn, h, t = q.shape[:3]
    nb = t // Q_BLOCK
    kidx = jnp.arange(t)
    def block(i):
        qs = lax.dynamic_slice_in_dim(q, i * Q_BLOCK, Q_BLOCK, axis=2)
        s = jnp.einsum("bhqcd,bhkcd->bchqk", qs, k).astype(jnp.float32) * (HEAD_DIM ** -0.5)
        qpos = i * Q_BLOCK + jnp.arange(Q_BLOCK)
        s = jnp.where(kidx[None, :] <= qpos[:, None], s, -jnp.inf)
        p = jax.nn.softmax(s, axis=-1)
        a = p[:, 0] - lam * p[:, 1]
        return jnp.einsum("bhqk,bhkd->bhqd", a.astype(v.dtype), v)
    out = lax.map(block, jnp.arange(nb))
    return out.transpose(1, 2, 0, 3, 4).reshape(bn, h, t, v.shape[-1])


def attn_mix(hn, w_in, w_out, lq1, lk1, lq2, lk2, subln_w, lambda_init):
    bn, t, _ = hn.shape
    qa, ka, va, qd, kd, vd = _split(hn @ w_in, AB_SIZES)
    def heads(a, n):
        return a.reshape(bn, t, n, -1).transpose(0, 2, 1, 3)
    ya = dilated_mixture_attention(heads(qa, A_HEADS), heads(ka, A_HEADS), heads(va, A_HEADS))
    ya = ya.transpose(0, 2, 1, 3).reshape(bn, t, A_WIDTH)
    lam = jnp.exp(jnp.sum(lq1 * lk1)) - jnp.exp(jnp.sum(lq2 * lk2)) + lambda_init
    qd = qd.reshape(bn, t, B_HEADS, 2, HEAD_DIM).transpose(0, 2, 1, 3, 4)
    kd = kd.reshape(bn, t, B_HEADS, 2, HEAD_DIM).transpose(0, 2, 1, 3, 4)
    yb = diff_attention(qd, kd, heads(vd, B_HEADS), lam)
    yb = rmsnorm(yb, subln_w) * (1.0 - lambda_init)
    yb = yb.transpose(0, 2, 1, 3).reshape(bn, t, B_V_WIDTH)
    return jnp.concatenate([ya, yb], axis=-1) @ w_out


def ssd_chunked(x, dt, a_neg, bm, cm):
    bn, t, h, p = x.shape
    g, n = bm.shape[2:]
    e = h // g
    nc = t // C_CHUNK
    x = x.reshape(bn, nc, C_CHUNK, g, e, p)
    dt = dt.reshape(bn, nc, C_CHUNK, g, e)
    bm = bm.reshape(bn, nc, C_CHUNK, g, n)
    cm = cm.reshape(bn, nc, C_CHUNK, g, n)
    xdt = x * dt[..., None]
    acum = jnp.cumsum(jnp.moveaxis(dt.astype(jnp.float32) * a_neg.reshape(g, e), 2, -1), axis=-1)
    causal = jnp.tril(jnp.ones((C_CHUNK, C_CHUNK), dtype=bool))
    decay = jnp.exp(jnp.where(causal, acum[..., :, None] - acum[..., None, :], -jnp.inf))
    cb = jnp.einsum("bclgn,bcsgn->bcgls", cm, bm)
    y_diag = jnp.einsum("bcgls,bcgels,bcsgep->bclgep", cb, decay, xdt)
    decay_end = jnp.exp(acum[..., -1:] - acum)
    states = jnp.einsum("bcsgn,bcges,bcsgep->bcgepn", bm, decay_end, xdt)
    chunk_decay = jnp.exp(acum[..., -1])
    def step(s, inp):
        st, dec = inp
        return dec[..., None, None] * s + st, s
    s0 = jnp.zeros(states.shape[:1] + states.shape[2:], states.dtype)
    _, s_in = lax.scan(step, s0, (jnp.moveaxis(states, 1, 0), jnp.moveaxis(chunk_decay, 1, 0)))
    s_in = jnp.moveaxis(s_in, 0, 1)
    y_off = jnp.einsum("bclgn,bcgepn,bcgel->bclgep", cm, s_in, jnp.exp(acum))
    return (y_diag + y_off).reshape(bn, t, h, p)


def mlstm_chunkwise(q, k, v, ig, logf):
    bn, t, h, dk = q.shape
    dv = v.shape[-1]
    nc = t // D_CHUNK
    q = q.reshape(bn, nc, D_CHUNK, h, dk)
    k = k.reshape(bn, nc, D_CHUNK, h, dk)
    v = v.reshape(bn, nc, D_CHUNK, h, dv)
    ig = jnp.moveaxis(ig.astype(jnp.float32).reshape(bn, nc, D_CHUNK, h), 2, -1)
    bcum = jnp.cumsum(jnp.moveaxis(logf.astype(jnp.float32).reshape(bn, nc, D_CHUNK, h), 2, -1), axis=-1)
    gtot = bcum[..., -1]
    causal = jnp.tril(jnp.ones((D_CHUNK, D_CHUNK), dtype=bool))
    dmat = jnp.where(causal, bcum[..., :, None] - bcum[..., None, :] + ig[..., None, :], -jnp.inf)
    w_end = gtot[..., None] - bcum + ig
    a_loc = jnp.max(w_end, axis=-1)
    e_end = jnp.exp(w_end - a_loc[..., None])
    c_loc = jnp.einsum("bchs,bcshv,bcshk->bchvk", e_end, v, k)
    n_loc = jnp.einsum("bchs,bcshk->bchk", e_end, k)
    def step(carry, inp):
        cs, ns, ms = carry
        cl, nl, al, gc = inp
        m_new = jnp.maximum(gc + ms, al)
        d_old = jnp.exp(gc + ms - m_new)
        d_loc = jnp.exp(al - m_new)
        c_new = d_old[..., None, None] * cs + d_loc[..., None, None] * cl
        n_new = d_old[..., None] * ns + d_loc[..., None] * nl
        return (c_new, n_new, m_new), (cs, ns, ms)
    init = (jnp.zeros((bn, h, dv, dk), c_loc.dtype), jnp.zeros((bn, h, dk), n_loc.dtype),
            jnp.zeros((bn, h), gtot.dtype))
    xs = tuple(jnp.moveaxis(a, 1, 0) for a in (c_loc, n_loc, a_loc, gtot))
    _, (c_in, n_in, m_in) = lax.scan(step, init, xs)
    c_in, n_in, m_in = (jnp.moveaxis(a, 0, 1) for a in (c_in, n_in, m_in))
    inter_log = bcum + m_in[..., None]
    m_out = jnp.maximum(jnp.max(dmat, axis=-1), inter_log)
    wqk = jnp.exp(dmat - m_out[..., None]) * jnp.einsum("bclhk,bcshk->bchls", q, k)
    e_inter = jnp.exp(inter_log - m_out)
    num = (jnp.einsum("bchls,bcshv->bclhv", wqk, v)
           + jnp.einsum("bclhk,bchvk->bclhv", q, c_in) * jnp.swapaxes(e_inter, 2, 3)[..., None])
    den = jnp.sum(wqk, axis=-1) + jnp.einsum("bclhk,bchk->bchl", q, n_in) * e_inter
    denom = jnp.maximum(jnp.abs(den), jnp.exp(-m_out))
    return (num / jnp.swapaxes(denom, 2, 3)[..., None]).reshape(bn, t, h, dv)


def headwise(x, w):
    bn, t, _ = x.shape
    y = jnp.einsum("btnj,nji->btni", x.reshape(bn, t, w.shape[0], D_QK_BLOCK), w)
    return y.reshape(bn, t, D_INNER)


def ssm_mix(hn, w_in, w_out, c_conv_w, c_conv_b, c_dt_bias, c_a_log, c_d_skip, c_norm_w,
            d_conv_w, d_conv_b, d_wq, d_wk, d_i_bias, d_f_bias, d_norm_w):
    bn, t, _ = hn.shape
    z, xbc, dt_raw, u, v, o_pre, i_pre, f_pre = _split(hn @ w_in, CD_SIZES)
    xbc = jax.nn.silu(causal_depthwise_conv(xbc, c_conv_w, c_conv_b))
    xs, bm, cm = _split(xbc, (C_INNER, C_GROUPS * C_STATE, C_GROUPS * C_STATE))
    dt = jax.nn.softplus((dt_raw + c_dt_bias).astype(jnp.float32))
    a_neg = -jnp.exp(c_a_log.astype(jnp.float32))
    xs_h = xs.reshape(bn, t, C_HEADS, C_HEAD_DIM)
    y = ssd_chunked(xs_h, dt, a_neg, bm.reshape(bn, t, C_GROUPS, C_STATE), cm.reshape(bn, t, C_GROUPS, C_STATE))
    y = (y + xs_h * c_d_skip[:, None]).astype(hn.dtype).reshape(bn, t, C_INNER)
    yc = rmsnorm(y * jax.nn.silu(z), c_norm_w)
    uc = jax.nn.silu(causal_depthwise_conv(u, d_conv_w, d_conv_b))
    q = headwise(uc, d_wq).reshape(bn, t, D_HEADS, D_HEAD_DIM)
    k = headwise(uc, d_wk).reshape(bn, t, D_HEADS, D_HEAD_DIM) * (D_HEAD_DIM ** -0.5)
    vh = v.reshape(bn, t, D_HEADS, D_HEAD_DIM)
    logf = jax.nn.log_sigmoid((f_pre + d_f_bias).astype(jnp.float32))
    hd = mlstm_chunkwise(q, k, vh, i_pre + d_i_bias, logf).astype(hn.dtype)
    hd = rmsnorm(hd, d_norm_w.reshape(D_HEADS, D_HEAD_DIM)).reshape(bn, t, D_INNER)
    yd = jax.nn.sigmoid(o_pre) * hd
    return jnp.concatenate([yc, yd], axis=-1) @ w_out


def swiglu(h, w_gate_up, w_down):
    g, u = jnp.split(h @ w_gate_up, 2, axis=-1)
    return (jax.nn.silu(g) * u) @ w_down


def setup_inputs(seed: int = 0) -> dict:
    key = jax.random.key(seed)
    ks = iter(jax.random.split(key, 40))
    def nrm(shape, scale):
        return jax.random.normal(next(ks), shape, jnp.float32) * scale
    ne = (DEPTH + 1) // 2
    no = DEPTH // 2
    ab_cols = sum(AB_SIZES)
    cd_cols = sum(CD_SIZES)
    xbc_w = C_INNER + 2 * C_GROUPS * C_STATE
    x = nrm((BATCH, SEQ, D_MODEL), 1.0)
    mix_norm_w = 1.0 + nrm((DEPTH, D_MODEL), 0.02)
    ffn_norm_w = 1.0 + nrm((DEPTH, D_MODEL), 0.02)
    ab_w_in = nrm((ne, D_MODEL, ab_cols), D_MODEL ** -0.5)
    ab_w_out = nrm((ne, A_WIDTH + B_V_WIDTH, D_MODEL), (A_WIDTH + B_V_WIDTH) ** -0.5)
    diff_lq1 = nrm((ne, HEAD_DIM), 0.1)
    diff_lk1 = nrm((ne, HEAD_DIM), 0.1)
    diff_lq2 = nrm((ne, HEAD_DIM), 0.1)
    diff_lk2 = nrm((ne, HEAD_DIM), 0.1)
    diff_subln_w = 1.0 + nrm((ne, 2 * HEAD_DIM), 0.02)
    cd_w_in = nrm((no, D_MODEL, cd_cols), D_MODEL ** -0.5)
    c_conv_w = nrm((no, C_CONV, xbc_w), C_CONV ** -0.5)
    c_conv_b = nrm((no, xbc_w), 0.02)
    dt0 = jnp.exp(jax.random.uniform(next(ks), (no, C_HEADS), jnp.float32,
                                     minval=math.log(1e-3), maxval=math.log(1e-1)))
    c_dt_bias = dt0 + jnp.log(-jnp.expm1(-dt0))
    c_a_log = jnp.log(jax.random.uniform(next(ks), (no, C_HEADS), jnp.float32, minval=1.0, maxval=16.0))
    c_d_skip = 1.0 + nrm((no, C_HEADS), 0.1)
    c_norm_w = 1.0 + nrm((no, C_INNER), 0.02)
    d_conv_w = nrm((no, D_CONV, D_INNER), D_CONV ** -0.5)
    d_conv_b = nrm((no, D_INNER), 0.02)
    d_wq = nrm((no, D_INNER // D_QK_BLOCK, D_QK_BLOCK, D_QK_BLOCK), D_QK_BLOCK ** -0.5)
    d_wk = nrm((no, D_INNER // D_QK_BLOCK, D_QK_BLOCK, D_QK_BLOCK), D_QK_BLOCK ** -0.5)
    d_i_bias = nrm((no, D_HEADS), 0.1)
    d_f_bias = jnp.linspace(3.0, 6.0, D_HEADS, dtype=jnp.float32)[None, :] + nrm((no, D_HEADS), 0.1)
    d_norm_w = 1.0 + nrm((no, D_INNER), 0.02)
    cd_w_out = nrm((no, C_INNER + D_INNER, D_MODEL), (C_INNER + D_INNER) ** -0.5)
    ffn_w_gate_up = nrm((DEPTH, D_MODEL, 2 * FFN_HIDDEN), D_MODEL ** -0.5)
    ffn_w_down = nrm((DEPTH, FFN_HIDDEN, D_MODEL), FFN_HIDDEN ** -0.5)
    final_norm_w = 1.0 + nrm((D_MODEL,), 0.02)
    return {"x": x, "mix_norm_w": mix_norm_w, "ffn_norm_w": ffn_norm_w,
            "ab_w_in": ab_w_in, "ab_w_out": ab_w_out,
            "diff_lq1": diff_lq1, "diff_lk1": diff_lk1, "diff_lq2": diff_lq2, "diff_lk2": diff_lk2,
            "diff_subln_w": diff_subln_w, "cd_w_in": cd_w_in,
            "c_conv_w": c_conv_w, "c_conv_b": c_conv_b, "c_dt_bias": c_dt_bias, "c_a_log": c_a_log,
            "c_d_skip": c_d_skip, "c_norm_w": c_norm_w,
            "d_conv_w": d_conv_w, "d_conv_b": d_conv_b, "d_wq": d_wq, "d_wk": d_wk,
            "d_i_bias": d_i_bias, "d_f_bias": d_f_bias, "d_norm_w": d_norm_w, "cd_w_out": cd_w_out,
            "ffn_w_gate_up": ffn_w_gate_up, "ffn_w_down": ffn_w_down, "final_norm_w": final_norm_w}


def reference(x, mix_norm_w, ffn_norm_w, ab_w_in, ab_w_out, diff_lq1, diff_lk1, diff_lq2, diff_lk2,
              diff_subln_w, cd_w_in, c_conv_w, c_conv_b, c_dt_bias, c_a_log, c_d_skip, c_norm_w,
              d_conv_w, d_conv_b, d_wq, d_wk, d_i_bias, d_f_bias, d_norm_w, cd_w_out,
              ffn_w_gate_up, ffn_w_down, final_norm_w):
    h = x
    for layer in range(DEPTH):
        j = layer // 2
        hn = rmsnorm(h, mix_norm_w[layer])
        if layer % 2 == 0:
            lambda_init = 0.8 - 0.6 * math.exp(-0.3 * layer)
            h = h + attn_mix(hn, ab_w_in[j], ab_w_out[j], diff_lq1[j], diff_lk1[j], diff_lq2[j],
                             diff_lk2[j], diff_subln_w[j], lambda_init)
        else:
            h = h + ssm_mix(hn, cd_w_in[j], cd_w_out[j], c_conv_w[j], c_conv_b[j], c_dt_bias[j],
                            c_a_log[j], c_d_skip[j], c_norm_w[j], d_conv_w[j], d_conv_b[j],
                            d_wq[j], d_wk[j], d_i_bias[j], d_f_bias[j], d_norm_w[j])
        h = h + swiglu(rmsnorm(h, ffn_norm_w[layer]), ffn_w_gate_up[layer], ffn_w_down[layer])
    return rmsnorm(h, final_norm_w)
```

```python
import math
from contextlib import ExitStack
import numpy as np
import concourse.bass as bass
import concourse.mybir as mybir
from concourse.bass_utils import run_bass_kernel_spmd

F32 = mybir.dt.float32
BF16 = mybir.dt.bfloat16
AF = mybir.ActivationFunctionType
ALU = mybir.AluOpType

T = 2048
D = 1024
NT = 16
FFN_H = 2816
EPS = 1e-6
AB_COLS = 3072
CD_COLS = 5656
NEGM = -30000.0


class _Dep:
    def __init__(self):
        self.w = {}
        self.r = {}
        self.dsem = None


class Buf:
    def __init__(self, ap, name="", owner=None):
        self.ap = ap
        self.name = name
        self.d = owner.d if owner is not None else _Dep()

    w = property(lambda self: self.d.w, lambda self, v: setattr(self.d, "w", v))
    r = property(lambda self: self.d.r, lambda self, v: setattr(self.d, "r", v))
    dsem = property(lambda self: self.d.dsem, lambda self, v: setattr(self.d, "dsem", v))


class Fw:
    ENG = ["tensor", "vector", "scalar", "gpsimd", "sync"]

    def __init__(self, nc):
        self.nc = nc
        self.sem = {}
        self.cnt = {}
        self.waited = {}
        self.semobj = {}
        for e in self.ENG:
            s = nc.alloc_semaphore(name="s_" + e)
            self.sem[e] = s
            self.cnt[e] = 0
            self.waited[e] = {}
        self.dma_total = {}
        self.nwaits = 0
        self.nops = 0

    def eng(self, e):
        return getattr(self.nc, e)

    def _wait(self, e, deps):
        w = self.waited[e]
        for k, v in deps.items():
            if k[0] == 'e':
                if k[1] == e and e == "tensor":
                    continue
                sem = self.sem[k[1]]
            else:
                sem = self.semobj[k[1]]
                v = max(v, self.dma_total[k[1]])
            if w.get(k, 0) >= v:
                continue
            self.eng(e).wait_ge(sem, v)
            self.nwaits += 1
            w[k] = v

    @staticmethod
    def _merge(d, k, v):
        if d.get(k, 0) < v:
            d[k] = v

    def _deps(self, reads, writes):
        deps = {}
        for b in reads:
            for k, v in b.w.items():
                self._merge(deps, k, v)
        for b in writes:
            for k, v in b.w.items():
                self._merge(deps, k, v)
            for k, v in b.r.items():
                self._merge(deps, k, v)
        return deps

    def _record(self, key, val, reads, writes):
        for b in reads:
            self._merge(b.r, key, val)
        for b in writes:
            self._merge(b.w, key, val)
            b.r = {}

    def op(self, e, fn, reads=(), writes=()):
        self._wait(e, self._deps(reads, writes))
        ins = fn(self.eng(e))
        self.cnt[e] += 1
        ins.then_inc(self.sem[e], 1)
        self.nops += 1
        self._record(('e', e), self.cnt[e], reads, writes)
        return ins

    def dma(self, q, out_ap, in_ap, reads=(), writes=(), sbuf=None, **kw):
        self._wait(q, self._deps(reads, writes))
        if sbuf.dsem is None:
            sbuf.dsem = self.nc.alloc_semaphore(name="d_" + sbuf.name)
            self.semobj[id(sbuf.dsem)] = sbuf.dsem
            self.dma_total[id(sbuf.dsem)] = 0
        ins = self.eng(q).dma_start(out=out_ap, in_=in_ap, **kw)
        ins.then_inc(sbuf.dsem, 16)
        self.dma_total[id(sbuf.dsem)] += 16
        self._record(('d', id(sbuf.dsem)), self.dma_total[id(sbuf.dsem)], reads, writes)
        return ins

    def barrier(self):
        deps = {('e', e): self.cnt[e] for e in self.ENG if self.cnt[e] > 0}
        for k, v in self.dma_total.items():
            if v > 0:
                deps[('d', k)] = v
        for e in self.ENG:
            self._wait(e, dict(deps))


C_ID = 0
C_U = 128
C_NEG = 256
C_ONES = 384
C_TOT = 512
B_ID = 0
B_MA = 128
B_MB = B_MA + 2304
B_TOT = B_MB + 384


def make_consts():
    c = np.zeros((128, C_TOT), np.float32)
    cb = np.zeros((128, B_TOT), np.float32)
    s = np.arange(128)[:, None]
    c[:, C_ID:C_ID + 128] = np.eye(128, dtype=np.float32)
    cb[:, B_ID:B_ID + 128] = np.eye(128, dtype=np.float32)
    cc = np.arange(2304)[None, :]
    d = cc - 128 - s
    mult = ((d >= 0) & (d <= 128)).astype(np.float32)
    mult += ((d >= 0) & (d % 4 == 0) & (d <= 512)).astype(np.float32)
    mult += ((d >= 0) & (d % 16 == 0) & (d <= 2048)).astype(np.float32)
    cb[:, B_MA:B_MA + 2304] = mult
    cc = np.arange(384)[None, :]
    cb[:, B_MB:B_MB + 384] = ((cc - 128 - s) >= 0).astype(np.float32)
    l = np.arange(128)[None, :]
    c[:, C_U:C_U + 128] = (s <= l).astype(np.float32)
    c[:, C_NEG:C_NEG + 128] = np.where(s > l, NEGM, 0.0).astype(np.float32)
    c[:, C_ONES:C_ONES + 128] = 1.0
    return c, cb


def build_program(nseq, do_l0=True, do_l1=True, do_ffn=True, units=tuple(range(8)), ngroups=8, stage=9):
    nc = bass.Bass("TRN2", target_bir_lowering=False)
    fw = Fw(nc)

    def din(name, shape):
        return nc.dram_tensor(name, list(shape), F32, kind="ExternalInput").ap()

    x_d = din("x", [nseq, T, D])
    consts_d = din("consts", [128, C_TOT])
    constsb_d = din("constsb", [128, B_TOT])
    mixg_d = din("mix_norm_w", [2, D])
    ffng_d = din("ffn_norm_w", [2, D])
    fing_d = din("final_norm_w", [1, D])
    abin_d = din("ab_w_in", [D, AB_COLS])
    about_d = din("ab_w_out", [D, D])
    lq1_d = din("diff_lq1", [1, 64]); lk1_d = din("diff_lk1", [1, 64])
    lq2_d = din("diff_lq2", [1, 64]); lk2_d = din("diff_lk2", [1, 64])
    subln_d = din("diff_subln_w", [1, 128])
    cdin_d = din("cd_w_in", [D, CD_COLS])
    cconvw_d = din("c_conv_w", [4, 1536]); cconvb_d = din("c_conv_b", [1, 1536])
    cdtb_d = din("c_dt_bias", [1, 16]); calog_d = din("c_a_log", [1, 16]); cdskip_d = din("c_d_skip", [1, 16])
    cnormw_d = din("c_norm_w", [1, D])
    dconvw_d = din("d_conv_w", [4, D]); dconvb_d = din("d_conv_b", [1, D])
    dwq_d = din("d_wq_bd", [8, 128, 128]); dwk_d = din("d_wk_bd", [8, 128, 128])
    dib_d = din("d_i_bias", [1, 4]); dfb_d = din("d_f_bias", [1, 4])
    dnormw_d = din("d_norm_w", [1, D])
    cdout_d = din("cd_w_out", [2 * D, D])
    wgu_d = din("ffn_w_gate_up", [2, D, 2 * FFN_H])
    wdn_d = din("ffn_w_down", [2, FFN_H, D])
    out_d = nc.dram_tensor("out", [nseq, T, D], F32, kind="ExternalOutput").ap()

    cnt = [0]

    def sb(shape, dt=F32, name=None):
        cnt[0] += 1
        nm = (name or "t") + "_%d" % cnt[0]
        return Buf(nc.alloc_sbuf_tensor(nm, list(shape), dt).ap(), nm)

    def scoped_sb(scope):
        def f(shape, dt=F32, name=None):
            cnt[0] += 1
            nm = (name or "t") + "_%d" % cnt[0]
            return Buf(scope.enter_context(nc.sbuf_tensor(nm, list(shape), dt)).ap(), nm)
        return f

    hT = sb([128, 8, T], F32, "hT")
    cst = sb([128, C_TOT], F32, "cst")
    cstb = sb([128, B_TOT], BF16, "cstb")
    gam = sb([128, 5, 8], F32, "gam")
    PB = []
    for i in range(8):
        PB.append(Buf(nc.alloc_psum_tensor("pb%d" % i, [128, 512], F32).ap(), "pb%d" % i))
    pb_rr = [0]

    def bank():
        b = PB[pb_rr[0] % 8]
        pb_rr[0] += 1
        return b

    fw.dma("sync", cst.ap, consts_d, writes=[cst], sbuf=cst)
    fw.dma("gpsimd", cstb.ap, constsb_d, writes=[cstb], sbuf=cstb)
    for i, (g_d, row) in enumerate([(mixg_d, 0), (ffng_d, 0), (mixg_d, 1), (ffng_d, 1), (fing_d, 0)]):
        fw.dma("sync", gam.ap[:, i, :], g_d[row, :].rearrange("(c p) -> p c", p=128), writes=[gam], sbuf=gam,
               allow_slow_non_contiguous=True)
    ident_f = cst.ap[:, C_ID:C_ID + 128]
    ident_b = cstb.ap[:, B_ID:B_ID + 128]
    ones_f = cst.ap[:, C_ONES:C_ONES + 128]

    NW = 4
    wbf = [sb([128, 8, 128], BF16, "wbf") for _ in range(NW)]
    w_rr = [0]

    def loadw(dram_rows_cols, nchunk, ncols=128):
        i = w_rr[0] % NW
        w_rr[0] += 1
        bf = wbf[i]
        fw.dma("gpsimd", bf.ap[:, 0:nchunk, 0:ncols], dram_rows_cols.rearrange("(c p) n -> p c n", p=128),
               writes=[bf], sbuf=bf)
        return bf

    evac_rr = [0]

    def evac(out_ap, in_ap, reads, writes):
        evac_rr[0] += 1
        if evac_rr[0] % 2 == 0:
            fw.op("scalar", lambda e: e.activation(out=out_ap, in_=in_ap, func=AF.Copy), reads=reads, writes=writes)
        else:
            fw.op("vector", lambda e: e.tensor_copy(out=out_ap, in_=in_ap), reads=reads, writes=writes)

    def mm(out_ap, lhsT, rhs, start, stop, reads, writes):
        fw.op("tensor", lambda e: e.matmul(out_ap, lhsT=lhsT, rhs=rhs, start=start, stop=stop, skip_group_check=True),
              reads=reads, writes=writes)

    sq = [sb([128, 512], F32, "sq") for _ in range(2)]
    rstd = sb([128, 512], F32, "rstd")

    def rms_stats(tg):
        pb = bank()
        for c in range(8):
            s = sq[c % 2]
            fw.op("scalar", lambda e: e.activation(out=s.ap, in_=hT.ap[:, c, tg * 512:(tg + 1) * 512], func=AF.Square),
                  reads=[hT], writes=[s])
            mm(pb.ap, ones_f, s.ap, c == 0, c == 7, [s, cst], [pb])
        fw.op("scalar", lambda e: e.activation(out=rstd.ap, in_=pb.ap, func=AF.Ln, scale=1.0 / D, bias=EPS),
              reads=[pb], writes=[rstd])
        fw.op("scalar", lambda e: e.activation(out=rstd.ap, in_=rstd.ap, func=AF.Exp, scale=-0.5),
              reads=[rstd], writes=[rstd])

    def rmsnorm_to(dst, gi, dst_dtype_is_f32=False):
        for tg in range(4):
            rms_stats(tg)
            for c in range(8):
                fw.op("vector", lambda e: e.scalar_tensor_tensor(
                    out=dst.ap[:, c, tg * 512:(tg + 1) * 512], in0=hT.ap[:, c, tg * 512:(tg + 1) * 512],
                    scalar=gam.ap[:, gi, c:c + 1], in1=rstd.ap, op0=ALU.mult, op1=ALU.mult),
                    reads=[hT, gam, rstd], writes=[dst])

    hnT = sb([128, 8, T], BF16, "hnT")
    ytm = sb([128, NT, D], BF16, "ytm")
    actT = Buf(ytm.ap.rearrange("p t d -> p (t d)").rearrange("p (h t) -> p h t", h=8), "actT", owner=ytm)

    def load_x(s):
        sc = ExitStack()
        sbx = scoped_sb(sc)
        xin = [sbx([128, D], F32, "xin") for _ in range(2)]
        for t in range(NT):
            xt = xin[t % 2]
            fw.dma("sync", xt.ap, x_d[s, t * 128:(t + 1) * 128, :], writes=[xt], sbuf=xt)
            for g in range(2):
                pb = bank()
                for cc in range(4):
                    c = g * 4 + cc
                    mm(pb.ap[:, cc * 128:(cc + 1) * 128], xt.ap[:, c * 128:(c + 1) * 128], ident_f, True, True,
                       [xt, cst], [pb])
                evac(hT.ap[:, g * 4:(g + 1) * 4, t * 128:(t + 1) * 128],
                     pb.ap.rearrange("p (c n) -> p c n", c=4), [pb], [hT])
        fw.barrier()
        sc.close()

    def transpose_ytm_to_hnT():
        for t in range(NT):
            for g in range(2):
                pb = bank()
                for cc in range(4):
                    c = g * 4 + cc
                    mm(pb.ap[:, cc * 128:(cc + 1) * 128], ytm.ap[:, t, c * 128:(c + 1) * 128], ident_b, True, True,
                       [ytm, cstb], [pb])
                evac(hnT.ap[:, g * 4:(g + 1) * 4, t * 128:(t + 1) * 128],
                     pb.ap.rearrange("p (c n) -> p c n", c=4), [pb], [hnT])

    def out_proj(w_dram):
        for cb in range(8):
            wb = loadw(w_dram[:, cb * 128:(cb + 1) * 128], 8)
            for tg in range(4):
                pb = bank()
                for c in range(8):
                    mm(pb.ap, wb.ap[:, c, :], hnT.ap[:, c, tg * 512:(tg + 1) * 512], c == 0, c == 7, [wb, hnT], [pb])
                fw.op("vector", lambda e: e.tensor_tensor(out=hT.ap[:, cb, tg * 512:(tg + 1) * 512],
                                                          in0=hT.ap[:, cb, tg * 512:(tg + 1) * 512], in1=pb.ap, op=ALU.add),
                      reads=[hT, pb], writes=[hT])

    def layer0_mix():
        scope = ExitStack()
        sb = scoped_sb(scope)
        qT = sb([128, T], BF16, "qT")
        kT = sb([128, 2, T], BF16, "kT")
        vA = sb([128, NT, 2, 65], BF16, "vA")
        vB = sb([128, NT, 129], BF16, "vB")
        PT = [sb([128, 2, 256], BF16, "PT") for _ in range(2)]
        lamw = sb([128, 8], F32, "lamw")
        lqk = sb([128, 4, 64], F32, "lqk")
        sublnw = sb([128, 128], F32, "sublnw")
        fin_s = [sb([128, 8], F32, "fin_s") for _ in range(2)]
        fin_t = [sb([128, 128], F32, "fin_t") for _ in range(2)]
        fin_o = [sb([128, 128], F32, "fin_o") for _ in range(2)]
        fin_j = sb([128, 128], F32, "fin_j")

        fw.op("vector", lambda e: e.memset(kT.ap, 0.0), writes=[kT])
        fw.op("vector", lambda e: e.memset(vA.ap, 1.0), writes=[vA])
        fw.op("vector", lambda e: e.memset(vB.ap, 1.0), writes=[vB])
        for i, d_ in enumerate([lq1_d, lk1_d, lq2_d, lk2_d]):
            fw.dma("sync", lqk.ap[:, i, :], d_.partition_broadcast(128), writes=[lqk], sbuf=lqk)
        fw.dma("sync", sublnw.ap, subln_d.partition_broadcast(128), writes=[sublnw], sbuf=sublnw)
        lambda_init = 0.8 - 0.6 * math.exp(-0.3 * 0)
        fw.op("vector", lambda e: e.tensor_scalar(out=sublnw.ap, in0=sublnw.ap, scalar1=1.0 - lambda_init, scalar2=None,
                                                  op0=ALU.mult), reads=[sublnw], writes=[sublnw])
        fw.op("vector", lambda e: e.tensor_tensor(out=lqk.ap[:, 0, :], in0=lqk.ap[:, 0, :], in1=lqk.ap[:, 1, :], op=ALU.mult),
              reads=[lqk], writes=[lqk])
        fw.op("vector", lambda e: e.tensor_tensor(out=lqk.ap[:, 2, :], in0=lqk.ap[:, 2, :], in1=lqk.ap[:, 3, :], op=ALU.mult),
              reads=[lqk], writes=[lqk])
        fw.op("vector", lambda e: e.reduce_sum(out=lamw.ap[:, 1:2], in_=lqk.ap[:, 0, :], axis=mybir.AxisListType.X),
              reads=[lqk], writes=[lamw])
        fw.op("vector", lambda e: e.reduce_sum(out=lamw.ap[:, 2:3], in_=lqk.ap[:, 2, :], axis=mybir.AxisListType.X),
              reads=[lqk], writes=[lamw])
        fw.op("scalar", lambda e: e.activation(out=lamw.ap[:, 3:5], in_=lamw.ap[:, 1:3], func=AF.Exp), reads=[lamw], writes=[lamw])
        fw.op("vector", lambda e: e.tensor_tensor(out=lamw.ap[:, 0:1], in0=lamw.ap[:, 4:5], in1=lamw.ap[:, 3:4], op=ALU.subtract),
              reads=[lamw], writes=[lamw])
        fw.op("vector", lambda e: e.tensor_scalar(out=lamw.ap[:, 0:1], in0=lamw.ap[:, 0:1], scalar1=-lambda_init, scalar2=None,
                                                  op0=ALU.add), reads=[lamw], writes=[lamw])

        def attn_unit(u):
            isA = u < 4
            qoff = (0 if isA else 1536) + (u % 4) * 128
            koff = qoff + 512
            voff = qoff + 1024
            wq = loadw(abin_d[:, qoff:qoff + 128], 8)
            wk = loadw(abin_d[:, koff:koff + 128], 8)
            wv = loadw(abin_d[:, voff:voff + 128], 8)
            for (wb, dst) in ((wq, qT), (wk, kT)):
                for tg in range(4):
                    pb = bank()
                    for c in range(8):
                        mm(pb.ap, wb.ap[:, c, :], hnT.ap[:, c, tg * 512:(tg + 1) * 512], c == 0, c == 7, [wb, hnT], [pb])
                    if dst is qT:
                        evac(dst.ap[:, tg * 512:(tg + 1) * 512], pb.ap, [pb], [dst])
                    else:
                        evac(dst.ap[0:64, 0, tg * 512:(tg + 1) * 512], pb.ap[0:64, :], [pb], [dst])
                        evac(dst.ap[64:128, 1, tg * 512:(tg + 1) * 512], pb.ap[64:128, :], [pb], [dst])
            for t4 in range(4):
                pb = bank()
                for tt in range(4):
                    t = t4 * 4 + tt
                    for c in range(8):
                        mm(pb.ap[:, tt * 128:(tt + 1) * 128], hnT.ap[:, c, t * 128:(t + 1) * 128], wv.ap[:, c, :],
                           c == 0, c == 7, [wv, hnT], [pb])
                if isA:
                    evac(vA.ap[:, t4 * 4:(t4 + 1) * 4, :, 0:64], pb.ap.rearrange("p (t i d) -> p t i d", t=4, i=2), [pb], [vA])
                else:
                    evac(vB.ap[:, t4 * 4:(t4 + 1) * 4, 0:128], pb.ap.rearrange("p (t d) -> p t d", t=4), [pb], [vB])
            W = 65 if isA else 129
            k = 0
            if stage < 1:
                return
            for G in range(ngroups):
                acc = [[PB[0], PB[1]], [PB[2], PB[3]]]
                for i in range(2):
                    for b in range(2):
                        a = acc[i][b]
                        fw.op("vector", lambda e: e.memset(a.ap[:, 0:W], 0.0), writes=[a])
                for j in range(2 * G + 2):
                    sbk = PB[4 + (k % 2)]
                    pt = PT[k % 2]
                    k += 1
                    for i in range(2):
                        mm(sbk.ap[:, i * 256:(i + 1) * 256], kT.ap[:, i, j * 128:(j + 1) * 128],
                           qT.ap[:, G * 256:(G + 1) * 256], True, True, [kT, qT], [sbk])
                    fw.op("scalar", lambda e: e.activation(out=pt.ap, in_=sbk.ap.rearrange("p (i n) -> p i n", i=2),
                                                           func=AF.Exp, scale=0.125), reads=[sbk], writes=[pt])
                    if stage < 2:
                        continue
                    d0 = 2 * G - j
                    if isA:
                        m = cstb.ap[:, B_MA + 128 * (d0 + 1):B_MA + 128 * (d0 + 1) + 256]
                    elif d0 <= 0:
                        m = cstb.ap[:, B_MB + 128 * (d0 + 1):B_MB + 128 * (d0 + 1) + 256]
                    else:
                        m = None
                    if m is not None:
                        mb_ = m.unsqueeze(1).broadcast_to([128, 2, 256])
                        fw.op("vector", lambda e: e.tensor_tensor(out=pt.ap, in0=pt.ap, in1=mb_, op=ALU.mult),
                              reads=[pt, cstb], writes=[pt])
                    if stage < 3:
                        continue
                    for b in range(2):
                        if 2 * G + b < j:
                            continue
                        for i in range(2):
                            rhs = vA.ap[:, j, i, :] if isA else vB.ap[:, j, :]
                            mm(acc[i][b].ap[:, 0:W], pt.ap[:, i, b * 128:(b + 1) * 128], rhs, False, False,
                               [pt, vA if isA else vB], [acc[i][b]])
                if stage < 4:
                    continue
                for b in range(2):
                    qb = 2 * G + b
                    fs = fin_s[b]
                    if isA:
                        for i in range(2):
                            a = acc[i][b]
                            fw.op("vector", lambda e: e.reciprocal(out=fs.ap[:, i:i + 1], in_=a.ap[:, 64:65]), reads=[a], writes=[fs])
                            fw.op("vector", lambda e: e.tensor_scalar(
                                out=ytm.ap[:, qb, u * 128 + 64 * i:u * 128 + 64 * i + 64], in0=a.ap[:, 0:64],
                                scalar1=fs.ap[:, i:i + 1], scalar2=None, op0=ALU.mult), reads=[a, fs], writes=[ytm])
                    else:
                        a0, a1 = acc[0][b], acc[1][b]
                        ft, fo = fin_t[b], fin_o[b]
                        fw.op("vector", lambda e: e.reciprocal(out=fs.ap[:, 0:1], in_=a0.ap[:, 128:129]), reads=[a0], writes=[fs])
                        fw.op("vector", lambda e: e.reciprocal(out=fs.ap[:, 1:2], in_=a1.ap[:, 128:129]), reads=[a1], writes=[fs])
                        fw.op("vector", lambda e: e.tensor_tensor(out=fs.ap[:, 2:3], in0=fs.ap[:, 1:2], in1=lamw.ap[:, 0:1], op=ALU.mult),
                              reads=[fs, lamw], writes=[fs])
                        fw.op("vector", lambda e: e.tensor_scalar(out=ft.ap, in0=a0.ap[:, 0:128], scalar1=fs.ap[:, 0:1], scalar2=None,
                                                                  op0=ALU.mult), reads=[a0, fs], writes=[ft])
                        fw.op("vector", lambda e: e.scalar_tensor_tensor(out=fo.ap, in0=a1.ap[:, 0:128], scalar=fs.ap[:, 2:3],
                                                                         in1=ft.ap, op0=ALU.mult, op1=ALU.add),
                              reads=[a1, fs, ft], writes=[fo])
                        fw.op("scalar", lambda e: e.activation(out=fin_j.ap, in_=fo.ap, func=AF.Square, accum_out=fs.ap[:, 3:4]),
                              reads=[fo], writes=[fin_j, fs])
                        fw.op("scalar", lambda e: e.activation(out=fs.ap[:, 4:5], in_=fs.ap[:, 3:4], func=AF.Ln, scale=1.0 / 128, bias=EPS),
                              reads=[fs], writes=[fs])
                        fw.op("scalar", lambda e: e.activation(out=fs.ap[:, 5:6], in_=fs.ap[:, 4:5], func=AF.Exp, scale=-0.5),
                              reads=[fs], writes=[fs])
                        fw.op("vector", lambda e: e.scalar_tensor_tensor(
                            out=ytm.ap[:, qb, u * 128:(u + 1) * 128], in0=fo.ap, scalar=fs.ap[:, 5:6], in1=sublnw.ap,
                            op0=ALU.mult, op1=ALU.mult), reads=[fo, fs, sublnw], writes=[ytm])

        rmsnorm_to(hnT, 0)
        for u in units:
            attn_unit(u)
        transpose_ytm_to_hnT()
        out_proj(about_d)
        fw.barrier()
        scope.close()

    silu_t = sq

    def ffn(layer):
        rmsnorm_to(hnT, 1 + 2 * layer)
        for (b0, nb) in ((0, 8), (8, 7), (15, 7)):
            for hb in range(nb):
                col = (b0 + hb) * 128
                wg = loadw(wgu_d[layer, :, col:col + 128], 8)
                wu = loadw(wgu_d[layer, :, FFN_H + col:FFN_H + col + 128], 8)
                for tg in range(4):
                    pg = bank()
                    pu = bank()
                    for c in range(8):
                        mm(pg.ap, wg.ap[:, c, :], hnT.ap[:, c, tg * 512:(tg + 1) * 512], c == 0, c == 7, [wg, hnT], [pg])
                    for c in range(8):
                        mm(pu.ap, wu.ap[:, c, :], hnT.ap[:, c, tg * 512:(tg + 1) * 512], c == 0, c == 7, [wu, hnT], [pu])
                    st = silu_t[tg % 2]
                    fw.op("scalar", lambda e: e.activation(out=st.ap, in_=pg.ap, func=AF.Silu), reads=[pg], writes=[st])
                    fw.op("vector", lambda e: e.tensor_tensor(out=actT.ap[:, hb, tg * 512:(tg + 1) * 512], in0=st.ap, in1=pu.ap,
                                                              op=ALU.mult), reads=[st, pu], writes=[actT])
            for cb in range(8):
                wd = loadw(wdn_d[layer, b0 * 128:(b0 + nb) * 128, cb * 128:(cb + 1) * 128], nb)
                for tg in range(4):
                    pb = bank()
                    for hb in range(nb):
                        mm(pb.ap, wd.ap[:, hb, :], actT.ap[:, hb, tg * 512:(tg + 1) * 512], hb == 0, hb == nb - 1, [wd, actT], [pb])
                    fw.op("vector", lambda e: e.tensor_tensor(out=hT.ap[:, cb, tg * 512:(tg + 1) * 512],
                                                              in0=hT.ap[:, cb, tg * 512:(tg + 1) * 512], in1=pb.ap, op=ALU.add),
                          reads=[hT, pb], writes=[hT])

    def final_store(s):
        sc = ExitStack()
        sbx = scoped_sb(sc)
        onT = [sbx([128, 8, 128], F32, "onT") for _ in range(2)]
        ost = [sbx([128, D], F32, "ost") for _ in range(2)]
        for tg in range(4):
            rms_stats(tg)
            for tt in range(4):
                t = tg * 4 + tt
                o_n = onT[t % 2]
                fw.op("vector", lambda e: e.scalar_tensor_tensor(
                    out=o_n.ap, in0=hT.ap[:, :, t * 128:(t + 1) * 128], scalar=1.0,
                    in1=rstd.ap[:, tt * 128:(tt + 1) * 128].unsqueeze(1).broadcast_to([128, 8, 128]),
                    op0=ALU.mult, op1=ALU.mult), reads=[hT, rstd], writes=[o_n])
                fw.op("vector", lambda e: e.tensor_tensor(
                    out=o_n.ap, in0=o_n.ap, in1=gam.ap[:, 4, :].unsqueeze(2).broadcast_to([128, 8, 128]), op=ALU.mult),
                    reads=[o_n, gam], writes=[o_n])
                os_ = ost[t % 2]
                for g in range(2):
                    pb = bank()
                    for cc in range(4):
                        c = g * 4 + cc
                        mm(pb.ap[:, cc * 128:(cc + 1) * 128], o_n.ap[:, c, :], ident_f, True, True, [o_n, cst], [pb])
                    evac(os_.ap[:, g * 512:(g + 1) * 512], pb.ap, [pb], [os_])
                fw.dma("sync", out_d[s, t * 128:(t + 1) * 128, :], os_.ap, reads=[os_], sbuf=os_)
        fw.barrier()
        sc.close()

    U_f = cst.ap[:, C_U:C_U + 128]
    NEG_f = cst.ap[:, C_NEG:C_NEG + 128]

    def out_proj_tiled(w_dram):
        with ExitStack() as sc:
            sbl = scoped_sb(sc)
            yT = [sbl([128, 8, 512], BF16, "yT") for _ in range(2)]
            for tg in range(4):
                y_ = yT[tg % 2]
                for tt in range(4):
                    t = tg * 4 + tt
                    for g in range(2):
                        pb = bank()
                        for cc in range(4):
                            c = g * 4 + cc
                            mm(pb.ap[:, cc * 128:(cc + 1) * 128], ytm.ap[:, t, c * 128:(c + 1) * 128], ident_b, True, True,
                               [ytm, cstb], [pb])
                        evac(y_.ap[:, g * 4:(g + 1) * 4, tt * 128:(tt + 1) * 128],
                             pb.ap.rearrange("p (c n) -> p c n", c=4), [pb], [y_])
                for cb in range(8):
                    wb = loadw(w_dram[:, cb * 128:(cb + 1) * 128], 8)
                    pb = bank()
                    for c in range(8):
                        mm(pb.ap, wb.ap[:, c, :], y_.ap[:, c, :], c == 0, c == 7, [wb, y_], [pb])
                    fw.op("vector", lambda e: e.tensor_tensor(out=hT.ap[:, cb, tg * 512:(tg + 1) * 512],
                                                              in0=hT.ap[:, cb, tg * 512:(tg + 1) * 512], in1=pb.ap, op=ALU.add),
                          reads=[hT, pb], writes=[hT])
            fw.barrier()

    def layer1_mix():
        scope = ExitStack()
        sbl = scoped_sb(scope)
        rmsnorm_to(hnT, 2)
        V = lambda fn, r, w: fw.op("vector", fn, reads=r, writes=w)
        A = lambda fn, r, w: fw.op("scalar", fn, reads=r, writes=w)
        sm = sbl([128, NT, 24], F32, "sm")
        par = sbl([128, 64], F32, "par")
        for (o, n, d_) in ((0, 16, cdtb_d), (16, 16, calog_d), (32, 16, cdskip_d), (48, 4, dib_d), (52, 4, dfb_d)):
            fw.dma("sync", par.ap[:, o:o + n], d_.partition_broadcast(128), writes=[par], sbuf=par)
        wdt = loadw(cdin_d[:, 2560:2576], 8, 16)
        wif = loadw(cdin_d[:, 5648:5656], 8, 8)
        pb = bank()
        for t in range(NT):
            for c in range(8):
                mm(pb.ap[:, t * 24:t * 24 + 16], hnT.ap[:, c, t * 128:(t + 1) * 128], wdt.ap[:, c, 0:16], c == 0, c == 7,
                   [wdt, hnT], [pb])
            for c in range(8):
                mm(pb.ap[:, t * 24 + 16:t * 24 + 24], hnT.ap[:, c, t * 128:(t + 1) * 128], wif.ap[:, c, 0:8], c == 0, c == 7,
                   [wif, hnT], [pb])
        evac(sm.ap, pb.ap[:, 0:NT * 24].rearrange("p (t n) -> p t n", t=NT), [pb], [sm])

        def bc_t(ap2d, n):
            return ap2d.unsqueeze(1).broadcast_to([128, NT, n])
        A(lambda e: e.activation(out=par.ap[:, 16:32], in_=par.ap[:, 16:32], func=AF.Exp), [par], [par])
        V(lambda e: e.tensor_scalar(out=par.ap[:, 16:32], in0=par.ap[:, 16:32], scalar1=-1.0, scalar2=None, op0=ALU.mult), [par], [par])
        V(lambda e: e.tensor_tensor(out=sm.ap[:, :, 0:16], in0=sm.ap[:, :, 0:16], in1=bc_t(par.ap[:, 0:16], 16), op=ALU.add), [sm, par], [sm])
        dtt = sbl([128, NT, 16], F32, "dtt")
        lndt = sbl([128, NT, 16], F32, "lndt")
        A(lambda e: e.activation(out=dtt.ap, in_=sm.ap[:, :, 0:16], func=AF.Exp), [sm], [dtt])
        A(lambda e: e.activation(out=dtt.ap, in_=dtt.ap, func=AF.Ln, bias=1.0), [dtt], [dtt])
        A(lambda e: e.activation(out=lndt.ap, in_=dtt.ap, func=AF.Ln), [dtt], [lndt])
        g20 = sbl([128, NT, 20], F32, "g20")
        V(lambda e: e.tensor_tensor(out=g20.ap[:, :, 0:16], in0=dtt.ap, in1=bc_t(par.ap[:, 16:32], 16), op=ALU.mult), [dtt, par], [g20])
        V(lambda e: e.tensor_tensor(out=sm.ap[:, :, 20:24], in0=sm.ap[:, :, 20:24], in1=bc_t(par.ap[:, 52:56], 4), op=ALU.add), [sm, par], [sm])
        A(lambda e: e.activation(out=g20.ap[:, :, 16:20], in_=sm.ap[:, :, 20:24], func=AF.Exp, scale=-1.0), [sm], [g20])
        A(lambda e: e.activation(out=g20.ap[:, :, 16:20], in_=g20.ap[:, :, 16:20], func=AF.Ln, bias=1.0), [g20], [g20])
        V(lambda e: e.tensor_scalar(out=g20.ap[:, :, 16:20], in0=g20.ap[:, :, 16:20], scalar1=-1.0, scalar2=None, op0=ALU.mult), [g20], [g20])
        V(lambda e: e.tensor_tensor(out=sm.ap[:, :, 16:20], in0=sm.ap[:, :, 16:20], in1=bc_t(par.ap[:, 48:52], 4), op=ALU.add), [sm, par], [sm])
        acum = sbl([128, NT, 20], F32, "acum")
        tot = sbl([128, NT, 20], F32, "tot")
        bias = sbl([128, NT, 20], F32, "bias")
        pb = bank()
        mm(pb.ap[:, 0:320], U_f, g20.ap.rearrange("p t n -> p (t n)"), True, True, [cst, g20], [pb])
        evac(acum.ap, pb.ap[:, 0:320].rearrange("p (t n) -> p t n", t=NT), [pb], [acum])
        pb = bank()
        mm(pb.ap[:, 0:320], ones_f, g20.ap.rearrange("p t n -> p (t n)"), True, True, [cst, g20], [pb])
        evac(tot.ap, pb.ap[:, 0:320].rearrange("p (t n) -> p t n", t=NT), [pb], [tot])
        for t in range(1, NT):
            V(lambda e: e.tensor_tensor(out=tot.ap[:, t, :], in0=tot.ap[:, t, :], in1=tot.ap[:, t - 1, :], op=ALU.add), [tot], [tot])
        V(lambda e: e.tensor_tensor(out=acum.ap[:, 1:NT, :], in0=acum.ap[:, 1:NT, :], in1=tot.ap[:, 0:NT - 1, :], op=ALU.add), [acum, tot], [acum])
        V(lambda e: e.tensor_tensor(out=bias.ap[:, :, 0:16], in0=lndt.ap, in1=acum.ap[:, :, 0:16], op=ALU.subtract), [lndt, acum], [bias])
        V(lambda e: e.tensor_tensor(out=bias.ap[:, :, 16:20], in0=sm.ap[:, :, 16:20], in1=acum.ap[:, :, 16:20], op=ALU.subtract), [sm, acum], [bias])

        xpad = sbl([128, 515], F32, "xpad")
        cv = sbl([128, 512], F32, "cv")
        cw = sbl([128, 8], F32, "cw")
        Rb = sbl([128, 4, 128], F32, "Rb")
        Arow = sbl([128, 4, 128], F32, "Arow")
        Eb = [sbl([128, 128], F32, "Eb") for _ in range(2)]
        Mb = [sbl([128, 128], BF16, "Mb") for _ in range(2)]
        fT = sbl([128, T], BF16, "fT")
        tmp = [sbl([128, 256], F32, "tmp") for _ in range(3)]
        fsm = sbl([128, 8], F32, "fsm")
        ssum = sbl([128, NT, 4], F32, "ssum")

        def proj_fm_conv(col0, convw_d, convb_d, ch0, dst):
            wb = loadw(cdin_d[:, col0:col0 + 128], 8)
            fw.dma("sync", cw.ap[:, 0:4], convw_d[:, ch0:ch0 + 128].rearrange("k c -> c k"), writes=[cw], sbuf=cw,
                   allow_slow_non_contiguous=True)
            fw.dma("sync", cw.ap[:, 4:5], convb_d[:, ch0:ch0 + 128].rearrange("o c -> c o"), writes=[cw], sbuf=cw,
                   allow_slow_non_contiguous=True)
            V(lambda e: e.memset(xpad.ap[:, 0:3], 0.0), [], [xpad])
            for tg in range(4):
                pb = bank()
                for c in range(8):
                    mm(pb.ap, wb.ap[:, c, :], hnT.ap[:, c, tg * 512:(tg + 1) * 512], c == 0, c == 7, [wb, hnT], [pb])
                A(lambda e: e.activation(out=xpad.ap[:, 3:515], in_=pb.ap, func=AF.Copy), [pb], [xpad])
                V(lambda e: e.tensor_scalar(out=cv.ap, in0=xpad.ap[:, 0:512], scalar1=cw.ap[:, 0:1], scalar2=None, op0=ALU.mult),
                  [xpad, cw], [cv])
                for k_ in range(1, 4):
                    V(lambda e: e.scalar_tensor_tensor(out=cv.ap, in0=xpad.ap[:, k_:k_ + 512], scalar=cw.ap[:, k_:k_ + 1], in1=cv.ap,
                                                       op0=ALU.mult, op1=ALU.add), [xpad, cw, cv], [cv])
                A(lambda e: e.activation(out=dst.ap[:, tg * 512:(tg + 1) * 512], in_=cv.ap, func=AF.Silu, bias=cw.ap[:, 4:5]),
                  [cv, cw], [dst])
                V(lambda e: e.tensor_copy(out=xpad.ap[:, 0:3], in_=xpad.ap[:, 512:515]), [xpad], [xpad])

        def proj_tm(col0, dst, dcol0):
            wb = loadw(cdin_d[:, col0:col0 + 128], 8)
            for t4 in range(4):
                pb = bank()
                for tt in range(4):
                    t = t4 * 4 + tt
                    for c in range(8):
                        mm(pb.ap[:, tt * 128:(tt + 1) * 128], hnT.ap[:, c, t * 128:(t + 1) * 128], wb.ap[:, c, :], c == 0, c == 7,
                           [wb, hnT], [pb])
                evac(dst.ap[:, t4 * 4:(t4 + 1) * 4, dcol0:dcol0 + 128], pb.ap.rearrange("p (t d) -> p t d", t=4), [pb], [dst])

        def fm_to_tm(src, dst, dcol0):
            for t4 in range(4):
                pb = bank()
                for tt in range(4):
                    t = t4 * 4 + tt
                    mm(pb.ap[:, tt * 128:(tt + 1) * 128], src.ap[:, t * 128:(t + 1) * 128], ident_b, True, True, [src, cstb], [pb])
                evac(dst.ap[:, t4 * 4:(t4 + 1) * 4, dcol0:dcol0 + 128], pb.ap.rearrange("p (t d) -> p t d", t=4), [pb], [dst])

        kk = [0]

        def decay_attn(h0, nh, gT_fn, g_reads, v_fn, v_buf, W, w, finalize):
            for qb in range(NT):
                V(lambda e: e.tensor_tensor(out=Rb.ap[:, 0:nh, :], in0=ident_f.unsqueeze(1).broadcast_to([128, nh, 128]),
                                            in1=acum.ap[:, qb, h0:h0 + nh].unsqueeze(2).broadcast_to([128, nh, 128]), op=ALU.mult),
                  [cst, acum], [Rb])
                pr = PB[6]
                mm(pr.ap[:, 0:nh * 128], ones_f, Rb.ap[:, 0:nh, :].rearrange("p h l -> p (h l)"), True, True, [cst, Rb], [pr])
                A(lambda e: e.activation(out=Arow.ap[:, 0:nh, :], in_=pr.ap[:, 0:nh * 128].rearrange("p (h l) -> p h l", h=nh), func=AF.Copy),
                  [pr], [Arow])
                V(lambda e: e.tensor_tensor(out=Rb.ap[:, 0:nh, :], in0=Arow.ap[:, 0:nh, :],
                                            in1=NEG_f.unsqueeze(1).broadcast_to([128, nh, 128]), op=ALU.add), [Arow, cst], [Rb])
                acc = PB[qb % 2]
                V(lambda e: e.memset(acc.ap[:, 0:nh * W], 0.0), [], [acc])
                for j in range(qb + 1):
                    pg = PB[2 + (j % 2)]
                    gT_fn(j, qb, pg)
                    src = Rb if j == qb else Arow
                    for hi in range(nh):
                        E_ = Eb[kk[0] % 2]
                        M_ = Mb[kk[0] % 2]
                        kk[0] += 1
                        A(lambda e: e.activation(out=E_.ap, in_=src.ap[:, hi, :], func=AF.Exp,
                                                 bias=bias.ap[:, j, h0 + hi:h0 + hi + 1]), [src, bias], [E_])
                        V(lambda e: e.tensor_tensor(out=M_.ap, in0=pg.ap[:, 0:128], in1=E_.ap, op=ALU.mult), [pg, E_], [M_])
                        mm(acc.ap[:, hi * W:hi * W + w], M_.ap, v_fn(hi, j), False, False, [M_, v_buf], [acc])
                finalize(qb, acc)

        with ExitStack() as scC:
            sbc = scoped_sb(scC)
            bmT = sbc([128, T], BF16, "bmT")
            cmT = sbc([128, T], BF16, "cmT")
            xs_tm = sbc([128, NT, 256], BF16, "xs_tm")
            for cu in range(4):
                g = cu // 2
                if cu % 2 == 0:
                    proj_fm_conv(1024 + 1024 + g * 128, cconvw_d, cconvb_d, 1024 + g * 128, bmT)
                    proj_fm_conv(1024 + 1280 + g * 128, cconvw_d, cconvb_d, 1280 + g * 128, cmT)
                for blk in range(2):
                    ch0 = cu * 256 + blk * 128
                    proj_fm_conv(1024 + ch0, cconvw_d, cconvb_d, ch0, fT)
                    fm_to_tm(fT, xs_tm, blk * 128)
                    proj_tm(ch0, ytm, cu * 256 + blk * 128)

                def gT_c(j, qb, pg):
                    mm(pg.ap[:, 0:128], bmT.ap[:, j * 128:(j + 1) * 128], cmT.ap[:, qb * 128:(qb + 1) * 128], True, True,
                       [bmT, cmT], [pg])

                def fin_c(qb, acc, cu=cu):
                    y_, sz, _ = tmp
                    for hi in range(4):
                        V(lambda e: e.scalar_tensor_tensor(out=y_.ap[:, hi * 64:(hi + 1) * 64], in0=xs_tm.ap[:, qb, hi * 64:(hi + 1) * 64],
                                                           scalar=par.ap[:, 32 + cu * 4 + hi:32 + cu * 4 + hi + 1],
                                                           in1=acc.ap[:, hi * 64:(hi + 1) * 64], op0=ALU.mult, op1=ALU.add),
                          [xs_tm, par, acc], [y_])
                    A(lambda e: e.activation(out=sz.ap, in_=ytm.ap[:, qb, cu * 256:(cu + 1) * 256], func=AF.Silu), [ytm], [sz])
                    V(lambda e: e.tensor_tensor(out=ytm.ap[:, qb, cu * 256:(cu + 1) * 256], in0=y_.ap, in1=sz.ap, op=ALU.mult),
                      [y_, sz], [ytm])
                    A(lambda e: e.activation(out=sz.ap, in_=ytm.ap[:, qb, cu * 256:(cu + 1) * 256], func=AF.Square,
                                             accum_out=ssum.ap[:, qb, cu:cu + 1]), [ytm], [sz, ssum])

                decay_attn(cu * 4, 4, gT_c, [bmT, cmT], lambda hi, j: xs_tm.ap[:, j, hi * 64:(hi + 1) * 64], xs_tm, 64, 64, fin_c)
            fw.barrier()
        with ExitStack() as scN:
            sbn = scoped_sb(scN)
            cnw = sbn([128, D], F32, "cnw")
            rs = sbn([128, NT], F32, "rs")
            fw.dma("sync", cnw.ap, cnormw_d.partition_broadcast(128), writes=[cnw], sbuf=cnw)
            V(lambda e: e.reduce_sum(out=rs.ap, in_=ssum.ap, axis=mybir.AxisListType.X), [ssum], [rs])
            A(lambda e: e.activation(out=rs.ap, in_=rs.ap, func=AF.Ln, scale=1.0 / D, bias=EPS), [rs], [rs])
            A(lambda e: e.activation(out=rs.ap, in_=rs.ap, func=AF.Exp, scale=-0.5), [rs], [rs])
            for qb in range(NT):
                V(lambda e: e.scalar_tensor_tensor(out=ytm.ap[:, qb, :], in0=ytm.ap[:, qb, :], scalar=rs.ap[:, qb:qb + 1], in1=cnw.ap,
                                                   op0=ALU.mult, op1=ALU.mult), [ytm, rs, cnw], [ytm])
            fw.barrier()
        out_proj_tiled(cdout_d[0:D, :])

        with ExitStack() as scD:
            sbd = scoped_sb(scD)
            qT2 = sbd([128, 2, T], BF16, "qT2")
            kT2 = sbd([128, 2, T], BF16, "kT2")
            v_tm = sbd([128, NT, 258], BF16, "v_tm")
            dnw = sbd([128, 256], F32, "dnw")
            V(lambda e: e.memset(v_tm.ap[:, :, 256:258], 1.0), [], [v_tm])
            for h in range(4):
                fw.dma("sync", dnw.ap, dnormw_d[:, h * 256:(h + 1) * 256].partition_broadcast(128), writes=[dnw], sbuf=dnw)
                for blk in range(2):
                    ch0 = h * 256 + blk * 128
                    proj_fm_conv(2576 + ch0, dconvw_d, dconvb_d, ch0, fT)
                    wq_ = loadw(dwq_d[ch0 // 128], 1)
                    wk_ = loadw(dwk_d[ch0 // 128], 1)
                    for tg in range(4):
                        pb = bank()
                        mm(pb.ap, wq_.ap[:, 0, :], fT.ap[:, tg * 512:(tg + 1) * 512], True, True, [wq_, fT], [pb])
                        evac(qT2.ap[:, blk, tg * 512:(tg + 1) * 512], pb.ap, [pb], [qT2])
                        pb = bank()
                        mm(pb.ap, wk_.ap[:, 0, :], fT.ap[:, tg * 512:(tg + 1) * 512], True, True, [wk_, fT], [pb])
                        A(lambda e: e.activation(out=kT2.ap[:, blk, tg * 512:(tg + 1) * 512], in_=pb.ap, func=AF.Copy, scale=0.0625),
                          [pb], [kT2])
                    proj_tm(3600 + ch0, v_tm, blk * 128)
                    proj_tm(4624 + ch0, ytm, h * 256 + blk * 128)

                def gT_d(j, qb, pg):
                    for blk in range(2):
                        mm(pg.ap[:, 0:128], kT2.ap[:, blk, j * 128:(j + 1) * 128], qT2.ap[:, blk, qb * 128:(qb + 1) * 128],
                           blk == 0, blk == 1, [kT2, qT2], [pg])

                def fin_d(qb, acc, h=h):
                    hd, sg, t1 = tmp
                    V(lambda e: e.tensor_scalar(out=fsm.ap[:, 5:6], in0=acc.ap[:, 256:257], scalar1=-1.0, scalar2=None, op0=ALU.mult),
                      [acc], [fsm])
                    V(lambda e: e.tensor_tensor(out=fsm.ap[:, 0:1], in0=acc.ap[:, 256:257], in1=fsm.ap[:, 5:6], op=ALU.max), [acc, fsm], [fsm])
                    V(lambda e: e.tensor_scalar(out=fsm.ap[:, 0:1], in0=fsm.ap[:, 0:1], scalar1=1.0, scalar2=None, op0=ALU.max), [fsm], [fsm])
                    V(lambda e: e.reciprocal(out=fsm.ap[:, 1:2], in_=fsm.ap[:, 0:1]), [fsm], [fsm])
                    V(lambda e: e.tensor_scalar(out=hd.ap, in0=acc.ap[:, 0:256], scalar1=fsm.ap[:, 1:2], scalar2=None, op0=ALU.mult),
                      [acc, fsm], [hd])
                    A(lambda e: e.activation(out=t1.ap, in_=hd.ap, func=AF.Square, accum_out=fsm.ap[:, 2:3]), [hd], [t1, fsm])
                    A(lambda e: e.activation(out=fsm.ap[:, 3:4], in_=fsm.ap[:, 2:3], func=AF.Ln, scale=1.0 / 256, bias=EPS), [fsm], [fsm])
                    A(lambda e: e.activation(out=fsm.ap[:, 4:5], in_=fsm.ap[:, 3:4], func=AF.Exp, scale=-0.5), [fsm], [fsm])
                    A(lambda e: e.activation(out=sg.ap, in_=ytm.ap[:, qb, h * 256:(h + 1) * 256], func=AF.Sigmoid), [ytm], [sg])
                    V(lambda e: e.scalar_tensor_tensor(out=t1.ap, in0=hd.ap, scalar=fsm.ap[:, 4:5], in1=dnw.ap, op0=ALU.mult, op1=ALU.mult),
                      [hd, fsm, dnw], [t1])
                    V(lambda e: e.tensor_tensor(out=ytm.ap[:, qb, h * 256:(h + 1) * 256], in0=t1.ap, in1=sg.ap, op=ALU.mult),
                      [t1, sg], [ytm])

                decay_attn(16 + h, 1, gT_d, [kT2, qT2], lambda hi, j: v_tm.ap[:, j, 0:257], v_tm, 257, 257, fin_d)
            fw.barrier()
        out_proj_tiled(cdout_d[D:2 * D, :])
        fw.barrier()
        scope.close()

    for s in range(nseq):
        load_x(s)
        if do_l0:
            layer0_mix()
            if do_ffn:
                ffn(0)
        if do_l1:
            layer1_mix()
            if do_ffn:
                ffn(1)
        final_store(s)
    fw.barrier()
    print("program: ops=%d waits=%d" % (fw.nops, fw.nwaits))
    return nc


def block_diag(w):
    w = np.asarray(w, np.float32)
    o = np.zeros((8, 128, 128), np.float32)
    for n in range(256):
        b, r = divmod(n, 32)
        o[b, 4 * r:4 * r + 4, 4 * r:4 * r + 4] = w[n]
    return o


NSEQ = 4
_prog_cache = {}


def make_in_maps(inputs, nseq, ncores):
    f = lambda a: np.ascontiguousarray(np.asarray(a, np.float32))
    cf, cb = make_consts()
    shared = {
        "consts": cf, "constsb": cb,
        "mix_norm_w": f(inputs["mix_norm_w"]), "ffn_norm_w": f(inputs["ffn_norm_w"]),
        "final_norm_w": f(inputs["final_norm_w"]).reshape(1, D),
        "ab_w_in": f(inputs["ab_w_in"][0]), "ab_w_out": f(inputs["ab_w_out"][0]),
        "diff_lq1": f(inputs["diff_lq1"]), "diff_lk1": f(inputs["diff_lk1"]),
        "diff_lq2": f(inputs["diff_lq2"]), "diff_lk2": f(inputs["diff_lk2"]),
        "diff_subln_w": f(inputs["diff_subln_w"]),
        "cd_w_in": f(inputs["cd_w_in"][0]),
        "c_conv_w": f(inputs["c_conv_w"][0]), "c_conv_b": f(inputs["c_conv_b"]),
        "c_dt_bias": f(inputs["c_dt_bias"]), "c_a_log": f(inputs["c_a_log"]), "c_d_skip": f(inputs["c_d_skip"]),
        "c_norm_w": f(inputs["c_norm_w"]),
        "d_conv_w": f(inputs["d_conv_w"][0]), "d_conv_b": f(inputs["d_conv_b"]),
        "d_wq_bd": block_diag(inputs["d_wq"][0]), "d_wk_bd": block_diag(inputs["d_wk"][0]),
        "d_i_bias": f(inputs["d_i_bias"]), "d_f_bias": f(inputs["d_f_bias"]),
        "d_norm_w": f(inputs["d_norm_w"]),
        "cd_w_out": f(inputs["cd_w_out"][0]),
        "ffn_w_gate_up": f(inputs["ffn_w_gate_up"]), "ffn_w_down": f(inputs["ffn_w_down"]),
    }
    x = f(inputs["x"])
    maps = []
    for c in range(ncores):
        m = dict(shared)
        m["x"] = np.ascontiguousarray(x[c * nseq:(c + 1) * nseq])
        maps.append(m)
    return maps


def kernel(**inputs):
    ncores = 8
    nseq = NSEQ
    nc = build_program(nseq)
    in_maps = make_in_maps(inputs, nseq, ncores)
    res = run_bass_kernel_spmd(nc, in_maps, core_ids=list(range(ncores)))
    return np.concatenate([np.asarray(r["out"], np.float32) for r in res.results], axis=0)
```

```python
import math
from contextlib import ExitStack
import numpy as np
import concourse.bass as bass
import concourse.mybir as mybir
from concourse.bass_utils import run_bass_kernel_spmd

F32 = mybir.dt.float32
BF16 = mybir.dt.bfloat16
AF = mybir.ActivationFunctionType
ALU = mybir.AluOpType

T = 2048
D = 1024
NT = 16
FFN_H = 2816
EPS = 1e-6
AB_COLS = 3072
CD_COLS = 5656
NEGM = -30000.0


class _Dep:
    def __init__(self):
        self.w = {}
        self.r = {}
        self.dsem = None


class Buf:
    def __init__(self, ap, name="", owner=None):
        self.ap = ap
        self.name = name
        self.d = owner.d if owner is not None else _Dep()

    w = property(lambda self: self.d.w, lambda self, v: setattr(self.d, "w", v))
    r = property(lambda self: self.d.r, lambda self, v: setattr(self.d, "r", v))
    dsem = property(lambda self: self.d.dsem, lambda self, v: setattr(self.d, "dsem", v))


class Fw:
    ENG = ["tensor", "vector", "scalar", "gpsimd", "sync"]

    def __init__(self, nc):
        self.nc = nc
        self.sem = {}
        self.cnt = {}
        self.waited = {}
        self.semobj = {}
        for e in self.ENG:
            s = nc.alloc_semaphore(name="s_" + e)
            self.sem[e] = s
            self.cnt[e] = 0
            self.waited[e] = {}
        self.dma_total = {}
        self.nwaits = 0
        self.nops = 0

    def eng(self, e):
        return getattr(self.nc, e)

    def _wait(self, e, deps):
        w = self.waited[e]
        for k, v in deps.items():
            if k[0] == 'e':
                if k[1] == e and e == "tensor":
                    continue
                sem = self.sem[k[1]]
            else:
                sem = self.semobj[k[1]]
                v = max(v, self.dma_total[k[1]])
            if w.get(k, 0) >= v:
                continue
            self.eng(e).wait_ge(sem, v)
            self.nwaits += 1
            w[k] = v

    @staticmethod
    def _merge(d, k, v):
        if d.get(k, 0) < v:
            d[k] = v

    def _deps(self, reads, writes):
        deps = {}
        for b in reads:
            for k, v in b.w.items():
                self._merge(deps, k, v)
        for b in writes:
            for k, v in b.w.items():
                self._merge(deps, k, v)
            for k, v in b.r.items():
                self._merge(deps, k, v)
        return deps

    def _record(self, key, val, reads, writes):
        for b in reads:
            self._merge(b.r, key, val)
        for b in writes:
            self._merge(b.w, key, val)
            b.r = {}

    def op(self, e, fn, reads=(), writes=()):
        self._wait(e, self._deps(reads, writes))
        ins = fn(self.eng(e))
        self.cnt[e] += 1
        ins.then_inc(self.sem[e], 1)
        self.nops += 1
        self._record(('e', e), self.cnt[e], reads, writes)
        return ins

    def dma(self, q, out_ap, in_ap, reads=(), writes=(), sbuf=None, **kw):
        self._wait(q, self._deps(reads, writes))
        if sbuf.dsem is None:
            sbuf.dsem = self.nc.alloc_semaphore(name="d_" + sbuf.name)
            self.semobj[id(sbuf.dsem)] = sbuf.dsem
            self.dma_total[id(sbuf.dsem)] = 0
        ins = self.eng(q).dma_start(out=out_ap, in_=in_ap, **kw)
        ins.then_inc(sbuf.dsem, 16)
        self.dma_total[id(sbuf.dsem)] += 16
        self._record(('d', id(sbuf.dsem)), self.dma_total[id(sbuf.dsem)], reads, writes)
        return ins

    def barrier(self):
        deps = {('e', e): self.cnt[e] for e in self.ENG if self.cnt[e] > 0}
        for k, v in self.dma_total.items():
            if v > 0:
                deps[('d', k)] = v
        for e in self.ENG:
            self._wait(e, dict(deps))


C_ID = 0
C_U = 128
C_NEG = 256
C_ONES = 384
C_TOT = 512
B_ID = 0
B_MA = 128
B_MB = B_MA + 2304
B_TOT = B_MB + 384


def make_consts():
    c = np.zeros((128, C_TOT), np.float32)
    cb = np.zeros((128, B_TOT), np.float32)
    s = np.arange(128)[:, None]
    c[:, C_ID:C_ID + 128] = np.eye(128, dtype=np.float32)
    cb[:, B_ID:B_ID + 128] = np.eye(128, dtype=np.float32)
    cc = np.arange(2304)[None, :]
    d = cc - 128 - s
    mult = ((d >= 0) & (d <= 128)).astype(np.float32)
    mult += ((d >= 0) & (d % 4 == 0) & (d <= 512)).astype(np.float32)
    mult += ((d >= 0) & (d % 16 == 0) & (d <= 2048)).astype(np.float32)
    cb[:, B_MA:B_MA + 2304] = mult
    cc = np.arange(384)[None, :]
    cb[:, B_MB:B_MB + 384] = ((cc - 128 - s) >= 0).astype(np.float32)
    l = np.arange(128)[None, :]
    c[:, C_U:C_U + 128] = (s <= l).astype(np.float32)
    c[:, C_NEG:C_NEG + 128] = np.where(s > l, NEGM, 0.0).astype(np.float32)
    c[:, C_ONES:C_ONES + 128] = 1.0
    return c, cb


def build_program(nseq, do_l0=True, do_l1=True, do_ffn=True, units=tuple(range(8)), ngroups=8, stage=9):
    nc = bass.Bass("TRN2", target_bir_lowering=False)
    fw = Fw(nc)

    def din(name, shape):
        return nc.dram_tensor(name, list(shape), F32, kind="ExternalInput").ap()

    x_d = din("x", [nseq, T, D])
    consts_d = din("consts", [128, C_TOT])
    constsb_d = din("constsb", [128, B_TOT])
    mixg_d = din("mix_norm_w", [2, D])
    ffng_d = din("ffn_norm_w", [2, D])
    fing_d = din("final_norm_w", [1, D])
    abin_d = din("ab_w_in", [D, AB_COLS])
    about_d = din("ab_w_out", [D, D])
    lq1_d = din("diff_lq1", [1, 64]); lk1_d = din("diff_lk1", [1, 64])
    lq2_d = din("diff_lq2", [1, 64]); lk2_d = din("diff_lk2", [1, 64])
    subln_d = din("diff_subln_w", [1, 128])
    cdin_d = din("cd_w_in", [D, CD_COLS])
    cconvw_d = din("c_conv_w", [4, 1536]); cconvb_d = din("c_conv_b", [1, 1536])
    cdtb_d = din("c_dt_bias", [1, 16]); calog_d = din("c_a_log", [1, 16]); cdskip_d = din("c_d_skip", [1, 16])
    cnormw_d = din("c_norm_w", [1, D])
    dconvw_d = din("d_conv_w", [4, D]); dconvb_d = din("d_conv_b", [1, D])
    dwq_d = din("d_wq_bd", [8, 128, 128]); dwk_d = din("d_wk_bd", [8, 128, 128])
    dib_d = din("d_i_bias", [1, 4]); dfb_d = din("d_f_bias", [1, 4])
    dnormw_d = din("d_norm_w", [1, D])
    cdout_d = din("cd_w_out", [2 * D, D])
    wgu_d = din("ffn_w_gate_up", [2, D, 2 * FFN_H])
    wdn_d = din("ffn_w_down", [2, FFN_H, D])
    out_d = nc.dram_tensor("out", [nseq, T, D], F32, kind="ExternalOutput").ap()

    cnt = [0]

    def sb(shape, dt=F32, name=None):
        cnt[0] += 1
        nm = (name or "t") + "_%d" % cnt[0]
        return Buf(nc.alloc_sbuf_tensor(nm, list(shape), dt).ap(), nm)

    def scoped_sb(scope):
        def f(shape, dt=F32, name=None):
            cnt[0] += 1
            nm = (name or "t") + "_%d" % cnt[0]
            return Buf(scope.enter_context(nc.sbuf_tensor(nm, list(shape), dt)).ap(), nm)
        return f

    hT = sb([128, 8, T], F32, "hT")
    cst = sb([128, C_TOT], F32, "cst")
    cstb = sb([128, B_TOT], BF16, "cstb")
    gam = sb([128, 5, 8], F32, "gam")
    PB = []
    for i in range(8):
        PB.append(Buf(nc.alloc_psum_tensor("pb%d" % i, [128, 512], F32).ap(), "pb%d" % i))
    pb_rr = [0]

    def bank():
        b = PB[pb_rr[0] % 8]
        pb_rr[0] += 1
        return b

    fw.dma("sync", cst.ap, consts_d, writes=[cst], sbuf=cst)
    fw.dma("gpsimd", cstb.ap, constsb_d, writes=[cstb], sbuf=cstb)
    for i, (g_d, row) in enumerate([(mixg_d, 0), (ffng_d, 0), (mixg_d, 1), (ffng_d, 1), (fing_d, 0)]):
        fw.dma("sync", gam.ap[:, i, :], g_d[row, :].rearrange("(c p) -> p c", p=128), writes=[gam], sbuf=gam,
               allow_slow_non_contiguous=True)
    ident_f = cst.ap[:, C_ID:C_ID + 128]
    ident_b = cstb.ap[:, B_ID:B_ID + 128]
    ones_f = cst.ap[:, C_ONES:C_ONES + 128]

    NW = 4
    wbf = [sb([128, 8, 128], BF16, "wbf") for _ in range(NW)]
    w_rr = [0]

    def loadw(dram_rows_cols, nchunk, ncols=128):
        i = w_rr[0] % NW
        w_rr[0] += 1
        bf = wbf[i]
        fw.dma("gpsimd", bf.ap[:, 0:nchunk, 0:ncols], dram_rows_cols.rearrange("(c p) n -> p c n", p=128),
               writes=[bf], sbuf=bf)
        return bf

    evac_rr = [0]

    def evac(out_ap, in_ap, reads, writes):
        evac_rr[0] += 1
        if evac_rr[0] % 2 == 0:
            fw.op("scalar", lambda e: e.activation(out=out_ap, in_=in_ap, func=AF.Copy), reads=reads, writes=writes)
        else:
            fw.op("vector", lambda e: e.tensor_copy(out=out_ap, in_=in_ap), reads=reads, writes=writes)

    def mm(out_ap, lhsT, rhs, start, stop, reads, writes):
        fw.op("tensor", lambda e: e.matmul(out_ap, lhsT=lhsT, rhs=rhs, start=start, stop=stop, skip_group_check=True),
              reads=reads, writes=writes)

    sq = [sb([128, 512], F32, "sq") for _ in range(2)]
    rstd = sb([128, 512], F32, "rstd")

    def rms_stats(tg):
        pb = bank()
        for c in range(8):
            s = sq[c % 2]
            fw.op("scalar", lambda e: e.activation(out=s.ap, in_=hT.ap[:, c, tg * 512:(tg + 1) * 512], func=AF.Square),
                  reads=[hT], writes=[s])
            mm(pb.ap, ones_f, s.ap, c == 0, c == 7, [s, cst], [pb])
        fw.op("scalar", lambda e: e.activation(out=rstd.ap, in_=pb.ap, func=AF.Ln, scale=1.0 / D, bias=EPS),
              reads=[pb], writes=[rstd])
        fw.op("scalar", lambda e: e.activation(out=rstd.ap, in_=rstd.ap, func=AF.Exp, scale=-0.5),
              reads=[rstd], writes=[rstd])

    def rmsnorm_to(dst, gi, dst_dtype_is_f32=False):
        for tg in range(4):
            rms_stats(tg)
            for c in range(8):
                fw.op("vector", lambda e: e.scalar_tensor_tensor(
                    out=dst.ap[:, c, tg * 512:(tg + 1) * 512], in0=hT.ap[:, c, tg * 512:(tg + 1) * 512],
                    scalar=gam.ap[:, gi, c:c + 1], in1=rstd.ap, op0=ALU.mult, op1=ALU.mult),
                    reads=[hT, gam, rstd], writes=[dst])

    hnT = sb([128, 8, T], BF16, "hnT")
    ytm = sb([128, NT, D], BF16, "ytm")
    actT = Buf(ytm.ap.rearrange("p t d -> p (t d)").rearrange("p (h t) -> p h t", h=8), "actT", owner=ytm)

    def load_x(s):
        sc = ExitStack()
        sbx = scoped_sb(sc)
        xin = [sbx([128, D], F32, "xin") for _ in range(2)]
        for t in range(NT):
            xt = xin[t % 2]
            fw.dma("sync", xt.ap, x_d[s, t * 128:(t + 1) * 128, :], writes=[xt], sbuf=xt)
            for g in range(2):
                pb = bank()
                for cc in range(4):
                    c = g * 4 + cc
                    mm(pb.ap[:, cc * 128:(cc + 1) * 128], xt.ap[:, c * 128:(c + 1) * 128], ident_f, True, True,
                       [xt, cst], [pb])
                evac(hT.ap[:, g * 4:(g + 1) * 4, t * 128:(t + 1) * 128],
                     pb.ap.rearrange("p (c n) -> p c n", c=4), [pb], [hT])
        fw.barrier()
        sc.close()

    def transpose_ytm_to_hnT():
        for t in range(NT):
            for g in range(2):
                pb = bank()
                for cc in range(4):
                    c = g * 4 + cc
                    mm(pb.ap[:, cc * 128:(cc + 1) * 128], ytm.ap[:, t, c * 128:(c + 1) * 128], ident_b, True, True,
                       [ytm, cstb], [pb])
                evac(hnT.ap[:, g * 4:(g + 1) * 4, t * 128:(t + 1) * 128],
                     pb.ap.rearrange("p (c n) -> p c n", c=4), [pb], [hnT])

    def out_proj(w_dram):
        for cb in range(8):
            wb = loadw(w_dram[:, cb * 128:(cb + 1) * 128], 8)
            for tg in range(4):
                pb = bank()
                for c in range(8):
                    mm(pb.ap, wb.ap[:, c, :], hnT.ap[:, c, tg * 512:(tg + 1) * 512], c == 0, c == 7, [wb, hnT], [pb])
                fw.op("vector", lambda e: e.tensor_tensor(out=hT.ap[:, cb, tg * 512:(tg + 1) * 512],
                                                          in0=hT.ap[:, cb, tg * 512:(tg + 1) * 512], in1=pb.ap, op=ALU.add),
                      reads=[hT, pb], writes=[hT])

    def layer0_mix():
        scope = ExitStack()
        sb = scoped_sb(scope)
        qT = sb([128, T], BF16, "qT")
        kT = sb([128, 2, T], BF16, "kT")
        vA = sb([128, NT, 2, 65], BF16, "vA")
        vB = sb([128, NT, 129], BF16, "vB")
        PT = [sb([128, 2, 256], BF16, "PT") for _ in range(4)]
        lamw = sb([128, 8], F32, "lamw")
        lqk = sb([128, 4, 64], F32, "lqk")
        sublnw = sb([128, 128], F32, "sublnw")
        fin_s = [sb([128, 8], F32, "fin_s") for _ in range(2)]
        fin_t = [sb([128, 128], F32, "fin_t") for _ in range(2)]
        fin_o = [sb([128, 128], F32, "fin_o") for _ in range(2)]
        fin_j = sb([128, 128], F32, "fin_j")

        fw.op("vector", lambda e: e.memset(kT.ap, 0.0), writes=[kT])
        fw.op("vector", lambda e: e.memset(vA.ap, 1.0), writes=[vA])
        fw.op("vector", lambda e: e.memset(vB.ap, 1.0), writes=[vB])
        for i, d_ in enumerate([lq1_d, lk1_d, lq2_d, lk2_d]):
            fw.dma("sync", lqk.ap[:, i, :], d_.partition_broadcast(128), writes=[lqk], sbuf=lqk)
        fw.dma("sync", sublnw.ap, subln_d.partition_broadcast(128), writes=[sublnw], sbuf=sublnw)
        lambda_init = 0.8 - 0.6 * math.exp(-0.3 * 0)
        fw.op("vector", lambda e: e.tensor_scalar(out=sublnw.ap, in0=sublnw.ap, scalar1=1.0 - lambda_init, scalar2=None,
                                                  op0=ALU.mult), reads=[sublnw], writes=[sublnw])
        fw.op("vector", lambda e: e.tensor_tensor(out=lqk.ap[:, 0, :], in0=lqk.ap[:, 0, :], in1=lqk.ap[:, 1, :], op=ALU.mult),
              reads=[lqk], writes=[lqk])
        fw.op("vector", lambda e: e.tensor_tensor(out=lqk.ap[:, 2, :], in0=lqk.ap[:, 2, :], in1=lqk.ap[:, 3, :], op=ALU.mult),
              reads=[lqk], writes=[lqk])
        fw.op("vector", lambda e: e.reduce_sum(out=lamw.ap[:, 1:2], in_=lqk.ap[:, 0, :], axis=mybir.AxisListType.X),
              reads=[lqk], writes=[lamw])
        fw.op("vector", lambda e: e.reduce_sum(out=lamw.ap[:, 2:3], in_=lqk.ap[:, 2, :], axis=mybir.AxisListType.X),
              reads=[lqk], writes=[lamw])
        fw.op("scalar", lambda e: e.activation(out=lamw.ap[:, 3:5], in_=lamw.ap[:, 1:3], func=AF.Exp), reads=[lamw], writes=[lamw])
        fw.op("vector", lambda e: e.tensor_tensor(out=lamw.ap[:, 0:1], in0=lamw.ap[:, 4:5], in1=lamw.ap[:, 3:4], op=ALU.subtract),
              reads=[lamw], writes=[lamw])
        fw.op("vector", lambda e: e.tensor_scalar(out=lamw.ap[:, 0:1], in0=lamw.ap[:, 0:1], scalar1=-lambda_init, scalar2=None,
                                                  op0=ALU.add), reads=[lamw], writes=[lamw])

        def attn_unit(u):
            isA = u < 4
            qoff = (0 if isA else 1536) + (u % 4) * 128
            koff = qoff + 512
            voff = qoff + 1024
            wq = loadw(abin_d[:, qoff:qoff + 128], 8)
            wk = loadw(abin_d[:, koff:koff + 128], 8)
            wv = loadw(abin_d[:, voff:voff + 128], 8)
            for (wb, dst) in ((wq, qT), (wk, kT)):
                for tg in range(4):
                    pb = bank()
                    for c in range(8):
                        mm(pb.ap, wb.ap[:, c, :], hnT.ap[:, c, tg * 512:(tg + 1) * 512], c == 0, c == 7, [wb, hnT], [pb])
                    if dst is qT:
                        evac(dst.ap[:, tg * 512:(tg + 1) * 512], pb.ap, [pb], [dst])
                    else:
                        evac(dst.ap[0:64, 0, tg * 512:(tg + 1) * 512], pb.ap[0:64, :], [pb], [dst])
                        evac(dst.ap[64:128, 1, tg * 512:(tg + 1) * 512], pb.ap[64:128, :], [pb], [dst])
            for t4 in range(4):
                pb = bank()
                for tt in range(4):
                    t = t4 * 4 + tt
                    for c in range(8):
                        mm(pb.ap[:, tt * 128:(tt + 1) * 128], hnT.ap[:, c, t * 128:(t + 1) * 128], wv.ap[:, c, :],
                           c == 0, c == 7, [wv, hnT], [pb])
                if isA:
                    evac(vA.ap[:, t4 * 4:(t4 + 1) * 4, :, 0:64], pb.ap.rearrange("p (t i d) -> p t i d", t=4, i=2), [pb], [vA])
                else:
                    evac(vB.ap[:, t4 * 4:(t4 + 1) * 4, 0:128], pb.ap.rearrange("p (t d) -> p t d", t=4), [pb], [vB])
            W = 65 if isA else 129
            if stage < 1:
                return
            steps = [(G, j) for G in range(ngroups) for j in range(2 * G + 2)]
            nst = len(steps)
            vbuf = vA if isA else vB

            def acc_of(G, i, b):
                return PB[2 * (G % 2) + i], b * W

            def emit_S(k):
                G, j = steps[k]
                sbk = PB[4 + (k % 4)]
                for i in range(2):
                    mm(sbk.ap[:, i * 256:(i + 1) * 256], kT.ap[:, i, j * 128:(j + 1) * 128],
                       qT.ap[:, G * 256:(G + 1) * 256], True, True, [kT, qT], [sbk])

            def finalize(G):
                for b in range(2):
                    qb = 2 * G + b
                    fs = fin_s[b]
                    if isA:
                        for i in range(2):
                            a, o = acc_of(G, i, b)
                            fw.op("vector", lambda e: e.reciprocal(out=fs.ap[:, i:i + 1], in_=a.ap[:, o + 64:o + 65]), reads=[a], writes=[fs])
                            fw.op("vector", lambda e: e.tensor_scalar(
                                out=ytm.ap[:, qb, u * 128 + 64 * i:u * 128 + 64 * i + 64], in0=a.ap[:, o:o + 64],
                                scalar1=fs.ap[:, i:i + 1], scalar2=None, op0=ALU.mult), reads=[a, fs], writes=[ytm])
                    else:
                        a0, o0 = acc_of(G, 0, b)
                        a1, o1 = acc_of(G, 1, b)
                        ft, fo = fin_t[b], fin_o[b]
                        fw.op("vector", lambda e: e.reciprocal(out=fs.ap[:, 0:1], in_=a0.ap[:, o0 + 128:o0 + 129]), reads=[a0], writes=[fs])
                        fw.op("vector", lambda e: e.reciprocal(out=fs.ap[:, 1:2], in_=a1.ap[:, o1 + 128:o1 + 129]), reads=[a1], writes=[fs])
                        fw.op("vector", lambda e: e.tensor_tensor(out=fs.ap[:, 2:3], in0=fs.ap[:, 1:2], in1=lamw.ap[:, 0:1], op=ALU.mult),
                              reads=[fs, lamw], writes=[fs])
                        fw.op("vector", lambda e: e.tensor_scalar(out=ft.ap, in0=a0.ap[:, o0:o0 + 128], scalar1=fs.ap[:, 0:1], scalar2=None,
                                                                  op0=ALU.mult), reads=[a0, fs], writes=[ft])
                        fw.op("vector", lambda e: e.scalar_tensor_tensor(out=fo.ap, in0=a1.ap[:, o1:o1 + 128], scalar=fs.ap[:, 2:3],
                                                                         in1=ft.ap, op0=ALU.mult, op1=ALU.add),
                              reads=[a1, fs, ft], writes=[fo])
                        fw.op("scalar", lambda e: e.activation(out=fin_j.ap, in_=fo.ap, func=AF.Square, accum_out=fs.ap[:, 3:4]),
                              reads=[fo], writes=[fin_j, fs])
                        fw.op("scalar", lambda e: e.activation(out=fs.ap[:, 4:5], in_=fs.ap[:, 3:4], func=AF.Ln, scale=1.0 / 128, bias=EPS),
                              reads=[fs], writes=[fs])
                        fw.op("scalar", lambda e: e.activation(out=fs.ap[:, 5:6], in_=fs.ap[:, 4:5], func=AF.Exp, scale=-0.5),
                              reads=[fs], writes=[fs])
                        fw.op("vector", lambda e: e.scalar_tensor_tensor(
                            out=ytm.ap[:, qb, u * 128:(u + 1) * 128], in0=fo.ap, scalar=fs.ap[:, 5:6], in1=sublnw.ap,
                            op0=ALU.mult, op1=ALU.mult), reads=[fo, fs, sublnw], writes=[ytm])

            LA = 2
            for k0 in range(min(LA, nst)):
                emit_S(k0)
            pending = None
            for k, (G, j) in enumerate(steps):
                if j == 0:
                    for i in range(2):
                        a, _ = acc_of(G, i, 0)
                        fw.op("vector", lambda e: e.memset(a.ap[:, 0:2 * W], 0.0), writes=[a])
                if k + LA < nst:
                    emit_S(k + LA)
                sbk = PB[4 + (k % 4)]
                pt = PT[k % 4]
                fw.op("scalar", lambda e: e.activation(out=pt.ap, in_=sbk.ap.rearrange("p (i n) -> p i n", i=2),
                                                       func=AF.Exp, scale=0.125), reads=[sbk], writes=[pt])
                d0 = 2 * G - j
                if isA:
                    m = cstb.ap[:, B_MA + 128 * (d0 + 1):B_MA + 128 * (d0 + 1) + 256]
                elif d0 <= 0:
                    m = cstb.ap[:, B_MB + 128 * (d0 + 1):B_MB + 128 * (d0 + 1) + 256]
                else:
                    m = None
                if m is not None:
                    mb_ = m.unsqueeze(1).broadcast_to([128, 2, 256])
                    fw.op("vector", lambda e: e.tensor_tensor(out=pt.ap, in0=pt.ap, in1=mb_, op=ALU.mult),
                          reads=[pt, cstb], writes=[pt])
                for b in range(2):
                    if 2 * G + b < j:
                        continue
                    for i in range(2):
                        rhs = vA.ap[:, j, i, :] if isA else vB.ap[:, j, :]
                        a, o = acc_of(G, i, b)
                        mm(a.ap[:, o:o + W], pt.ap[:, i, b * 128:(b + 1) * 128], rhs, False, False, [pt, vbuf], [a])
                if pending is not None and k >= pending[1]:
                    finalize(pending[0])
                    pending = None
                if j == 2 * G + 1:
                    if pending is not None:
                        finalize(pending[0])
                    pending = (G, k + 2)
            if pending is not None:
                finalize(pending[0])

        rmsnorm_to(hnT, 0)
        for u in units:
            attn_unit(u)
        transpose_ytm_to_hnT()
        out_proj(about_d)
        fw.barrier()
        scope.close()

    silu_t = sq

    def ffn(layer):
        rmsnorm_to(hnT, 1 + 2 * layer)
        for (b0, nb) in ((0, 8), (8, 7), (15, 7)):
            for hb in range(nb):
                col = (b0 + hb) * 128
                wg = loadw(wgu_d[layer, :, col:col + 128], 8)
                wu = loadw(wgu_d[layer, :, FFN_H + col:FFN_H + col + 128], 8)
                for tg in range(4):
                    pg = bank()
                    pu = bank()
                    for c in range(8):
                        mm(pg.ap, wg.ap[:, c, :], hnT.ap[:, c, tg * 512:(tg + 1) * 512], c == 0, c == 7, [wg, hnT], [pg])
                    for c in range(8):
                        mm(pu.ap, wu.ap[:, c, :], hnT.ap[:, c, tg * 512:(tg + 1) * 512], c == 0, c == 7, [wu, hnT], [pu])
                    st = silu_t[tg % 2]
                    fw.op("scalar", lambda e: e.activation(out=st.ap, in_=pg.ap, func=AF.Silu), reads=[pg], writes=[st])
                    fw.op("vector", lambda e: e.tensor_tensor(out=actT.ap[:, hb, tg * 512:(tg + 1) * 512], in0=st.ap, in1=pu.ap,
                                                              op=ALU.mult), reads=[st, pu], writes=[actT])
            for cb in range(8):
                wd = loadw(wdn_d[layer, b0 * 128:(b0 + nb) * 128, cb * 128:(cb + 1) * 128], nb)
                for tg in range(4):
                    pb = bank()
                    for hb in range(nb):
                        mm(pb.ap, wd.ap[:, hb, :], actT.ap[:, hb, tg * 512:(tg + 1) * 512], hb == 0, hb == nb - 1, [wd, actT], [pb])
                    fw.op("vector", lambda e: e.tensor_tensor(out=hT.ap[:, cb, tg * 512:(tg + 1) * 512],
                                                              in0=hT.ap[:, cb, tg * 512:(tg + 1) * 512], in1=pb.ap, op=ALU.add),
                          reads=[hT, pb], writes=[hT])

    def final_store(s):
        sc = ExitStack()
        sbx = scoped_sb(sc)
        onT = [sbx([128, 8, 128], F32, "onT") for _ in range(2)]
        ost = [sbx([128, D], F32, "ost") for _ in range(2)]
        for tg in range(4):
            rms_stats(tg)
            for tt in range(4):
                t = tg * 4 + tt
                o_n = onT[t % 2]
                fw.op("vector", lambda e: e.scalar_tensor_tensor(
                    out=o_n.ap, in0=hT.ap[:, :, t * 128:(t + 1) * 128], scalar=1.0,
                    in1=rstd.ap[:, tt * 128:(tt + 1) * 128].unsqueeze(1).broadcast_to([128, 8, 128]),
                    op0=ALU.mult, op1=ALU.mult), reads=[hT, rstd], writes=[o_n])
                fw.op("vector", lambda e: e.tensor_tensor(
                    out=o_n.ap, in0=o_n.ap, in1=gam.ap[:, 4, :].unsqueeze(2).broadcast_to([128, 8, 128]), op=ALU.mult),
                    reads=[o_n, gam], writes=[o_n])
                os_ = ost[t % 2]
                for g in range(2):
                    pb = bank()
                    for cc in range(4):
                        c = g * 4 + cc
                        mm(pb.ap[:, cc * 128:(cc + 1) * 128], o_n.ap[:, c, :], ident_f, True, True, [o_n, cst], [pb])
                    evac(os_.ap[:, g * 512:(g + 1) * 512], pb.ap, [pb], [os_])
                fw.dma("sync", out_d[s, t * 128:(t + 1) * 128, :], os_.ap, reads=[os_], sbuf=os_)
        fw.barrier()
        sc.close()

    U_f = cst.ap[:, C_U:C_U + 128]
    NEG_f = cst.ap[:, C_NEG:C_NEG + 128]

    def out_proj_tiled(w_dram):
        with ExitStack() as sc:
            sbl = scoped_sb(sc)
            yT = [sbl([128, 8, 512], BF16, "yT") for _ in range(2)]
            for tg in range(4):
                y_ = yT[tg % 2]
                for tt in range(4):
                    t = tg * 4 + tt
                    for g in range(2):
                        pb = bank()
                        for cc in range(4):
                            c = g * 4 + cc
                            mm(pb.ap[:, cc * 128:(cc + 1) * 128], ytm.ap[:, t, c * 128:(c + 1) * 128], ident_b, True, True,
                               [ytm, cstb], [pb])
                        evac(y_.ap[:, g * 4:(g + 1) * 4, tt * 128:(tt + 1) * 128],
                             pb.ap.rearrange("p (c n) -> p c n", c=4), [pb], [y_])
                for cb in range(8):
                    wb = loadw(w_dram[:, cb * 128:(cb + 1) * 128], 8)
                    pb = bank()
                    for c in range(8):
                        mm(pb.ap, wb.ap[:, c, :], y_.ap[:, c, :], c == 0, c == 7, [wb, y_], [pb])
                    fw.op("vector", lambda e: e.tensor_tensor(out=hT.ap[:, cb, tg * 512:(tg + 1) * 512],
                                                              in0=hT.ap[:, cb, tg * 512:(tg + 1) * 512], in1=pb.ap, op=ALU.add),
                          reads=[hT, pb], writes=[hT])
            fw.barrier()

    def layer1_mix():
        scope = ExitStack()
        sbl = scoped_sb(scope)
        rmsnorm_to(hnT, 2)
        V = lambda fn, r, w: fw.op("vector", fn, reads=r, writes=w)
        A = lambda fn, r, w: fw.op("scalar", fn, reads=r, writes=w)
        par = sbl([128, 64], F32, "par")
        acum = sbl([128, NT, 20], F32, "acum")
        bias = sbl([128, NT, 20], F32, "bias")
        sctmp = ExitStack()
        sbt = scoped_sb(sctmp)
        sm = sbt([128, NT, 24], F32, "sm")
        for (o, n, d_) in ((0, 16, cdtb_d), (16, 16, calog_d), (32, 16, cdskip_d), (48, 4, dib_d), (52, 4, dfb_d)):
            fw.dma("sync", par.ap[:, o:o + n], d_.partition_broadcast(128), writes=[par], sbuf=par)
        wdt = loadw(cdin_d[:, 2560:2576], 8, 16)
        wif = loadw(cdin_d[:, 5648:5656], 8, 8)
        pb = bank()
        for t in range(NT):
            for c in range(8):
                mm(pb.ap[:, t * 24:t * 24 + 16], hnT.ap[:, c, t * 128:(t + 1) * 128], wdt.ap[:, c, 0:16], c == 0, c == 7,
                   [wdt, hnT], [pb])
            for c in range(8):
                mm(pb.ap[:, t * 24 + 16:t * 24 + 24], hnT.ap[:, c, t * 128:(t + 1) * 128], wif.ap[:, c, 0:8], c == 0, c == 7,
                   [wif, hnT], [pb])
        evac(sm.ap, pb.ap[:, 0:NT * 24].rearrange("p (t n) -> p t n", t=NT), [pb], [sm])

        def bc_t(ap2d, n):
            return ap2d.unsqueeze(1).broadcast_to([128, NT, n])
        A(lambda e: e.activation(out=par.ap[:, 16:32], in_=par.ap[:, 16:32], func=AF.Exp), [par], [par])
        V(lambda e: e.tensor_scalar(out=par.ap[:, 16:32], in0=par.ap[:, 16:32], scalar1=-1.0, scalar2=None, op0=ALU.mult), [par], [par])
        V(lambda e: e.tensor_tensor(out=sm.ap[:, :, 0:16], in0=sm.ap[:, :, 0:16], in1=bc_t(par.ap[:, 0:16], 16), op=ALU.add), [sm, par], [sm])
        dtt = sbt([128, NT, 16], F32, "dtt")
        lndt = sbt([128, NT, 16], F32, "lndt")
        A(lambda e: e.activation(out=dtt.ap, in_=sm.ap[:, :, 0:16], func=AF.Exp), [sm], [dtt])
        A(lambda e: e.activation(out=dtt.ap, in_=dtt.ap, func=AF.Ln, bias=1.0), [dtt], [dtt])
        A(lambda e: e.activation(out=lndt.ap, in_=dtt.ap, func=AF.Ln), [dtt], [lndt])
        g20 = sbt([128, NT, 20], F32, "g20")
        V(lambda e: e.tensor_tensor(out=g20.ap[:, :, 0:16], in0=dtt.ap, in1=bc_t(par.ap[:, 16:32], 16), op=ALU.mult), [dtt, par], [g20])
        V(lambda e: e.tensor_tensor(out=sm.ap[:, :, 20:24], in0=sm.ap[:, :, 20:24], in1=bc_t(par.ap[:, 52:56], 4), op=ALU.add), [sm, par], [sm])
        A(lambda e: e.activation(out=g20.ap[:, :, 16:20], in_=sm.ap[:, :, 20:24], func=AF.Exp, scale=-1.0), [sm], [g20])
        A(lambda e: e.activation(out=g20.ap[:, :, 16:20], in_=g20.ap[:, :, 16:20], func=AF.Ln, bias=1.0), [g20], [g20])
        V(lambda e: e.tensor_scalar(out=g20.ap[:, :, 16:20], in0=g20.ap[:, :, 16:20], scalar1=-1.0, scalar2=None, op0=ALU.mult), [g20], [g20])
        V(lambda e: e.tensor_tensor(out=sm.ap[:, :, 16:20], in0=sm.ap[:, :, 16:20], in1=bc_t(par.ap[:, 48:52], 4), op=ALU.add), [sm, par], [sm])
        tot = sbt([128, NT, 20], F32, "tot")
        pb = bank()
        mm(pb.ap[:, 0:320], U_f, g20.ap.rearrange("p t n -> p (t n)"), True, True, [cst, g20], [pb])
        evac(acum.ap, pb.ap[:, 0:320].rearrange("p (t n) -> p t n", t=NT), [pb], [acum])
        pb = bank()
        mm(pb.ap[:, 0:320], ones_f, g20.ap.rearrange("p t n -> p (t n)"), True, True, [cst, g20], [pb])
        evac(tot.ap, pb.ap[:, 0:320].rearrange("p (t n) -> p t n", t=NT), [pb], [tot])
        for t in range(1, NT):
            V(lambda e: e.tensor_tensor(out=tot.ap[:, t, :], in0=tot.ap[:, t, :], in1=tot.ap[:, t - 1, :], op=ALU.add), [tot], [tot])
        V(lambda e: e.tensor_tensor(out=acum.ap[:, 1:NT, :], in0=acum.ap[:, 1:NT, :], in1=tot.ap[:, 0:NT - 1, :], op=ALU.add), [acum, tot], [acum])
        V(lambda e: e.tensor_tensor(out=bias.ap[:, :, 0:16], in0=lndt.ap, in1=acum.ap[:, :, 0:16], op=ALU.subtract), [lndt, acum], [bias])
        V(lambda e: e.tensor_tensor(out=bias.ap[:, :, 16:20], in0=sm.ap[:, :, 16:20], in1=acum.ap[:, :, 16:20], op=ALU.subtract), [sm, acum], [bias])

        fw.barrier()
        sctmp.close()
        xpad = sbl([128, 515], F32, "xpad")
        cv = sbl([128, 512], F32, "cv")
        cw = sbl([128, 8], F32, "cw")
        RbL = [sbl([128, 4, 128], F32, "Rb") for _ in range(2)]
        ArL = [sbl([128, 4, 128], F32, "Arow") for _ in range(2)]
        Eb = [sbl([128, 4, 128], BF16, "Eb") for _ in range(3)]
        Mb = [sbl([128, 4, 128], BF16, "Mb") for _ in range(3)]
        fT = sbl([128, T], BF16, "fT")
        tmp = [sbl([128, 256], F32, "tmp") for _ in range(3)]
        fsm = sbl([128, 8], F32, "fsm")
        ssum = sbl([128, NT, 4], F32, "ssum")

        def proj_fm_conv(col0, convw_d, convb_d, ch0, dst):
            wb = loadw(cdin_d[:, col0:col0 + 128], 8)
            fw.dma("sync", cw.ap[:, 0:4], convw_d[:, ch0:ch0 + 128].rearrange("k c -> c k"), writes=[cw], sbuf=cw,
                   allow_slow_non_contiguous=True)
            fw.dma("sync", cw.ap[:, 4:5], convb_d[:, ch0:ch0 + 128].rearrange("o c -> c o"), writes=[cw], sbuf=cw,
                   allow_slow_non_contiguous=True)
            V(lambda e: e.memset(xpad.ap[:, 0:3], 0.0), [], [xpad])
            for tg in range(4):
                pb = bank()
                for c in range(8):
                    mm(pb.ap, wb.ap[:, c, :], hnT.ap[:, c, tg * 512:(tg + 1) * 512], c == 0, c == 7, [wb, hnT], [pb])
                A(lambda e: e.activation(out=xpad.ap[:, 3:515], in_=pb.ap, func=AF.Copy), [pb], [xpad])
                V(lambda e: e.tensor_scalar(out=cv.ap, in0=xpad.ap[:, 0:512], scalar1=cw.ap[:, 0:1], scalar2=None, op0=ALU.mult),
                  [xpad, cw], [cv])
                for k_ in range(1, 4):
                    V(lambda e: e.scalar_tensor_tensor(out=cv.ap, in0=xpad.ap[:, k_:k_ + 512], scalar=cw.ap[:, k_:k_ + 1], in1=cv.ap,
                                                       op0=ALU.mult, op1=ALU.add), [xpad, cw, cv], [cv])
                A(lambda e: e.activation(out=dst.ap[:, tg * 512:(tg + 1) * 512], in_=cv.ap, func=AF.Silu, bias=cw.ap[:, 4:5]),
                  [cv, cw], [dst])
                V(lambda e: e.tensor_copy(out=xpad.ap[:, 0:3], in_=xpad.ap[:, 512:515]), [xpad], [xpad])

        def proj_tm(col0, dst, dcol0):
            wb = loadw(cdin_d[:, col0:col0 + 128], 8)
            for t4 in range(4):
                pb = bank()
                for tt in range(4):
                    t = t4 * 4 + tt
                    for c in range(8):
                        mm(pb.ap[:, tt * 128:(tt + 1) * 128], hnT.ap[:, c, t * 128:(t + 1) * 128], wb.ap[:, c, :], c == 0, c == 7,
                           [wb, hnT], [pb])
                evac(dst.ap[:, t4 * 4:(t4 + 1) * 4, dcol0:dcol0 + 128], pb.ap.rearrange("p (t d) -> p t d", t=4), [pb], [dst])

        def fm_to_tm(src, dst, dcol0):
            for t4 in range(4):
                pb = bank()
                for tt in range(4):
                    t = t4 * 4 + tt
                    mm(pb.ap[:, tt * 128:(tt + 1) * 128], src.ap[:, t * 128:(t + 1) * 128], ident_b, True, True, [src, cstb], [pb])
                evac(dst.ap[:, t4 * 4:(t4 + 1) * 4, dcol0:dcol0 + 128], pb.ap.rearrange("p (t d) -> p t d", t=4), [pb], [dst])

        def setup1(h0, nh, qb):
            Rb_, Ar_, pr = RbL[qb % 2], ArL[qb % 2], PB[6 + (qb % 2)]
            V(lambda e: e.tensor_tensor(out=Rb_.ap[:, 0:nh, :], in0=ident_f.unsqueeze(1).broadcast_to([128, nh, 128]),
                                        in1=acum.ap[:, qb, h0:h0 + nh].unsqueeze(2).broadcast_to([128, nh, 128]), op=ALU.mult),
              [cst, acum], [Rb_])
            mm(pr.ap[:, 0:nh * 128], ones_f, Rb_.ap[:, 0:nh, :].rearrange("p h l -> p (h l)"), True, True, [cst, Rb_], [pr])
            A(lambda e: e.activation(out=Ar_.ap[:, 0:nh, :], in_=pr.ap[:, 0:nh * 128].rearrange("p (h l) -> p h l", h=nh), func=AF.Copy),
              [pr], [Ar_])

        def setup2(h0, nh, qb):
            Rb_, Ar_ = RbL[qb % 2], ArL[qb % 2]
            V(lambda e: e.tensor_tensor(out=Rb_.ap[:, 0:nh, :], in0=Ar_.ap[:, 0:nh, :],
                                        in1=NEG_f.unsqueeze(1).broadcast_to([128, nh, 128]), op=ALU.add), [Ar_, cst], [Rb_])

        def decay_attn(h0, nh, gT_fn, g_reads, v_fn, v_buf, W, w, finalize):
            steps = [(qb, j) for qb in range(NT) for j in range(qb + 1)]
            nst = len(steps)
            LA = 2
            setup1(h0, nh, 0)
            setup2(h0, nh, 0)
            for k0 in range(min(LA, nst)):
                gT_fn(steps[k0][1], steps[k0][0], PB[2 + (k0 % 4)])
            for k, (qb, j) in enumerate(steps):
                acc = PB[qb % 2]
                if j == 0:
                    if qb + 1 < NT:
                        setup1(h0, nh, qb + 1)
                    V(lambda e: e.memset(acc.ap[:, 0:nh * W], 0.0), [], [acc])
                if k + LA < nst:
                    gT_fn(steps[k + LA][1], steps[k + LA][0], PB[2 + ((k + LA) % 4)])
                pg = PB[2 + (k % 4)]
                src = RbL[qb % 2] if j == qb else ArL[qb % 2]
                E_ = Eb[k % 3]
                M_ = Mb[k % 3]
                for hi in range(nh):
                    A(lambda e: e.activation(out=E_.ap[:, hi, :], in_=src.ap[:, hi, :], func=AF.Exp,
                                             bias=bias.ap[:, j, h0 + hi:h0 + hi + 1]), [src, bias], [E_])
                V(lambda e: e.tensor_tensor(out=M_.ap[:, 0:nh, :], in0=pg.ap[:, 0:128].unsqueeze(1).broadcast_to([128, nh, 128]),
                                            in1=E_.ap[:, 0:nh, :], op=ALU.mult), [pg, E_], [M_])
                for hi in range(nh):
                    mm(acc.ap[:, hi * W:hi * W + w], M_.ap[:, hi, :], v_fn(hi, j), False, False, [M_, v_buf], [acc])
                if j == qb:
                    if qb + 1 < NT:
                        setup2(h0, nh, qb + 1)
                    finalize(qb, acc)

        with ExitStack() as scC:
            sbc = scoped_sb(scC)
            bmT = sbc([128, T], BF16, "bmT")
            cmT = sbc([128, T], BF16, "cmT")
            xs_tm = sbc([128, NT, 256], BF16, "xs_tm")
            for cu in range(4):
                g = cu // 2
                if cu % 2 == 0:
                    proj_fm_conv(1024 + 1024 + g * 128, cconvw_d, cconvb_d, 1024 + g * 128, bmT)
                    proj_fm_conv(1024 + 1280 + g * 128, cconvw_d, cconvb_d, 1280 + g * 128, cmT)
                for blk in range(2):
                    ch0 = cu * 256 + blk * 128
                    proj_fm_conv(1024 + ch0, cconvw_d, cconvb_d, ch0, fT)
                    fm_to_tm(fT, xs_tm, blk * 128)
                    proj_tm(ch0, ytm, cu * 256 + blk * 128)

                def gT_c(j, qb, pg):
                    mm(pg.ap[:, 0:128], bmT.ap[:, j * 128:(j + 1) * 128], cmT.ap[:, qb * 128:(qb + 1) * 128], True, True,
                       [bmT, cmT], [pg])

                def fin_c(qb, acc, cu=cu):
                    y_, sz, _ = tmp
                    for hi in range(4):
                        V(lambda e: e.scalar_tensor_tensor(out=y_.ap[:, hi * 64:(hi + 1) * 64], in0=xs_tm.ap[:, qb, hi * 64:(hi + 1) * 64],
                                                           scalar=par.ap[:, 32 + cu * 4 + hi:32 + cu * 4 + hi + 1],
                                                           in1=acc.ap[:, hi * 64:(hi + 1) * 64], op0=ALU.mult, op1=ALU.add),
                          [xs_tm, par, acc], [y_])
                    A(lambda e: e.activation(out=sz.ap, in_=ytm.ap[:, qb, cu * 256:(cu + 1) * 256], func=AF.Silu), [ytm], [sz])
                    V(lambda e: e.tensor_tensor(out=ytm.ap[:, qb, cu * 256:(cu + 1) * 256], in0=y_.ap, in1=sz.ap, op=ALU.mult),
                      [y_, sz], [ytm])
                    A(lambda e: e.activation(out=sz.ap, in_=ytm.ap[:, qb, cu * 256:(cu + 1) * 256], func=AF.Square,
                                             accum_out=ssum.ap[:, qb, cu:cu + 1]), [ytm], [sz, ssum])

                decay_attn(cu * 4, 4, gT_c, [bmT, cmT], lambda hi, j: xs_tm.ap[:, j, hi * 64:(hi + 1) * 64], xs_tm, 64, 64, fin_c)
            fw.barrier()
        with ExitStack() as scN:
            sbn = scoped_sb(scN)
            cnw = sbn([128, D], F32, "cnw")
            rs = sbn([128, NT], F32, "rs")
            fw.dma("sync", cnw.ap, cnormw_d.partition_broadcast(128), writes=[cnw], sbuf=cnw)
            V(lambda e: e.reduce_sum(out=rs.ap, in_=ssum.ap, axis=mybir.AxisListType.X), [ssum], [rs])
            A(lambda e: e.activation(out=rs.ap, in_=rs.ap, func=AF.Ln, scale=1.0 / D, bias=EPS), [rs], [rs])
            A(lambda e: e.activation(out=rs.ap, in_=rs.ap, func=AF.Exp, scale=-0.5), [rs], [rs])
            for qb in range(NT):
                V(lambda e: e.scalar_tensor_tensor(out=ytm.ap[:, qb, :], in0=ytm.ap[:, qb, :], scalar=rs.ap[:, qb:qb + 1], in1=cnw.ap,
                                                   op0=ALU.mult, op1=ALU.mult), [ytm, rs, cnw], [ytm])
            fw.barrier()
        out_proj_tiled(cdout_d[0:D, :])

        with ExitStack() as scD:
            sbd = scoped_sb(scD)
            qT2 = sbd([128, 2, T], BF16, "qT2")
            kT2 = sbd([128, 2, T], BF16, "kT2")
            v_tm = sbd([128, NT, 258], BF16, "v_tm")
            dnw = sbd([128, 256], F32, "dnw")
            V(lambda e: e.memset(v_tm.ap[:, :, 256:258], 1.0), [], [v_tm])
            for h in range(4):
                fw.dma("sync", dnw.ap, dnormw_d[:, h * 256:(h + 1) * 256].partition_broadcast(128), writes=[dnw], sbuf=dnw)
                for blk in range(2):
                    ch0 = h * 256 + blk * 128
                    proj_fm_conv(2576 + ch0, dconvw_d, dconvb_d, ch0, fT)
                    wq_ = loadw(dwq_d[ch0 // 128], 1)
                    wk_ = loadw(dwk_d[ch0 // 128], 1)
                    for tg in range(4):
                        pb = bank()
                        mm(pb.ap, wq_.ap[:, 0, :], fT.ap[:, tg * 512:(tg + 1) * 512], True, True, [wq_, fT], [pb])
                        evac(qT2.ap[:, blk, tg * 512:(tg + 1) * 512], pb.ap, [pb], [qT2])
                        pb = bank()
                        mm(pb.ap, wk_.ap[:, 0, :], fT.ap[:, tg * 512:(tg + 1) * 512], True, True, [wk_, fT], [pb])
                        A(lambda e: e.activation(out=kT2.ap[:, blk, tg * 512:(tg + 1) * 512], in_=pb.ap, func=AF.Copy, scale=0.0625),
                          [pb], [kT2])
                    proj_tm(3600 + ch0, v_tm, blk * 128)
                    proj_tm(4624 + ch0, ytm, h * 256 + blk * 128)

                def gT_d(j, qb, pg):
                    for blk in range(2):
                        mm(pg.ap[:, 0:128], kT2.ap[:, blk, j * 128:(j + 1) * 128], qT2.ap[:, blk, qb * 128:(qb + 1) * 128],
                           blk == 0, blk == 1, [kT2, qT2], [pg])

                def fin_d(qb, acc, h=h):
                    hd, sg, t1 = tmp
                    V(lambda e: e.tensor_scalar(out=fsm.ap[:, 5:6], in0=acc.ap[:, 256:257], scalar1=-1.0, scalar2=None, op0=ALU.mult),
                      [acc], [fsm])
                    V(lambda e: e.tensor_tensor(out=fsm.ap[:, 0:1], in0=acc.ap[:, 256:257], in1=fsm.ap[:, 5:6], op=ALU.max), [acc, fsm], [fsm])
                    V(lambda e: e.tensor_scalar(out=fsm.ap[:, 0:1], in0=fsm.ap[:, 0:1], scalar1=1.0, scalar2=None, op0=ALU.max), [fsm], [fsm])
                    V(lambda e: e.reciprocal(out=fsm.ap[:, 1:2], in_=fsm.ap[:, 0:1]), [fsm], [fsm])
                    V(lambda e: e.tensor_scalar(out=hd.ap, in0=acc.ap[:, 0:256], scalar1=fsm.ap[:, 1:2], scalar2=None, op0=ALU.mult),
                      [acc, fsm], [hd])
                    A(lambda e: e.activation(out=t1.ap, in_=hd.ap, func=AF.Square, accum_out=fsm.ap[:, 2:3]), [hd], [t1, fsm])
                    A(lambda e: e.activation(out=fsm.ap[:, 3:4], in_=fsm.ap[:, 2:3], func=AF.Ln, scale=1.0 / 256, bias=EPS), [fsm], [fsm])
                    A(lambda e: e.activation(out=fsm.ap[:, 4:5], in_=fsm.ap[:, 3:4], func=AF.Exp, scale=-0.5), [fsm], [fsm])
                    A(lambda e: e.activation(out=sg.ap, in_=ytm.ap[:, qb, h * 256:(h + 1) * 256], func=AF.Sigmoid), [ytm], [sg])
                    V(lambda e: e.scalar_tensor_tensor(out=t1.ap, in0=hd.ap, scalar=fsm.ap[:, 4:5], in1=dnw.ap, op0=ALU.mult, op1=ALU.mult),
                      [hd, fsm, dnw], [t1])
                    V(lambda e: e.tensor_tensor(out=ytm.ap[:, qb, h * 256:(h + 1) * 256], in0=t1.ap, in1=sg.ap, op=ALU.mult),
                      [t1, sg], [ytm])

                decay_attn(16 + h, 1, gT_d, [kT2, qT2], lambda hi, j: v_tm.ap[:, j, 0:257], v_tm, 257, 257, fin_d)
            fw.barrier()
        out_proj_tiled(cdout_d[D:2 * D, :])
        fw.barrier()
        scope.close()

    for s in range(nseq):
        load_x(s)
        if do_l0:
            layer0_mix()
            if do_ffn:
                ffn(0)
        if do_l1:
            layer1_mix()
            if do_ffn:
                ffn(1)
        final_store(s)
    fw.barrier()
    print("program: ops=%d waits=%d" % (fw.nops, fw.nwaits))
    return nc


def block_diag(w):
    w = np.asarray(w, np.float32)
    o = np.zeros((8, 128, 128), np.float32)
    for n in range(256):
        b, r = divmod(n, 32)
        o[b, 4 * r:4 * r + 4, 4 * r:4 * r + 4] = w[n]
    return o


NSEQ = 4
_prog_cache = {}


def make_in_maps(inputs, nseq, ncores):
    f = lambda a: np.ascontiguousarray(np.asarray(a, np.float32))
    cf, cb = make_consts()
    shared = {
        "consts": cf, "constsb": cb,
        "mix_norm_w": f(inputs["mix_norm_w"]), "ffn_norm_w": f(inputs["ffn_norm_w"]),
        "final_norm_w": f(inputs["final_norm_w"]).reshape(1, D),
        "ab_w_in": f(inputs["ab_w_in"][0]), "ab_w_out": f(inputs["ab_w_out"][0]),
        "diff_lq1": f(inputs["diff_lq1"]), "diff_lk1": f(inputs["diff_lk1"]),
        "diff_lq2": f(inputs["diff_lq2"]), "diff_lk2": f(inputs["diff_lk2"]),
        "diff_subln_w": f(inputs["diff_subln_w"]),
        "cd_w_in": f(inputs["cd_w_in"][0]),
        "c_conv_w": f(inputs["c_conv_w"][0]), "c_conv_b": f(inputs["c_conv_b"]),
        "c_dt_bias": f(inputs["c_dt_bias"]), "c_a_log": f(inputs["c_a_log"]), "c_d_skip": f(inputs["c_d_skip"]),
        "c_norm_w": f(inputs["c_norm_w"]),
        "d_conv_w": f(inputs["d_conv_w"][0]), "d_conv_b": f(inputs["d_conv_b"]),
        "d_wq_bd": block_diag(inputs["d_wq"][0]), "d_wk_bd": block_diag(inputs["d_wk"][0]),
        "d_i_bias": f(inputs["d_i_bias"]), "d_f_bias": f(inputs["d_f_bias"]),
        "d_norm_w": f(inputs["d_norm_w"]),
        "cd_w_out": f(inputs["cd_w_out"][0]),
        "ffn_w_gate_up": f(inputs["ffn_w_gate_up"]), "ffn_w_down": f(inputs["ffn_w_down"]),
    }
    x = f(inputs["x"])
    maps = []
    for c in range(ncores):
        m = dict(shared)
        m["x"] = np.ascontiguousarray(x[c * nseq:(c + 1) * nseq])
        maps.append(m)
    return maps


def kernel(**inputs):
    ncores = 8
    nseq = NSEQ
    nc = build_program(nseq)
    in_maps = make_in_maps(inputs, nseq, ncores)
    res = run_bass_kernel_spmd(nc, in_maps, core_ids=list(range(ncores)))
    return np.concatenate([np.asarray(r["out"], np.float32) for r in res.results], axis=0)
```

```python
import math
from contextlib import ExitStack
import numpy as np
import concourse.bass as bass
import concourse.mybir as mybir
from concourse.bass_utils import run_bass_kernel_spmd

F32 = mybir.dt.float32
BF16 = mybir.dt.bfloat16
AF = mybir.ActivationFunctionType
ALU = mybir.AluOpType

T = 2048
D = 1024
NT = 16
FFN_H = 2816
EPS = 1e-6
AB_COLS = 3072
CD_COLS = 5656
NEGM = -30000.0


class _Dep:
    def __init__(self):
        self.w = {}
        self.r = {}
        self.dsem = None


class Buf:
    def __init__(self, ap, name="", owner=None):
        self.ap = ap
        self.name = name
        self.d = owner.d if owner is not None else _Dep()

    w = property(lambda self: self.d.w, lambda self, v: setattr(self.d, "w", v))
    r = property(lambda self: self.d.r, lambda self, v: setattr(self.d, "r", v))
    dsem = property(lambda self: self.d.dsem, lambda self, v: setattr(self.d, "dsem", v))


class Fw:
    ENG = ["tensor", "vector", "scalar", "gpsimd", "sync"]

    def __init__(self, nc):
        self.nc = nc
        self.sem = {}
        self.cnt = {}
        self.waited = {}
        self.semobj = {}
        for e in self.ENG:
            s = nc.alloc_semaphore(name="s_" + e)
            self.sem[e] = s
            self.cnt[e] = 0
            self.waited[e] = {}
        self.dma_total = {}
        self.nwaits = 0
        self.nops = 0

    def eng(self, e):
        return getattr(self.nc, e)

    def _wait(self, e, deps):
        w = self.waited[e]
        for k, v in deps.items():
            if k[0] == 'e':
                if k[1] == e and e == "tensor":
                    continue
                sem = self.sem[k[1]]
            else:
                sem = self.semobj[k[1]]
                v = max(v, self.dma_total[k[1]])
            if w.get(k, 0) >= v:
                continue
            self.eng(e).wait_ge(sem, v)
            self.nwaits += 1
            w[k] = v

    @staticmethod
    def _merge(d, k, v):
        if d.get(k, 0) < v:
            d[k] = v

    def _deps(self, reads, writes):
        deps = {}
        for b in reads:
            for k, v in b.w.items():
                self._merge(deps, k, v)
        for b in writes:
            for k, v in b.w.items():
                self._merge(deps, k, v)
            for k, v in b.r.items():
                self._merge(deps, k, v)
        return deps

    def _record(self, key, val, reads, writes):
        for b in reads:
            self._merge(b.r, key, val)
        for b in writes:
            self._merge(b.w, key, val)
            b.r = {}

    def op(self, e, fn, reads=(), writes=()):
        self._wait(e, self._deps(reads, writes))
        ins = fn(self.eng(e))
        self.cnt[e] += 1
        ins.then_inc(self.sem[e], 1)
        self.nops += 1
        self._record(('e', e), self.cnt[e], reads, writes)
        return ins

    def dma(self, q, out_ap, in_ap, reads=(), writes=(), sbuf=None, **kw):
        self._wait(q, self._deps(reads, writes))
        if sbuf.dsem is None:
            sbuf.dsem = self.nc.alloc_semaphore(name="d_" + sbuf.name)
            self.semobj[id(sbuf.dsem)] = sbuf.dsem
            self.dma_total[id(sbuf.dsem)] = 0
        ins = self.eng(q).dma_start(out=out_ap, in_=in_ap, **kw)
        ins.then_inc(sbuf.dsem, 16)
        self.dma_total[id(sbuf.dsem)] += 16
        self._record(('d', id(sbuf.dsem)), self.dma_total[id(sbuf.dsem)], reads, writes)
        return ins

    def barrier(self):
        deps = {('e', e): self.cnt[e] for e in self.ENG if self.cnt[e] > 0}
        for k, v in self.dma_total.items():
            if v > 0:
                deps[('d', k)] = v
        for e in self.ENG:
            self._wait(e, dict(deps))


C_ID = 0
C_U = 128
C_NEG = 256
C_ONES = 384
C_TOT = 512
B_ID = 0
B_MA = 128
B_MB = B_MA + 2304
B_TOT = B_MB + 384


def make_consts():
    c = np.zeros((128, C_TOT), np.float32)
    cb = np.zeros((128, B_TOT), np.float32)
    s = np.arange(128)[:, None]
    c[:, C_ID:C_ID + 128] = np.eye(128, dtype=np.float32)
    cb[:, B_ID:B_ID + 128] = np.eye(128, dtype=np.float32)
    cc = np.arange(2304)[None, :]
    d = cc - 128 - s
    mult = ((d >= 0) & (d <= 128)).astype(np.float32)
    mult += ((d >= 0) & (d % 4 == 0) & (d <= 512)).astype(np.float32)
    mult += ((d >= 0) & (d % 16 == 0) & (d <= 2048)).astype(np.float32)
    cb[:, B_MA:B_MA + 2304] = mult
    cc = np.arange(384)[None, :]
    cb[:, B_MB:B_MB + 384] = ((cc - 128 - s) >= 0).astype(np.float32)
    l = np.arange(128)[None, :]
    c[:, C_U:C_U + 128] = (s <= l).astype(np.float32)
    c[:, C_NEG:C_NEG + 128] = np.where(s > l, NEGM, 0.0).astype(np.float32)
    c[:, C_ONES:C_ONES + 128] = 1.0
    return c, cb


def build_program(nseq, do_l0=True, do_l1=True, do_ffn=True, units=tuple(range(8)), ngroups=8, stage=9):
    nc = bass.Bass("TRN2", target_bir_lowering=False)
    fw = Fw(nc)

    def din(name, shape):
        return nc.dram_tensor(name, list(shape), F32, kind="ExternalInput").ap()

    x_d = din("x", [nseq, T, D])
    consts_d = din("consts", [128, C_TOT])
    constsb_d = din("constsb", [128, B_TOT])
    mixg_d = din("mix_norm_w", [2, D])
    ffng_d = din("ffn_norm_w", [2, D])
    fing_d = din("final_norm_w", [1, D])
    abin_d = din("ab_w_in", [D, AB_COLS])
    about_d = din("ab_w_out", [D, D])
    lq1_d = din("diff_lq1", [1, 64]); lk1_d = din("diff_lk1", [1, 64])
    lq2_d = din("diff_lq2", [1, 64]); lk2_d = din("diff_lk2", [1, 64])
    subln_d = din("diff_subln_w", [1, 128])
    cdin_d = din("cd_w_in", [D, CD_COLS])
    cconvw_d = din("c_conv_w", [4, 1536]); cconvb_d = din("c_conv_b", [1, 1536])
    cdtb_d = din("c_dt_bias", [1, 16]); calog_d = din("c_a_log", [1, 16]); cdskip_d = din("c_d_skip", [1, 16])
    cnormw_d = din("c_norm_w", [1, D])
    dconvw_d = din("d_conv_w", [4, D]); dconvb_d = din("d_conv_b", [1, D])
    dwq_d = din("d_wq_bd", [8, 128, 128]); dwk_d = din("d_wk_bd", [8, 128, 128])
    dib_d = din("d_i_bias", [1, 4]); dfb_d = din("d_f_bias", [1, 4])
    dnormw_d = din("d_norm_w", [1, D])
    cdout_d = din("cd_w_out", [2 * D, D])
    wgu_d = din("ffn_w_gate_up", [2, D, 2 * FFN_H])
    wdn_d = din("ffn_w_down", [2, FFN_H, D])
    out_d = nc.dram_tensor("out", [nseq, T, D], F32, kind="ExternalOutput").ap()

    cnt = [0]

    def sb(shape, dt=F32, name=None):
        cnt[0] += 1
        nm = (name or "t") + "_%d" % cnt[0]
        return Buf(nc.alloc_sbuf_tensor(nm, list(shape), dt).ap(), nm)

    def scoped_sb(scope):
        def f(shape, dt=F32, name=None):
            cnt[0] += 1
            nm = (name or "t") + "_%d" % cnt[0]
            return Buf(scope.enter_context(nc.sbuf_tensor(nm, list(shape), dt)).ap(), nm)
        return f

    hT = sb([128, 8, T], F32, "hT")
    cst = sb([128, C_TOT], F32, "cst")
    cstb = sb([128, B_TOT], BF16, "cstb")
    gam = sb([128, 5, 8], F32, "gam")
    PB = []
    for i in range(8):
        PB.append(Buf(nc.alloc_psum_tensor("pb%d" % i, [128, 512], F32).ap(), "pb%d" % i))
    pb_rr = [0]

    def bank():
        b = PB[pb_rr[0] % 8]
        pb_rr[0] += 1
        return b

    fw.dma("sync", cst.ap, consts_d, writes=[cst], sbuf=cst)
    fw.dma("gpsimd", cstb.ap, constsb_d, writes=[cstb], sbuf=cstb)
    for i, (g_d, row) in enumerate([(mixg_d, 0), (ffng_d, 0), (mixg_d, 1), (ffng_d, 1), (fing_d, 0)]):
        fw.dma("sync", gam.ap[:, i, :], g_d[row, :].rearrange("(c p) -> p c", p=128), writes=[gam], sbuf=gam,
               allow_slow_non_contiguous=True)
    ident_f = cst.ap[:, C_ID:C_ID + 128]
    ident_b = cstb.ap[:, B_ID:B_ID + 128]
    ones_f = cst.ap[:, C_ONES:C_ONES + 128]

    NW = 4
    wbf = [sb([128, 8, 128], BF16, "wbf") for _ in range(NW)]
    w_rr = [0]

    def loadw(dram_rows_cols, nchunk, ncols=128):
        i = w_rr[0] % NW
        w_rr[0] += 1
        bf = wbf[i]
        fw.dma("gpsimd", bf.ap[:, 0:nchunk, 0:ncols], dram_rows_cols.rearrange("(c p) n -> p c n", p=128),
               writes=[bf], sbuf=bf)
        return bf

    evac_rr = [0]

    def evac(out_ap, in_ap, reads, writes):
        evac_rr[0] += 1
        if evac_rr[0] % 2 == 0:
            fw.op("scalar", lambda e: e.activation(out=out_ap, in_=in_ap, func=AF.Copy), reads=reads, writes=writes)
        else:
            fw.op("vector", lambda e: e.tensor_copy(out=out_ap, in_=in_ap), reads=reads, writes=writes)

    def mm(out_ap, lhsT, rhs, start, stop, reads, writes):
        fw.op("tensor", lambda e: e.matmul(out_ap, lhsT=lhsT, rhs=rhs, start=start, stop=stop, skip_group_check=True),
              reads=reads, writes=writes)

    sq = [sb([128, 512], F32, "sq") for _ in range(2)]
    rstd = sb([128, 512], F32, "rstd")

    def rms_stats(tg):
        pb = bank()
        for c in range(8):
            s = sq[c % 2]
            fw.op("scalar", lambda e: e.activation(out=s.ap, in_=hT.ap[:, c, tg * 512:(tg + 1) * 512], func=AF.Square),
                  reads=[hT], writes=[s])
            mm(pb.ap, ones_f, s.ap, c == 0, c == 7, [s, cst], [pb])
        fw.op("scalar", lambda e: e.activation(out=rstd.ap, in_=pb.ap, func=AF.Ln, scale=1.0 / D, bias=EPS),
              reads=[pb], writes=[rstd])
        fw.op("scalar", lambda e: e.activation(out=rstd.ap, in_=rstd.ap, func=AF.Exp, scale=-0.5),
              reads=[rstd], writes=[rstd])

    def rmsnorm_to(dst, gi, dst_dtype_is_f32=False):
        for tg in range(4):
            rms_stats(tg)
            for c in range(8):
                fw.op("vector", lambda e: e.scalar_tensor_tensor(
                    out=dst.ap[:, c, tg * 512:(tg + 1) * 512], in0=hT.ap[:, c, tg * 512:(tg + 1) * 512],
                    scalar=gam.ap[:, gi, c:c + 1], in1=rstd.ap, op0=ALU.mult, op1=ALU.mult),
                    reads=[hT, gam, rstd], writes=[dst])

    hnT = sb([128, 8, T], BF16, "hnT")
    ytm = sb([128, NT, D], BF16, "ytm")
    actT = Buf(ytm.ap.rearrange("p t d -> p (t d)").rearrange("p (h t) -> p h t", h=8), "actT", owner=ytm)

    def load_x(s):
        sc = ExitStack()
        sbx = scoped_sb(sc)
        xin = [sbx([128, D], F32, "xin") for _ in range(2)]
        for t in range(NT):
            xt = xin[t % 2]
            fw.dma("sync", xt.ap, x_d[s, t * 128:(t + 1) * 128, :], writes=[xt], sbuf=xt)
            for g in range(2):
                pb = bank()
                for cc in range(4):
                    c = g * 4 + cc
                    mm(pb.ap[:, cc * 128:(cc + 1) * 128], xt.ap[:, c * 128:(c + 1) * 128], ident_f, True, True,
                       [xt, cst], [pb])
                evac(hT.ap[:, g * 4:(g + 1) * 4, t * 128:(t + 1) * 128],
                     pb.ap.rearrange("p (c n) -> p c n", c=4), [pb], [hT])
        fw.barrier()
        sc.close()

    def transpose_ytm_to_hnT():
        for t in range(NT):
            for g in range(2):
                pb = bank()
                for cc in range(4):
                    c = g * 4 + cc
                    mm(pb.ap[:, cc * 128:(cc + 1) * 128], ytm.ap[:, t, c * 128:(c + 1) * 128], ident_b, True, True,
                       [ytm, cstb], [pb])
                evac(hnT.ap[:, g * 4:(g + 1) * 4, t * 128:(t + 1) * 128],
                     pb.ap.rearrange("p (c n) -> p c n", c=4), [pb], [hnT])

    def out_proj(w_dram):
        for cb in range(8):
            wb = loadw(w_dram[:, cb * 128:(cb + 1) * 128], 8)
            for tg in range(4):
                pb = bank()
                for c in range(8):
                    mm(pb.ap, wb.ap[:, c, :], hnT.ap[:, c, tg * 512:(tg + 1) * 512], c == 0, c == 7, [wb, hnT], [pb])
                fw.op("vector", lambda e: e.tensor_tensor(out=hT.ap[:, cb, tg * 512:(tg + 1) * 512],
                                                          in0=hT.ap[:, cb, tg * 512:(tg + 1) * 512], in1=pb.ap, op=ALU.add),
                      reads=[hT, pb], writes=[hT])

    def layer0_mix():
        scope = ExitStack()
        sb = scoped_sb(scope)
        qT = sb([128, T], BF16, "qT")
        kT = sb([128, 2, T], BF16, "kT")
        vA = sb([128, NT, 2, 65], BF16, "vA")
        vB = sb([128, NT, 129], BF16, "vB")
        PT = [sb([128, 2, 256], BF16, "PT") for _ in range(4)]
        lamw = sb([128, 8], F32, "lamw")
        lqk = sb([128, 4, 64], F32, "lqk")
        sublnw = sb([128, 128], F32, "sublnw")
        fin_s = [sb([128, 8], F32, "fin_s") for _ in range(2)]
        fin_t = [sb([128, 128], F32, "fin_t") for _ in range(2)]
        fin_o = [sb([128, 128], F32, "fin_o") for _ in range(2)]
        fin_j = sb([128, 128], F32, "fin_j")

        fw.op("vector", lambda e: e.memset(kT.ap, 0.0), writes=[kT])
        fw.op("vector", lambda e: e.memset(vA.ap, 1.0), writes=[vA])
        fw.op("vector", lambda e: e.memset(vB.ap, 1.0), writes=[vB])
        for i, d_ in enumerate([lq1_d, lk1_d, lq2_d, lk2_d]):
            fw.dma("sync", lqk.ap[:, i, :], d_.partition_broadcast(128), writes=[lqk], sbuf=lqk)
        fw.dma("sync", sublnw.ap, subln_d.partition_broadcast(128), writes=[sublnw], sbuf=sublnw)
        lambda_init = 0.8 - 0.6 * math.exp(-0.3 * 0)
        fw.op("vector", lambda e: e.tensor_scalar(out=sublnw.ap, in0=sublnw.ap, scalar1=1.0 - lambda_init, scalar2=None,
                                                  op0=ALU.mult), reads=[sublnw], writes=[sublnw])
        fw.op("vector", lambda e: e.tensor_tensor(out=lqk.ap[:, 0, :], in0=lqk.ap[:, 0, :], in1=lqk.ap[:, 1, :], op=ALU.mult),
              reads=[lqk], writes=[lqk])
        fw.op("vector", lambda e: e.tensor_tensor(out=lqk.ap[:, 2, :], in0=lqk.ap[:, 2, :], in1=lqk.ap[:, 3, :], op=ALU.mult),
              reads=[lqk], writes=[lqk])
        fw.op("vector", lambda e: e.reduce_sum(out=lamw.ap[:, 1:2], in_=lqk.ap[:, 0, :], axis=mybir.AxisListType.X),
              reads=[lqk], writes=[lamw])
        fw.op("vector", lambda e: e.reduce_sum(out=lamw.ap[:, 2:3], in_=lqk.ap[:, 2, :], axis=mybir.AxisListType.X),
              reads=[lqk], writes=[lamw])
        fw.op("scalar", lambda e: e.activation(out=lamw.ap[:, 3:5], in_=lamw.ap[:, 1:3], func=AF.Exp), reads=[lamw], writes=[lamw])
        fw.op("vector", lambda e: e.tensor_tensor(out=lamw.ap[:, 0:1], in0=lamw.ap[:, 4:5], in1=lamw.ap[:, 3:4], op=ALU.subtract),
              reads=[lamw], writes=[lamw])
        fw.op("vector", lambda e: e.tensor_scalar(out=lamw.ap[:, 0:1], in0=lamw.ap[:, 0:1], scalar1=-lambda_init, scalar2=None,
                                                  op0=ALU.add), reads=[lamw], writes=[lamw])

        def attn_unit(u):
            isA = u < 4
            qoff = (0 if isA else 1536) + (u % 4) * 128
            koff = qoff + 512
            voff = qoff + 1024
            wq = loadw(abin_d[:, qoff:qoff + 128], 8)
            wk = loadw(abin_d[:, koff:koff + 128], 8)
            wv = loadw(abin_d[:, voff:voff + 128], 8)
            for (wb, dst) in ((wq, qT), (wk, kT)):
                for tg in range(4):
                    pb = bank()
                    for c in range(8):
                        mm(pb.ap, wb.ap[:, c, :], hnT.ap[:, c, tg * 512:(tg + 1) * 512], c == 0, c == 7, [wb, hnT], [pb])
                    if dst is qT:
                        evac(dst.ap[:, tg * 512:(tg + 1) * 512], pb.ap, [pb], [dst])
                    else:
                        evac(dst.ap[0:64, 0, tg * 512:(tg + 1) * 512], pb.ap[0:64, :], [pb], [dst])
                        evac(dst.ap[64:128, 1, tg * 512:(tg + 1) * 512], pb.ap[64:128, :], [pb], [dst])
            for t4 in range(4):
                pb = bank()
                for tt in range(4):
                    t = t4 * 4 + tt
                    for c in range(8):
                        mm(pb.ap[:, tt * 128:(tt + 1) * 128], hnT.ap[:, c, t * 128:(t + 1) * 128], wv.ap[:, c, :],
                           c == 0, c == 7, [wv, hnT], [pb])
                if isA:
                    evac(vA.ap[:, t4 * 4:(t4 + 1) * 4, :, 0:64], pb.ap.rearrange("p (t i d) -> p t i d", t=4, i=2), [pb], [vA])
                else:
                    evac(vB.ap[:, t4 * 4:(t4 + 1) * 4, 0:128], pb.ap.rearrange("p (t d) -> p t d", t=4), [pb], [vB])
            W = 65 if isA else 129
            if stage < 1:
                return
            steps = [(G, j) for G in range(ngroups) for j in range(2 * G + 2)]
            nst = len(steps)
            vbuf = vA if isA else vB

            def acc_of(G, i, b):
                return PB[2 * (G % 2) + i], b * W

            def emit_S(k):
                G, j = steps[k]
                sbk = PB[4 + (k % 4)]
                for i in range(2):
                    mm(sbk.ap[:, i * 256:(i + 1) * 256], kT.ap[:, i, j * 128:(j + 1) * 128],
                       qT.ap[:, G * 256:(G + 1) * 256], True, True, [kT, qT], [sbk])

            def finalize(G):
                for b in range(2):
                    qb = 2 * G + b
                    fs = fin_s[b]
                    if isA:
                        for i in range(2):
                            a, o = acc_of(G, i, b)
                            fw.op("vector", lambda e: e.reciprocal(out=fs.ap[:, i:i + 1], in_=a.ap[:, o + 64:o + 65]), reads=[a], writes=[fs])
                            fw.op("vector", lambda e: e.tensor_scalar(
                                out=ytm.ap[:, qb, u * 128 + 64 * i:u * 128 + 64 * i + 64], in0=a.ap[:, o:o + 64],
                                scalar1=fs.ap[:, i:i + 1], scalar2=None, op0=ALU.mult), reads=[a, fs], writes=[ytm])
                    else:
                        a0, o0 = acc_of(G, 0, b)
                        a1, o1 = acc_of(G, 1, b)
                        ft, fo = fin_t[b], fin_o[b]
                        fw.op("vector", lambda e: e.reciprocal(out=fs.ap[:, 0:1], in_=a0.ap[:, o0 + 128:o0 + 129]), reads=[a0], writes=[fs])
                        fw.op("vector", lambda e: e.reciprocal(out=fs.ap[:, 1:2], in_=a1.ap[:, o1 + 128:o1 + 129]), reads=[a1], writes=[fs])
                        fw.op("vector", lambda e: e.tensor_tensor(out=fs.ap[:, 2:3], in0=fs.ap[:, 1:2], in1=lamw.ap[:, 0:1], op=ALU.mult),
                              reads=[fs, lamw], writes=[fs])
                        fw.op("vector", lambda e: e.tensor_scalar(out=ft.ap, in0=a0.ap[:, o0:o0 + 128], scalar1=fs.ap[:, 0:1], scalar2=None,
                                                                  op0=ALU.mult), reads=[a0, fs], writes=[ft])
                        fw.op("vector", lambda e: e.scalar_tensor_tensor(out=fo.ap, in0=a1.ap[:, o1:o1 + 128], scalar=fs.ap[:, 2:3],
                                                                         in1=ft.ap, op0=ALU.mult, op1=ALU.add),
                              reads=[a1, fs, ft], writes=[fo])
                        fw.op("scalar", lambda e: e.activation(out=fin_j.ap, in_=fo.ap, func=AF.Square, accum_out=fs.ap[:, 3:4]),
                              reads=[fo], writes=[fin_j, fs])
                        fw.op("scalar", lambda e: e.activation(out=fs.ap[:, 4:5], in_=fs.ap[:, 3:4], func=AF.Ln, scale=1.0 / 128, bias=EPS),
                              reads=[fs], writes=[fs])
                        fw.op("scalar", lambda e: e.activation(out=fs.ap[:, 5:6], in_=fs.ap[:, 4:5], func=AF.Exp, scale=-0.5),
                              reads=[fs], writes=[fs])
                        fw.op("vector", lambda e: e.scalar_tensor_tensor(
                            out=ytm.ap[:, qb, u * 128:(u + 1) * 128], in0=fo.ap, scalar=fs.ap[:, 5:6], in1=sublnw.ap,
                            op0=ALU.mult, op1=ALU.mult), reads=[fo, fs, sublnw], writes=[ytm])

            LA = 2
            for k0 in range(min(LA, nst)):
                emit_S(k0)
            pending = None
            for k, (G, j) in enumerate(steps):
                if j == 0:
                    for i in range(2):
                        a, _ = acc_of(G, i, 0)
                        fw.op("vector", lambda e: e.memset(a.ap[:, 0:2 * W], 0.0), writes=[a])
                if k + LA < nst:
                    emit_S(k + LA)
                sbk = PB[4 + (k % 4)]
                pt = PT[k % 4]
                fw.op("scalar", lambda e: e.activation(out=pt.ap, in_=sbk.ap.rearrange("p (i n) -> p i n", i=2),
                                                       func=AF.Exp, scale=0.125), reads=[sbk], writes=[pt])
                d0 = 2 * G - j
                if isA:
                    m = cstb.ap[:, B_MA + 128 * (d0 + 1):B_MA + 128 * (d0 + 1) + 256]
                elif d0 <= 0:
                    m = cstb.ap[:, B_MB + 128 * (d0 + 1):B_MB + 128 * (d0 + 1) + 256]
                else:
                    m = None
                if m is not None:
                    mb_ = m.unsqueeze(1).broadcast_to([128, 2, 256])
                    fw.op("vector", lambda e: e.tensor_tensor(out=pt.ap, in0=pt.ap, in1=mb_, op=ALU.mult),
                          reads=[pt, cstb], writes=[pt])
                for b in range(2):
                    if 2 * G + b < j:
                        continue
                    for i in range(2):
                        rhs = vA.ap[:, j, i, :] if isA else vB.ap[:, j, :]
                        a, o = acc_of(G, i, b)
                        mm(a.ap[:, o:o + W], pt.ap[:, i, b * 128:(b + 1) * 128], rhs, False, False, [pt, vbuf], [a])
                if pending is not None and k >= pending[1]:
                    finalize(pending[0])
                    pending = None
                if j == 2 * G + 1:
                    if pending is not None:
                        finalize(pending[0])
                    pending = (G, k + 2)
            if pending is not None:
                finalize(pending[0])

        rmsnorm_to(hnT, 0)
        for u in units:
            attn_unit(u)
        transpose_ytm_to_hnT()
        out_proj(about_d)
        fw.barrier()
        scope.close()

    silu_t = sq

    def ffn(layer):
        rmsnorm_to(hnT, 1 + 2 * layer)
        for (b0, nb) in ((0, 8), (8, 7), (15, 7)):
            for hb in range(nb):
                col = (b0 + hb) * 128
                wg = loadw(wgu_d[layer, :, col:col + 128], 8)
                wu = loadw(wgu_d[layer, :, FFN_H + col:FFN_H + col + 128], 8)
                for tg in range(4):
                    pg = bank()
                    pu = bank()
                    for c in range(8):
                        mm(pg.ap, wg.ap[:, c, :], hnT.ap[:, c, tg * 512:(tg + 1) * 512], c == 0, c == 7, [wg, hnT], [pg])
                    for c in range(8):
                        mm(pu.ap, wu.ap[:, c, :], hnT.ap[:, c, tg * 512:(tg + 1) * 512], c == 0, c == 7, [wu, hnT], [pu])
                    st = silu_t[tg % 2]
                    fw.op("scalar", lambda e: e.activation(out=st.ap, in_=pg.ap, func=AF.Silu), reads=[pg], writes=[st])
                    fw.op("vector", lambda e: e.tensor_tensor(out=actT.ap[:, hb, tg * 512:(tg + 1) * 512], in0=st.ap, in1=pu.ap,
                                                              op=ALU.mult), reads=[st, pu], writes=[actT])
            for cb in range(8):
                wd = loadw(wdn_d[layer, b0 * 128:(b0 + nb) * 128, cb * 128:(cb + 1) * 128], nb)
                for tg in range(4):
                    pb = bank()
                    for hb in range(nb):
                        mm(pb.ap, wd.ap[:, hb, :], actT.ap[:, hb, tg * 512:(tg + 1) * 512], hb == 0, hb == nb - 1, [wd, actT], [pb])
                    fw.op("vector", lambda e: e.tensor_tensor(out=hT.ap[:, cb, tg * 512:(tg + 1) * 512],
                                                              in0=hT.ap[:, cb, tg * 512:(tg + 1) * 512], in1=pb.ap, op=ALU.add),
                          reads=[hT, pb], writes=[hT])

    def final_store(s):
        sc = ExitStack()
        sbx = scoped_sb(sc)
        onT = [sbx([128, 8, 128], F32, "onT") for _ in range(2)]
        ost = [sbx([128, D], F32, "ost") for _ in range(2)]
        for tg in range(4):
            rms_stats(tg)
            for tt in range(4):
                t = tg * 4 + tt
                o_n = onT[t % 2]
                fw.op("vector", lambda e: e.scalar_tensor_tensor(
                    out=o_n.ap, in0=hT.ap[:, :, t * 128:(t + 1) * 128], scalar=1.0,
                    in1=rstd.ap[:, tt * 128:(tt + 1) * 128].unsqueeze(1).broadcast_to([128, 8, 128]),
                    op0=ALU.mult, op1=ALU.mult), reads=[hT, rstd], writes=[o_n])
                fw.op("vector", lambda e: e.tensor_tensor(
                    out=o_n.ap, in0=o_n.ap, in1=gam.ap[:, 4, :].unsqueeze(2).broadcast_to([128, 8, 128]), op=ALU.mult),
                    reads=[o_n, gam], writes=[o_n])
                os_ = ost[t % 2]
                for g in range(2):
                    pb = bank()
                    for cc in range(4):
                        c = g * 4 + cc
                        mm(pb.ap[:, cc * 128:(cc + 1) * 128], o_n.ap[:, c, :], ident_f, True, True, [o_n, cst], [pb])
                    evac(os_.ap[:, g * 512:(g + 1) * 512], pb.ap, [pb], [os_])
                fw.dma("sync", out_d[s, t * 128:(t + 1) * 128, :], os_.ap, reads=[os_], sbuf=os_)
        fw.barrier()
        sc.close()

    U_f = cst.ap[:, C_U:C_U + 128]
    NEG_f = cst.ap[:, C_NEG:C_NEG + 128]

    def out_proj_tiled(w_dram):
        with ExitStack() as sc:
            sbl = scoped_sb(sc)
            yT = [sbl([128, 8, 512], BF16, "yT") for _ in range(2)]
            for tg in range(4):
                y_ = yT[tg % 2]
                for tt in range(4):
                    t = tg * 4 + tt
                    for g in range(2):
                        pb = bank()
                        for cc in range(4):
                            c = g * 4 + cc
                            mm(pb.ap[:, cc * 128:(cc + 1) * 128], ytm.ap[:, t, c * 128:(c + 1) * 128], ident_b, True, True,
                               [ytm, cstb], [pb])
                        evac(y_.ap[:, g * 4:(g + 1) * 4, tt * 128:(tt + 1) * 128],
                             pb.ap.rearrange("p (c n) -> p c n", c=4), [pb], [y_])
                for cb in range(8):
                    wb = loadw(w_dram[:, cb * 128:(cb + 1) * 128], 8)
                    pb = bank()
                    for c in range(8):
                        mm(pb.ap, wb.ap[:, c, :], y_.ap[:, c, :], c == 0, c == 7, [wb, y_], [pb])
                    fw.op("vector", lambda e: e.tensor_tensor(out=hT.ap[:, cb, tg * 512:(tg + 1) * 512],
                                                              in0=hT.ap[:, cb, tg * 512:(tg + 1) * 512], in1=pb.ap, op=ALU.add),
                          reads=[hT, pb], writes=[hT])
            fw.barrier()

    def layer1_mix():
        scope = ExitStack()
        sbl = scoped_sb(scope)
        rmsnorm_to(hnT, 2)
        V = lambda fn, r, w: fw.op("vector", fn, reads=r, writes=w)
        A = lambda fn, r, w: fw.op("scalar", fn, reads=r, writes=w)
        par = sbl([128, 64], F32, "par")
        acum = sbl([128, NT, 20], F32, "acum")
        bias = sbl([128, NT, 20], F32, "bias")
        U_ = sbl([128, NT, 20], F32, "U_")
        Wt = sbl([128, NT, 20], F32, "Wt")
        Dc = sbl([128, NT, 20], F32, "Dc")
        sctmp = ExitStack()
        sbt = scoped_sb(sctmp)
        sm = sbt([128, NT, 24], F32, "sm")
        for (o, n, d_) in ((0, 16, cdtb_d), (16, 16, calog_d), (32, 16, cdskip_d), (48, 4, dib_d), (52, 4, dfb_d)):
            fw.dma("sync", par.ap[:, o:o + n], d_.partition_broadcast(128), writes=[par], sbuf=par)
        wdt = loadw(cdin_d[:, 2560:2576], 8, 16)
        wif = loadw(cdin_d[:, 5648:5656], 8, 8)
        pb = bank()
        for t in range(NT):
            for c in range(8):
                mm(pb.ap[:, t * 24:t * 24 + 16], hnT.ap[:, c, t * 128:(t + 1) * 128], wdt.ap[:, c, 0:16], c == 0, c == 7,
                   [wdt, hnT], [pb])
            for c in range(8):
                mm(pb.ap[:, t * 24 + 16:t * 24 + 24], hnT.ap[:, c, t * 128:(t + 1) * 128], wif.ap[:, c, 0:8], c == 0, c == 7,
                   [wif, hnT], [pb])
        evac(sm.ap, pb.ap[:, 0:NT * 24].rearrange("p (t n) -> p t n", t=NT), [pb], [sm])

        def bc_t(ap2d, n):
            return ap2d.unsqueeze(1).broadcast_to([128, NT, n])
        A(lambda e: e.activation(out=par.ap[:, 16:32], in_=par.ap[:, 16:32], func=AF.Exp), [par], [par])
        V(lambda e: e.tensor_scalar(out=par.ap[:, 16:32], in0=par.ap[:, 16:32], scalar1=-1.0, scalar2=None, op0=ALU.mult), [par], [par])
        V(lambda e: e.tensor_tensor(out=sm.ap[:, :, 0:16], in0=sm.ap[:, :, 0:16], in1=bc_t(par.ap[:, 0:16], 16), op=ALU.add), [sm, par], [sm])
        dtt = sbt([128, NT, 16], F32, "dtt")
        lndt = sbt([128, NT, 16], F32, "lndt")
        A(lambda e: e.activation(out=dtt.ap, in_=sm.ap[:, :, 0:16], func=AF.Exp), [sm], [dtt])
        A(lambda e: e.activation(out=dtt.ap, in_=dtt.ap, func=AF.Ln, bias=1.0), [dtt], [dtt])
        A(lambda e: e.activation(out=lndt.ap, in_=dtt.ap, func=AF.Ln), [dtt], [lndt])
        g20 = sbt([128, NT, 20], F32, "g20")
        V(lambda e: e.tensor_tensor(out=g20.ap[:, :, 0:16], in0=dtt.ap, in1=bc_t(par.ap[:, 16:32], 16), op=ALU.mult), [dtt, par], [g20])
        V(lambda e: e.tensor_tensor(out=sm.ap[:, :, 20:24], in0=sm.ap[:, :, 20:24], in1=bc_t(par.ap[:, 52:56], 4), op=ALU.add), [sm, par], [sm])
        A(lambda e: e.activation(out=g20.ap[:, :, 16:20], in_=sm.ap[:, :, 20:24], func=AF.Exp, scale=-1.0), [sm], [g20])
        A(lambda e: e.activation(out=g20.ap[:, :, 16:20], in_=g20.ap[:, :, 16:20], func=AF.Ln, bias=1.0), [g20], [g20])
        V(lambda e: e.tensor_scalar(out=g20.ap[:, :, 16:20], in0=g20.ap[:, :, 16:20], scalar1=-1.0, scalar2=None, op0=ALU.mult), [g20], [g20])
        V(lambda e: e.tensor_tensor(out=sm.ap[:, :, 16:20], in0=sm.ap[:, :, 16:20], in1=bc_t(par.ap[:, 48:52], 4), op=ALU.add), [sm, par], [sm])
        tot = sbt([128, NT, 20], F32, "tot")
        pb = bank()
        mm(pb.ap[:, 0:320], U_f, g20.ap.rearrange("p t n -> p (t n)"), True, True, [cst, g20], [pb])
        evac(acum.ap, pb.ap[:, 0:320].rearrange("p (t n) -> p t n", t=NT), [pb], [acum])
        pb = bank()
        mm(pb.ap[:, 0:320], ones_f, g20.ap.rearrange("p t n -> p (t n)"), True, True, [cst, g20], [pb])
        evac(tot.ap, pb.ap[:, 0:320].rearrange("p (t n) -> p t n", t=NT), [pb], [tot])
        for t in range(1, NT):
            V(lambda e: e.tensor_tensor(out=tot.ap[:, t, :], in0=tot.ap[:, t, :], in1=tot.ap[:, t - 1, :], op=ALU.add), [tot], [tot])
        V(lambda e: e.tensor_tensor(out=acum.ap[:, 1:NT, :], in0=acum.ap[:, 1:NT, :], in1=tot.ap[:, 0:NT - 1, :], op=ALU.add), [acum, tot], [acum])
        V(lambda e: e.tensor_tensor(out=bias.ap[:, :, 0:16], in0=lndt.ap, in1=acum.ap[:, :, 0:16], op=ALU.subtract), [lndt, acum], [bias])
        V(lambda e: e.tensor_tensor(out=bias.ap[:, :, 16:20], in0=sm.ap[:, :, 16:20], in1=acum.ap[:, :, 16:20], op=ALU.subtract), [sm, acum], [bias])

        V(lambda e: e.tensor_tensor(out=U_.ap, in0=bias.ap, in1=tot.ap, op=ALU.add), [bias, tot], [U_])
        A(lambda e: e.activation(out=U_.ap, in_=U_.ap, func=AF.Exp), [U_], [U_])
        V(lambda e: e.memset(Wt.ap[:, 0:1, :], 0.0), [], [Wt])
        V(lambda e: e.memset(Dc.ap[:, 0:1, :], 0.0), [], [Dc])
        V(lambda e: e.tensor_tensor(out=Wt.ap[:, 1:NT, :], in0=acum.ap[:, 1:NT, :], in1=tot.ap[:, 0:NT - 1, :], op=ALU.subtract),
          [acum, tot], [Wt])
        A(lambda e: e.activation(out=Wt.ap[:, 1:NT, :], in_=Wt.ap[:, 1:NT, :], func=AF.Exp), [Wt], [Wt])
        V(lambda e: e.tensor_tensor(out=Dc.ap[:, 1:NT, :], in0=tot.ap[:, 1:NT, :], in1=tot.ap[:, 0:NT - 1, :], op=ALU.subtract),
          [tot], [Dc])
        A(lambda e: e.activation(out=Dc.ap[:, 1:NT, :], in_=Dc.ap[:, 1:NT, :], func=AF.Exp), [Dc], [Dc])
        fw.barrier()
        sctmp.close()
        xpad = sbl([128, 515], F32, "xpad")
        cv = sbl([128, 512], F32, "cv")
        cw = sbl([128, 8], F32, "cw")
        fT = sbl([128, T], BF16, "fT")
        tmp = [sbl([128, 260], F32, "tmp") for _ in range(3)]
        fsm = sbl([128, 8], F32, "fsm")
        ssum = sbl([128, NT, 4], F32, "ssum")

        def proj_fm_conv(col0, convw_d, convb_d, ch0, dst):
            wb = loadw(cdin_d[:, col0:col0 + 128], 8)
            fw.dma("sync", cw.ap[:, 0:4], convw_d[:, ch0:ch0 + 128].rearrange("k c -> c k"), writes=[cw], sbuf=cw,
                   allow_slow_non_contiguous=True)
            fw.dma("sync", cw.ap[:, 4:5], convb_d[:, ch0:ch0 + 128].rearrange("o c -> c o"), writes=[cw], sbuf=cw,
                   allow_slow_non_contiguous=True)
            V(lambda e: e.memset(xpad.ap[:, 0:3], 0.0), [], [xpad])
            for tg in range(4):
                pb = bank()
                for c in range(8):
                    mm(pb.ap, wb.ap[:, c, :], hnT.ap[:, c, tg * 512:(tg + 1) * 512], c == 0, c == 7, [wb, hnT], [pb])
                A(lambda e: e.activation(out=xpad.ap[:, 3:515], in_=pb.ap, func=AF.Copy), [pb], [xpad])
                V(lambda e: e.tensor_scalar(out=cv.ap, in0=xpad.ap[:, 0:512], scalar1=cw.ap[:, 0:1], scalar2=None, op0=ALU.mult),
                  [xpad, cw], [cv])
                for k_ in range(1, 4):
                    V(lambda e: e.scalar_tensor_tensor(out=cv.ap, in0=xpad.ap[:, k_:k_ + 512], scalar=cw.ap[:, k_:k_ + 1], in1=cv.ap,
                                                       op0=ALU.mult, op1=ALU.add), [xpad, cw, cv], [cv])
                A(lambda e: e.activation(out=dst.ap[:, tg * 512:(tg + 1) * 512], in_=cv.ap, func=AF.Silu, bias=cw.ap[:, 4:5]),
                  [cv, cw], [dst])
                V(lambda e: e.tensor_copy(out=xpad.ap[:, 0:3], in_=xpad.ap[:, 512:515]), [xpad], [xpad])

        def proj_tm(col0, dst, dcol0):
            wb = loadw(cdin_d[:, col0:col0 + 128], 8)
            for t4 in range(4):
                pb = bank()
                for tt in range(4):
                    t = t4 * 4 + tt
                    for c in range(8):
                        mm(pb.ap[:, tt * 128:(tt + 1) * 128], hnT.ap[:, c, t * 128:(t + 1) * 128], wb.ap[:, c, :], c == 0, c == 7,
                           [wb, hnT], [pb])
                evac(dst.ap[:, t4 * 4:(t4 + 1) * 4, dcol0:dcol0 + 128], pb.ap.rearrange("p (t d) -> p t d", t=4), [pb], [dst])

        def fm_to_tm(src, dst, dcol0):
            for t4 in range(4):
                pb = bank()
                for tt in range(4):
                    t = t4 * 4 + tt
                    mm(pb.ap[:, tt * 128:(tt + 1) * 128], src.ap[:, t * 128:(t + 1) * 128], ident_b, True, True, [src, cstb], [pb])
                evac(dst.ap[:, t4 * 4:(t4 + 1) * 4, dcol0:dcol0 + 128], pb.ap.rearrange("p (t d) -> p t d", t=4), [pb], [dst])

        def chunk_attn(sbu, h0, nh, W, isC, gT_fn, g_bufs, v_view, v_hi, v_buf, kfm_fn, k_buf, nkb, q_fn, q_buf, finalize):
            nhW = nh * W
            RbL = [sbu([128, nh, 128], F32, "Rb") for _ in range(2)]
            ArL = [sbu([128, nh, 128], F32, "Ar") for _ in range(2)]
            El = [sbu([128, nh, 128], BF16, "E") for _ in range(2)]
            Ml = [sbu([128, nh, 128], BF16, "M") for _ in range(2)]
            xsuL = [sbu([128, nhW], BF16, "xsu") for _ in range(2)]
            ktmL = [sbu([128, nkb * 128], BF16, "ktm") for _ in range(2)]
            S32 = sbu([128, nkb, nhW], F32, "S32")
            SbfL = [sbu([128, nkb, nhW], BF16, "Sbf") for _ in range(2)]
            tS = sbu([128, nhW], F32, "tS")

            def banks(c):
                p = c % 2
                return PB[4 * p], PB[4 * p + 1], PB[4 * p + 2], PB[4 * p + 3]

            def pr_of(c):
                bA, bB, bC, bD = banks(c)
                return (bD, 0) if isC else (bB, nhW)

            def st_of(c, b):
                bA, bB, bC, bD = banks(c)
                bk = bC if (isC or b == 0) else bD
                return bk, 0, (256 if isC else 257)

            def setup1(c):
                Rb_, Ar_ = RbL[c % 2], ArL[c % 2]
                pr, po = pr_of(c)
                V(lambda e: e.tensor_tensor(out=Rb_.ap, in0=ident_f.unsqueeze(1).broadcast_to([128, nh, 128]),
                                            in1=acum.ap[:, c, h0:h0 + nh].unsqueeze(2).broadcast_to([128, nh, 128]), op=ALU.mult),
                  [cst, acum], [Rb_])
                mm(pr.ap[:, po:po + nh * 128], ones_f, Rb_.ap.rearrange("p h l -> p (h l)"), True, True, [cst, Rb_], [pr])
                A(lambda e: e.activation(out=Ar_.ap, in_=pr.ap[:, po:po + nh * 128].rearrange("p (h l) -> p h l", h=nh), func=AF.Copy),
                  [pr], [Ar_])

            def setup2(c):
                Rb_, Ar_ = RbL[c % 2], ArL[c % 2]
                V(lambda e: e.tensor_tensor(out=Rb_.ap, in0=Ar_.ap, in1=NEG_f.unsqueeze(1).broadcast_to([128, nh, 128]), op=ALU.add),
                  [Ar_, cst], [Rb_])

            def prep(c):
                xsu, ktm = xsuL[c % 2], ktmL[c % 2]
                V(lambda e: e.tensor_tensor(out=xsu.ap.rearrange("p (h w) -> p h w", h=nh), in0=v_view(c),
                                            in1=U_.ap[:, c, h0:h0 + nh].unsqueeze(2).broadcast_to([128, nh, W]), op=ALU.mult),
                  [v_buf, U_], [xsu])
                for b in range(nkb):
                    bk, so, ko = st_of(c, b)
                    mm(bk.ap[:, ko:ko + 128], kfm_fn(c, b), ident_b, True, True, [k_buf, cstb], [bk])
                    evac(ktm.ap[:, b * 128:(b + 1) * 128], bk.ap[:, ko:ko + 128], [bk], [ktm])
                for b in range(nkb):
                    bk, so, ko = st_of(c, b)
                    mm(bk.ap[:, so:so + nhW], ktm.ap[:, b * 128:(b + 1) * 128], xsu.ap, True, True, [ktm, xsu], [bk])

            setup1(0)
            setup2(0)
            prep(0)
            for c in range(NT):
                bA, bB, bC, bD = banks(c)
                if c + 1 < NT:
                    setup1(c + 1)
                V(lambda e: e.memset(bA.ap[:, 0:nhW], 0.0), [], [bA])
                if c > 0:
                    V(lambda e: e.memset(bB.ap[:, 0:nhW], 0.0), [], [bB])
                gT_fn(c, bA.ap[:, nhW:nhW + 128], bA)
                if c + 1 < NT:
                    prep(c + 1)
                E_, M_, Rb_ = El[c % 2], Ml[c % 2], RbL[c % 2]
                for hi in range(nh):
                    A(lambda e: e.activation(out=E_.ap[:, hi, :], in_=Rb_.ap[:, hi, :], func=AF.Exp,
                                             bias=bias.ap[:, c, h0 + hi:h0 + hi + 1]), [Rb_, bias], [E_])
                V(lambda e: e.tensor_tensor(out=M_.ap, in0=bA.ap[:, nhW:nhW + 128].unsqueeze(1).broadcast_to([128, nh, 128]),
                                            in1=E_.ap, op=ALU.mult), [bA, E_], [M_])
                for hi in range(nh):
                    mm(bA.ap[:, hi * W:(hi + 1) * W], M_.ap[:, hi, :], v_hi(c, hi), False, False, [M_, v_buf], [bA])
                if c > 0:
                    Sb = SbfL[c % 2]
                    for b in range(nkb):
                        mm(bB.ap[:, 0:nhW], q_fn(c, b), Sb.ap[:, b, :], False, False, [q_buf, Sb], [bB])
                if c + 1 < NT:
                    Sn = SbfL[(c + 1) % 2]
                    for b in range(nkb):
                        bk, so, ko = st_of(c, b)
                        if c == 0:
                            V(lambda e: e.tensor_copy(out=S32.ap[:, b, :], in_=bk.ap[:, so:so + nhW]), [bk], [S32])
                            A(lambda e: e.activation(out=Sn.ap[:, b, :], in_=bk.ap[:, so:so + nhW], func=AF.Copy), [bk], [Sn])
                        else:
                            V(lambda e: e.tensor_tensor(out=tS.ap.rearrange("p (h w) -> p h w", h=nh),
                                                        in0=S32.ap[:, b, :].rearrange("p (h w) -> p h w", h=nh),
                                                        in1=Dc.ap[:, c, h0:h0 + nh].unsqueeze(2).broadcast_to([128, nh, W]), op=ALU.mult),
                              [S32, Dc], [tS])
                            V(lambda e: e.tensor_tensor(out=S32.ap[:, b, :], in0=tS.ap, in1=bk.ap[:, so:so + nhW], op=ALU.add),
                              [tS, bk], [S32])
                            A(lambda e: e.activation(out=Sn.ap[:, b, :], in_=S32.ap[:, b, :], func=AF.Copy), [S32], [Sn])
                    setup2(c + 1)
                finalize(c, bA, (bB if c > 0 else None))

        with ExitStack() as scC:
            sbc = scoped_sb(scC)
            bmT = sbc([128, T], BF16, "bmT")
            cmT = sbc([128, T], BF16, "cmT")
            xs_tm = sbc([128, NT, 256], BF16, "xs_tm")
            for cu in range(4):
                g = cu // 2
                if cu % 2 == 0:
                    proj_fm_conv(1024 + 1024 + g * 128, cconvw_d, cconvb_d, 1024 + g * 128, bmT)
                    proj_fm_conv(1024 + 1280 + g * 128, cconvw_d, cconvb_d, 1280 + g * 128, cmT)
                for blk in range(2):
                    ch0 = cu * 256 + blk * 128
                    proj_fm_conv(1024 + ch0, cconvw_d, cconvb_d, ch0, fT)
                    fm_to_tm(fT, xs_tm, blk * 128)
                    proj_tm(ch0, ytm, cu * 256 + blk * 128)
                A(lambda e: e.activation(out=ytm.ap[:, :, cu * 256:(cu + 1) * 256], in_=ytm.ap[:, :, cu * 256:(cu + 1) * 256],
                                         func=AF.Silu), [ytm], [ytm])

                def gT_c(c, out_ap, bk):
                    mm(out_ap, bmT.ap[:, c * 128:(c + 1) * 128], cmT.ap[:, c * 128:(c + 1) * 128], True, True, [bmT, cmT], [bk])

                def fin_c(c, bA, bB, cu=cu):
                    y_, t1, sz = tmp
                    h0 = cu * 4
                    V(lambda e: e.tensor_tensor(out=y_.ap[:, 0:256].rearrange("p (h w) -> p h w", h=4),
                                                in0=xs_tm.ap[:, c, :].rearrange("p (h w) -> p h w", h=4),
                                                in1=par.ap[:, 32 + h0:32 + h0 + 4].unsqueeze(2).broadcast_to([128, 4, 64]), op=ALU.mult),
                      [xs_tm, par], [y_])
                    V(lambda e: e.tensor_tensor(out=y_.ap[:, 0:256], in0=y_.ap[:, 0:256], in1=bA.ap[:, 0:256], op=ALU.add), [y_, bA], [y_])
                    if bB is not None:
                        V(lambda e: e.tensor_tensor(out=t1.ap[:, 0:256].rearrange("p (h w) -> p h w", h=4),
                                                    in0=bB.ap[:, 0:256].rearrange("p (h w) -> p h w", h=4),
                                                    in1=Wt.ap[:, c, h0:h0 + 4].unsqueeze(2).broadcast_to([128, 4, 64]), op=ALU.mult),
                          [bB, Wt], [t1])
                        V(lambda e: e.tensor_tensor(out=y_.ap[:, 0:256], in0=y_.ap[:, 0:256], in1=t1.ap[:, 0:256], op=ALU.add), [y_, t1], [y_])
                    V(lambda e: e.tensor_tensor(out=ytm.ap[:, c, cu * 256:(cu + 1) * 256], in0=y_.ap[:, 0:256],
                                                in1=ytm.ap[:, c, cu * 256:(cu + 1) * 256], op=ALU.mult), [y_, ytm], [ytm])
                    A(lambda e: e.activation(out=sz.ap[:, 0:256], in_=ytm.ap[:, c, cu * 256:(cu + 1) * 256], func=AF.Square,
                                             accum_out=ssum.ap[:, c, cu:cu + 1]), [ytm], [sz, ssum])

                with ExitStack() as scu:
                    chunk_attn(scoped_sb(scu), cu * 4, 4, 64, True, gT_c, [bmT, cmT],
                               lambda c: xs_tm.ap[:, c, :].rearrange("p (h w) -> p h w", h=4),
                               lambda c, hi: xs_tm.ap[:, c, hi * 64:(hi + 1) * 64], xs_tm,
                               lambda c, b: bmT.ap[:, c * 128:(c + 1) * 128], bmT, 1,
                               lambda c, b: cmT.ap[:, c * 128:(c + 1) * 128], cmT, fin_c)
                    fw.barrier()
            fw.barrier()
        with ExitStack() as scN:
            sbn = scoped_sb(scN)
            cnw = sbn([128, D], F32, "cnw")
            rs = sbn([128, NT], F32, "rs")
            fw.dma("sync", cnw.ap, cnormw_d.partition_broadcast(128), writes=[cnw], sbuf=cnw)
            V(lambda e: e.reduce_sum(out=rs.ap, in_=ssum.ap, axis=mybir.AxisListType.X), [ssum], [rs])
            A(lambda e: e.activation(out=rs.ap, in_=rs.ap, func=AF.Ln, scale=1.0 / D, bias=EPS), [rs], [rs])
            A(lambda e: e.activation(out=rs.ap, in_=rs.ap, func=AF.Exp, scale=-0.5), [rs], [rs])
            for qb in range(NT):
                V(lambda e: e.scalar_tensor_tensor(out=ytm.ap[:, qb, :], in0=ytm.ap[:, qb, :], scalar=rs.ap[:, qb:qb + 1], in1=cnw.ap,
                                                   op0=ALU.mult, op1=ALU.mult), [ytm, rs, cnw], [ytm])
            fw.barrier()
        out_proj_tiled(cdout_d[0:D, :])

        with ExitStack() as scD:
            sbd = scoped_sb(scD)
            qT2 = sbd([128, 2, T], BF16, "qT2")
            kT2 = sbd([128, 2, T], BF16, "kT2")
            v_tm = sbd([128, NT, 258], BF16, "v_tm")
            dnw = sbd([128, 256], F32, "dnw")
            V(lambda e: e.memset(v_tm.ap[:, :, 256:258], 1.0), [], [v_tm])
            for h in range(4):
                fw.dma("sync", dnw.ap, dnormw_d[:, h * 256:(h + 1) * 256].partition_broadcast(128), writes=[dnw], sbuf=dnw)
                for blk in range(2):
                    ch0 = h * 256 + blk * 128
                    proj_fm_conv(2576 + ch0, dconvw_d, dconvb_d, ch0, fT)
                    wq_ = loadw(dwq_d[ch0 // 128], 1)
                    wk_ = loadw(dwk_d[ch0 // 128], 1)
                    for tg in range(4):
                        pb = bank()
                        mm(pb.ap, wq_.ap[:, 0, :], fT.ap[:, tg * 512:(tg + 1) * 512], True, True, [wq_, fT], [pb])
                        evac(qT2.ap[:, blk, tg * 512:(tg + 1) * 512], pb.ap, [pb], [qT2])
                        pb = bank()
                        mm(pb.ap, wk_.ap[:, 0, :], fT.ap[:, tg * 512:(tg + 1) * 512], True, True, [wk_, fT], [pb])
                        A(lambda e: e.activation(out=kT2.ap[:, blk, tg * 512:(tg + 1) * 512], in_=pb.ap, func=AF.Copy, scale=0.0625),
                          [pb], [kT2])
                    proj_tm(3600 + ch0, v_tm, blk * 128)
                    proj_tm(4624 + ch0, ytm, h * 256 + blk * 128)
                A(lambda e: e.activation(out=ytm.ap[:, :, h * 256:(h + 1) * 256], in_=ytm.ap[:, :, h * 256:(h + 1) * 256],
                                         func=AF.Sigmoid), [ytm], [ytm])

                def gT_d(c, out_ap, bk):
                    for blk in range(2):
                        mm(out_ap, kT2.ap[:, blk, c * 128:(c + 1) * 128], qT2.ap[:, blk, c * 128:(c + 1) * 128],
                           blk == 0, blk == 1, [kT2, qT2], [bk])

                def fin_d(c, bA, bB, h=h):
                    t_, hd, t1 = tmp
                    if bB is not None:
                        V(lambda e: e.tensor_scalar(out=t_.ap[:, 0:257], in0=bB.ap[:, 0:257], scalar1=Wt.ap[:, c, 16 + h:17 + h], scalar2=None,
                                                    op0=ALU.mult), [bB, Wt], [t_])
                        V(lambda e: e.tensor_tensor(out=t_.ap[:, 0:257], in0=t_.ap[:, 0:257], in1=bA.ap[:, 0:257], op=ALU.add), [t_, bA], [t_])
                    else:
                        V(lambda e: e.tensor_copy(out=t_.ap[:, 0:257], in_=bA.ap[:, 0:257]), [bA], [t_])
                    V(lambda e: e.tensor_scalar(out=fsm.ap[:, 5:6], in0=t_.ap[:, 256:257], scalar1=-1.0, scalar2=None, op0=ALU.mult),
                      [t_], [fsm])
                    V(lambda e: e.tensor_tensor(out=fsm.ap[:, 0:1], in0=t_.ap[:, 256:257], in1=fsm.ap[:, 5:6], op=ALU.max), [t_, fsm], [fsm])
                    V(lambda e: e.tensor_scalar(out=fsm.ap[:, 0:1], in0=fsm.ap[:, 0:1], scalar1=1.0, scalar2=None, op0=ALU.max), [fsm], [fsm])
                    V(lambda e: e.reciprocal(out=fsm.ap[:, 1:2], in_=fsm.ap[:, 0:1]), [fsm], [fsm])
                    V(lambda e: e.tensor_scalar(out=hd.ap[:, 0:256], in0=t_.ap[:, 0:256], scalar1=fsm.ap[:, 1:2], scalar2=None, op0=ALU.mult),
                      [t_, fsm], [hd])
                    A(lambda e: e.activation(out=t1.ap[:, 0:256], in_=hd.ap[:, 0:256], func=AF.Square, accum_out=fsm.ap[:, 2:3]), [hd], [t1, fsm])
                    A(lambda e: e.activation(out=fsm.ap[:, 3:4], in_=fsm.ap[:, 2:3], func=AF.Ln, scale=1.0 / 256, bias=EPS), [fsm], [fsm])
                    A(lambda e: e.activation(out=fsm.ap[:, 4:5], in_=fsm.ap[:, 3:4], func=AF.Exp, scale=-0.5), [fsm], [fsm])
                    V(lambda e: e.scalar_tensor_tensor(out=t1.ap[:, 0:256], in0=hd.ap[:, 0:256], scalar=fsm.ap[:, 4:5], in1=dnw.ap,
                                                       op0=ALU.mult, op1=ALU.mult), [hd, fsm, dnw], [t1])
                    V(lambda e: e.tensor_tensor(out=ytm.ap[:, c, h * 256:(h + 1) * 256], in0=t1.ap[:, 0:256],
                                                in1=ytm.ap[:, c, h * 256:(h + 1) * 256], op=ALU.mult), [t1, ytm], [ytm])

                with ExitStack() as scu:
                    chunk_attn(scoped_sb(scu), 16 + h, 1, 257, False, gT_d, [kT2, qT2],
                               lambda c: v_tm.ap[:, c:c + 1, 0:257],
                               lambda c, hi: v_tm.ap[:, c, 0:257], v_tm,
                               lambda c, b: kT2.ap[:, b, c * 128:(c + 1) * 128], kT2, 2,
                               lambda c, b: qT2.ap[:, b, c * 128:(c + 1) * 128], qT2, fin_d)
                    fw.barrier()
            fw.barrier()
        out_proj_tiled(cdout_d[D:2 * D, :])
        fw.barrier()
        scope.close()

    for s in range(nseq):
        load_x(s)
        if do_l0:
            layer0_mix()
            if do_ffn:
                ffn(0)
        if do_l1:
            layer1_mix()
            if do_ffn:
                ffn(1)
        final_store(s)
    fw.barrier()
    print("program: ops=%d waits=%d" % (fw.nops, fw.nwaits))
    return nc


def block_diag(w):
    w = np.asarray(w, np.float32)
    o = np.zeros((8, 128, 128), np.float32)
    for n in range(256):
        b, r = divmod(n, 32)
        o[b, 4 * r:4 * r + 4, 4 * r:4 * r + 4] = w[n]
    return o


NSEQ = 4
_prog_cache = {}


def make_in_maps(inputs, nseq, ncores):
    f = lambda a: np.ascontiguousarray(np.asarray(a, np.float32))
    cf, cb = make_consts()
    shared = {
        "consts": cf, "constsb": cb,
        "mix_norm_w": f(inputs["mix_norm_w"]), "ffn_norm_w": f(inputs["ffn_norm_w"]),
        "final_norm_w": f(inputs["final_norm_w"]).reshape(1, D),
        "ab_w_in": f(inputs["ab_w_in"][0]), "ab_w_out": f(inputs["ab_w_out"][0]),
        "diff_lq1": f(inputs["diff_lq1"]), "diff_lk1": f(inputs["diff_lk1"]),
        "diff_lq2": f(inputs["diff_lq2"]), "diff_lk2": f(inputs["diff_lk2"]),
        "diff_subln_w": f(inputs["diff_subln_w"]),
        "cd_w_in": f(inputs["cd_w_in"][0]),
        "c_conv_w": f(inputs["c_conv_w"][0]), "c_conv_b": f(inputs["c_conv_b"]),
        "c_dt_bias": f(inputs["c_dt_bias"]), "c_a_log": f(inputs["c_a_log"]), "c_d_skip": f(inputs["c_d_skip"]),
        "c_norm_w": f(inputs["c_norm_w"]),
        "d_conv_w": f(inputs["d_conv_w"][0]), "d_conv_b": f(inputs["d_conv_b"]),
        "d_wq_bd": block_diag(inputs["d_wq"][0]), "d_wk_bd": block_diag(inputs["d_wk"][0]),
        "d_i_bias": f(inputs["d_i_bias"]), "d_f_bias": f(inputs["d_f_bias"]),
        "d_norm_w": f(inputs["d_norm_w"]),
        "cd_w_out": f(inputs["cd_w_out"][0]),
        "ffn_w_gate_up": f(inputs["ffn_w_gate_up"]), "ffn_w_down": f(inputs["ffn_w_down"]),
    }
    x = f(inputs["x"])
    maps = []
    for c in range(ncores):
        m = dict(shared)
        m["x"] = np.ascontiguousarray(x[c * nseq:(c + 1) * nseq])
        maps.append(m)
    return maps


def kernel(**inputs):
    ncores = 8
    nseq = NSEQ
    nc = build_program(nseq)
    in_maps = make_in_maps(inputs, nseq, ncores)
    res = run_bass_kernel_spmd(nc, in_maps, core_ids=list(range(ncores)))
    return np.concatenate([np.asarray(r["out"], np.float32) for r in res.results], axis=0)
```

```python
import math
from contextlib import ExitStack
import numpy as np
import concourse.bass as bass
import concourse.mybir as mybir
from concourse.bass_utils import run_bass_kernel_spmd

F32 = mybir.dt.float32
BF16 = mybir.dt.bfloat16
AF = mybir.ActivationFunctionType
ALU = mybir.AluOpType

T = 2048
D = 1024
NT = 16
FFN_H = 2816
EPS = 1e-6
AB_COLS = 3072
CD_COLS = 5656
NEGM = -30000.0
DEFER_FINALIZE = True


class _Dep:
    def __init__(self):
        self.w = {}
        self.r = {}
        self.dsem = None


class Buf:
    def __init__(self, ap, name="", owner=None, excl=False):
        self.ap = ap
        self.name = name
        self.excl = excl
        self.d = owner.d if owner is not None else _Dep()

    w = property(lambda self: self.d.w, lambda self, v: setattr(self.d, "w", v))
    r = property(lambda self: self.d.r, lambda self, v: setattr(self.d, "r", v))
    dsem = property(lambda self: self.d.dsem, lambda self, v: setattr(self.d, "dsem", v))


class Fw:
    ENG = ["tensor", "vector", "scalar", "gpsimd", "sync"]

    def __init__(self, nc):
        self.nc = nc
        self.sem = {}
        self.cnt = {}
        self.waited = {}
        self.semobj = {}
        for e in self.ENG:
            s = nc.alloc_semaphore(name="s_" + e)
            self.sem[e] = s
            self.cnt[e] = 0
            self.waited[e] = {}
        self.dma_total = {}
        self.nwaits = 0
        self.nops = 0

    def eng(self, e):
        return getattr(self.nc, e)

    def _wait(self, e, deps):
        w = self.waited[e]
        for k, v in deps.items():
            if k[0] == 'e':
                if k[1] == e and e == "tensor":
                    continue
                sem = self.sem[k[1]]
            else:
                sem = self.semobj[k[1]]
                v = max(v, self.dma_total[k[1]])
            if w.get(k, 0) >= v:
                continue
            self.eng(e).wait_ge(sem, v)
            self.nwaits += 1
            w[k] = v

    @staticmethod
    def _merge(d, k, v):
        if d.get(k, 0) < v:
            d[k] = v

    def _deps(self, reads, writes):
        deps = {}
        for b in reads:
            for k, v in b.w.items():
                self._merge(deps, k, v)
        for b in writes:
            for k, v in b.w.items():
                self._merge(deps, k, v)
            for k, v in b.r.items():
                self._merge(deps, k, v)
        return deps

    def _record(self, key, val, reads, writes):
        for b in reads:
            self._merge(b.r, key, val)
        for b in writes:
            self._merge(b.w, key, val)
            b.r = {}

    def op(self, e, fn, reads=(), writes=()):
        if any(b.excl for b in reads):
            writes = list(writes) + [b for b in reads if b.excl]
            reads = [b for b in reads if not b.excl]
        self._wait(e, self._deps(reads, writes))
        ins = fn(self.eng(e))
        self.cnt[e] += 1
        ins.then_inc(self.sem[e], 1)
        self.nops += 1
        self._record(('e', e), self.cnt[e], reads, writes)
        return ins

    def dma(self, q, out_ap, in_ap, reads=(), writes=(), sbuf=None, **kw):
        self._wait(q, self._deps(reads, writes))
        if sbuf.dsem is None:
            sbuf.dsem = self.nc.alloc_semaphore(name="d_" + sbuf.name)
            self.semobj[id(sbuf.dsem)] = sbuf.dsem
            self.dma_total[id(sbuf.dsem)] = 0
        ins = self.eng(q).dma_start(out=out_ap, in_=in_ap, **kw)
        ins.then_inc(sbuf.dsem, 16)
        self.dma_total[id(sbuf.dsem)] += 16
        self._record(('d', id(sbuf.dsem)), self.dma_total[id(sbuf.dsem)], reads, writes)
        return ins

    def barrier(self):
        deps = {('e', e): self.cnt[e] for e in self.ENG if self.cnt[e] > 0}
        for k, v in self.dma_total.items():
            if v > 0:
                deps[('d', k)] = v
        for e in self.ENG:
            self._wait(e, dict(deps))


C_ID = 0
C_U = 128
C_NEG = 256
C_ONES = 384
C_TOT = 512
B_ID = 0
B_MA = 128
B_MB = B_MA + 2304
B_TOT = B_MB + 384


def make_consts():
    c = np.zeros((128, C_TOT), np.float32)
    cb = np.zeros((128, B_TOT), np.float32)
    s = np.arange(128)[:, None]
    c[:, C_ID:C_ID + 128] = np.eye(128, dtype=np.float32)
    cb[:, B_ID:B_ID + 128] = np.eye(128, dtype=np.float32)
    cc = np.arange(2304)[None, :]
    d = cc - 128 - s
    mult = ((d >= 0) & (d <= 128)).astype(np.float32)
    mult += ((d >= 0) & (d % 4 == 0) & (d <= 512)).astype(np.float32)
    mult += ((d >= 0) & (d % 16 == 0) & (d <= 2048)).astype(np.float32)
    cb[:, B_MA:B_MA + 2304] = mult
    cc = np.arange(384)[None, :]
    cb[:, B_MB:B_MB + 384] = ((cc - 128 - s) >= 0).astype(np.float32)
    l = np.arange(128)[None, :]
    c[:, C_U:C_U + 128] = (s <= l).astype(np.float32)
    c[:, C_NEG:C_NEG + 128] = np.where(s > l, NEGM, 0.0).astype(np.float32)
    c[:, C_ONES:C_ONES + 128] = 1.0
    return c, cb


def build_program(nseq, do_l0=True, do_l1=True, do_ffn=True, units=tuple(range(8)), ngroups=8, stage=9):
    nc = bass.Bass("TRN2", target_bir_lowering=False)
    fw = Fw(nc)

    def din(name, shape):
        return nc.dram_tensor(name, list(shape), F32, kind="ExternalInput").ap()

    x_d = din("x", [nseq, T, D])
    consts_d = din("consts", [128, C_TOT])
    constsb_d = din("constsb", [128, B_TOT])
    mixg_d = din("mix_norm_w", [2, D])
    ffng_d = din("ffn_norm_w", [2, D])
    fing_d = din("final_norm_w", [1, D])
    abin_d = din("ab_w_in", [D, AB_COLS])
    about_d = din("ab_w_out", [D, D])
    lq1_d = din("diff_lq1", [1, 64]); lk1_d = din("diff_lk1", [1, 64])
    lq2_d = din("diff_lq2", [1, 64]); lk2_d = din("diff_lk2", [1, 64])
    subln_d = din("diff_subln_w", [1, 128])
    cdin_d = din("cd_w_in", [D, CD_COLS])
    cconvw_d = din("c_conv_w", [4, 1536]); cconvb_d = din("c_conv_b", [1, 1536])
    cdtb_d = din("c_dt_bias", [1, 16]); calog_d = din("c_a_log", [1, 16]); cdskip_d = din("c_d_skip", [1, 16])
    cnormw_d = din("c_norm_w", [1, D])
    dconvw_d = din("d_conv_w", [4, D]); dconvb_d = din("d_conv_b", [1, D])
    dwq_d = din("d_wq_bd", [8, 128, 128]); dwk_d = din("d_wk_bd", [8, 128, 128])
    dib_d = din("d_i_bias", [1, 4]); dfb_d = din("d_f_bias", [1, 4])
    dnormw_d = din("d_norm_w", [1, D])
    cdout_d = din("cd_w_out", [2 * D, D])
    wgu_d = din("ffn_w_gate_up", [2, D, 2 * FFN_H])
    wdn_d = din("ffn_w_down", [2, FFN_H, D])
    out_d = nc.dram_tensor("out", [nseq, T, D], F32, kind="ExternalOutput").ap()

    cnt = [0]

    def sb(shape, dt=F32, name=None):
        cnt[0] += 1
        nm = (name or "t") + "_%d" % cnt[0]
        return Buf(nc.alloc_sbuf_tensor(nm, list(shape), dt).ap(), nm)

    def scoped_sb(scope):
        def f(shape, dt=F32, name=None):
            cnt[0] += 1
            nm = (name or "t") + "_%d" % cnt[0]
            return Buf(scope.enter_context(nc.sbuf_tensor(nm, list(shape), dt)).ap(), nm)
        return f

    hT = sb([128, 8, T], F32, "hT")
    cst = sb([128, C_TOT], F32, "cst")
    cstb = sb([128, B_TOT], BF16, "cstb")
    gam = sb([128, 5, 8], F32, "gam")
    PB = []
    for i in range(8):
        PB.append(Buf(nc.alloc_psum_tensor("pb%d" % i, [128, 512], F32).ap(), "pb%d" % i, excl=True))
    pb_rr = [0]

    def bank():
        b = PB[pb_rr[0] % 8]
        pb_rr[0] += 1
        return b

    fw.dma("sync", cst.ap, consts_d, writes=[cst], sbuf=cst)
    fw.dma("gpsimd", cstb.ap, constsb_d, writes=[cstb], sbuf=cstb)
    for i, (g_d, row) in enumerate([(mixg_d, 0), (ffng_d, 0), (mixg_d, 1), (ffng_d, 1), (fing_d, 0)]):
        fw.dma("sync", gam.ap[:, i, :], g_d[row, :].rearrange("(c p) -> p c", p=128), writes=[gam], sbuf=gam,
               allow_slow_non_contiguous=True)
    ident_f = cst.ap[:, C_ID:C_ID + 128]
    ident_b = cstb.ap[:, B_ID:B_ID + 128]
    ones_f = cst.ap[:, C_ONES:C_ONES + 128]

    NW = 4
    wbf = [sb([128, 8, 128], BF16, "wbf") for _ in range(NW)]
    w_rr = [0]

    def loadw(dram_rows_cols, nchunk, ncols=128):
        i = w_rr[0] % NW
        w_rr[0] += 1
        bf = wbf[i]
        fw.dma("gpsimd", bf.ap[:, 0:nchunk, 0:ncols], dram_rows_cols.rearrange("(c p) n -> p c n", p=128),
               writes=[bf], sbuf=bf)
        return bf

    evac_rr = [0]
    evac_act_only = [False]

    def evac(out_ap, in_ap, reads, writes):
        evac_rr[0] += 1
        if evac_act_only[0] or evac_rr[0] % 2 == 0:
            fw.op("scalar", lambda e: e.activation(out=out_ap, in_=in_ap, func=AF.Copy), reads=reads, writes=writes)
        else:
            fw.op("vector", lambda e: e.tensor_copy(out=out_ap, in_=in_ap), reads=reads, writes=writes)

    def mm(out_ap, lhsT, rhs, start, stop, reads, writes):
        fw.op("tensor", lambda e: e.matmul(out_ap, lhsT=lhsT, rhs=rhs, start=start, stop=stop, skip_group_check=True),
              reads=reads, writes=writes)

    sq = [sb([128, 512], F32, "sq") for _ in range(2)]
    rstd = sb([128, 512], F32, "rstd")

    def rms_stats(tg):
        pb = bank()
        for c in range(8):
            s = sq[c % 2]
            fw.op("scalar", lambda e: e.activation(out=s.ap, in_=hT.ap[:, c, tg * 512:(tg + 1) * 512], func=AF.Square),
                  reads=[hT], writes=[s])
            mm(pb.ap, ones_f, s.ap, c == 0, c == 7, [s, cst], [pb])
        fw.op("scalar", lambda e: e.activation(out=rstd.ap, in_=pb.ap, func=AF.Ln, scale=1.0 / D, bias=EPS),
              reads=[pb], writes=[rstd])
        fw.op("scalar", lambda e: e.activation(out=rstd.ap, in_=rstd.ap, func=AF.Exp, scale=-0.5),
              reads=[rstd], writes=[rstd])

    def rmsnorm_to(dst, gi, dst_dtype_is_f32=False):
        for tg in range(4):
            rms_stats(tg)
            for c in range(8):
                fw.op("vector", lambda e: e.scalar_tensor_tensor(
                    out=dst.ap[:, c, tg * 512:(tg + 1) * 512], in0=hT.ap[:, c, tg * 512:(tg + 1) * 512],
                    scalar=gam.ap[:, gi, c:c + 1], in1=rstd.ap, op0=ALU.mult, op1=ALU.mult),
                    reads=[hT, gam, rstd], writes=[dst])

    hnT = sb([128, 8, T], BF16, "hnT")
    ytm = sb([128, NT, D], BF16, "ytm")
    actT = Buf(ytm.ap.rearrange("p t d -> p (t d)").rearrange("p (h t) -> p h t", h=8), "actT", owner=ytm)

    def load_x(s):
        sc = ExitStack()
        sbx = scoped_sb(sc)
        xin = [sbx([128, D], F32, "xin") for _ in range(2)]
        for t in range(NT):
            xt = xin[t % 2]
            fw.dma("sync", xt.ap, x_d[s, t * 128:(t + 1) * 128, :], writes=[xt], sbuf=xt)
            for g in range(2):
                pb = bank()
                for cc in range(4):
                    c = g * 4 + cc
                    mm(pb.ap[:, cc * 128:(cc + 1) * 128], xt.ap[:, c * 128:(c + 1) * 128], ident_f, True, True,
                       [xt, cst], [pb])
                evac(hT.ap[:, g * 4:(g + 1) * 4, t * 128:(t + 1) * 128],
                     pb.ap.rearrange("p (c n) -> p c n", c=4), [pb], [hT])
        fw.barrier()
        sc.close()

    def transpose_ytm_to_hnT():
        for t in range(NT):
            for g in range(2):
                pb = bank()
                for cc in range(4):
                    c = g * 4 + cc
                    mm(pb.ap[:, cc * 128:(cc + 1) * 128], ytm.ap[:, t, c * 128:(c + 1) * 128], ident_b, True, True,
                       [ytm, cstb], [pb])
                evac(hnT.ap[:, g * 4:(g + 1) * 4, t * 128:(t + 1) * 128],
                     pb.ap.rearrange("p (c n) -> p c n", c=4), [pb], [hnT])

    def out_proj(w_dram):
        for cb in range(8):
            wb = loadw(w_dram[:, cb * 128:(cb + 1) * 128], 8)
            for tg in range(4):
                pb = bank()
                for c in range(8):
                    mm(pb.ap, wb.ap[:, c, :], hnT.ap[:, c, tg * 512:(tg + 1) * 512], c == 0, c == 7, [wb, hnT], [pb])
                fw.op("vector", lambda e: e.tensor_tensor(out=hT.ap[:, cb, tg * 512:(tg + 1) * 512],
                                                          in0=hT.ap[:, cb, tg * 512:(tg + 1) * 512], in1=pb.ap, op=ALU.add),
                      reads=[hT, pb], writes=[hT])

    def layer0_mix():
        scope = ExitStack()
        sb = scoped_sb(scope)
        qT = sb([128, T], BF16, "qT")
        kT = sb([128, 2, T], BF16, "kT")
        vA = sb([128, NT, 2, 65], BF16, "vA")
        vB = sb([128, NT, 129], BF16, "vB")
        PT = [sb([128, 2, 256], BF16, "PT") for _ in range(4)]
        lamw = sb([128, 8], F32, "lamw")
        lqk = sb([128, 4, 64], F32, "lqk")
        sublnw = sb([128, 128], F32, "sublnw")
        fin_s = [sb([128, 8], F32, "fin_s") for _ in range(2)]
        fin_t = [sb([128, 128], F32, "fin_t") for _ in range(2)]
        fin_o = [sb([128, 128], F32, "fin_o") for _ in range(2)]
        fin_j = sb([128, 128], F32, "fin_j")

        fw.op("vector", lambda e: e.memset(kT.ap, 0.0), writes=[kT])
        fw.op("vector", lambda e: e.memset(vA.ap, 1.0), writes=[vA])
        fw.op("vector", lambda e: e.memset(vB.ap, 1.0), writes=[vB])
        for i, d_ in enumerate([lq1_d, lk1_d, lq2_d, lk2_d]):
            fw.dma("sync", lqk.ap[:, i, :], d_.partition_broadcast(128), writes=[lqk], sbuf=lqk)
        fw.dma("sync", sublnw.ap, subln_d.partition_broadcast(128), writes=[sublnw], sbuf=sublnw)
        lambda_init = 0.8 - 0.6 * math.exp(-0.3 * 0)
        fw.op("vector", lambda e: e.tensor_scalar(out=sublnw.ap, in0=sublnw.ap, scalar1=1.0 - lambda_init, scalar2=None,
                                                  op0=ALU.mult), reads=[sublnw], writes=[sublnw])
        fw.op("vector", lambda e: e.tensor_tensor(out=lqk.ap[:, 0, :], in0=lqk.ap[:, 0, :], in1=lqk.ap[:, 1, :], op=ALU.mult),
              reads=[lqk], writes=[lqk])
        fw.op("vector", lambda e: e.tensor_tensor(out=lqk.ap[:, 2, :], in0=lqk.ap[:, 2, :], in1=lqk.ap[:, 3, :], op=ALU.mult),
              reads=[lqk], writes=[lqk])
        fw.op("vector", lambda e: e.reduce_sum(out=lamw.ap[:, 1:2], in_=lqk.ap[:, 0, :], axis=mybir.AxisListType.X),
              reads=[lqk], writes=[lamw])
        fw.op("vector", lambda e: e.reduce_sum(out=lamw.ap[:, 2:3], in_=lqk.ap[:, 2, :], axis=mybir.AxisListType.X),
              reads=[lqk], writes=[lamw])
        fw.op("scalar", lambda e: e.activation(out=lamw.ap[:, 3:5], in_=lamw.ap[:, 1:3], func=AF.Exp), reads=[lamw], writes=[lamw])
        fw.op("vector", lambda e: e.tensor_tensor(out=lamw.ap[:, 0:1], in0=lamw.ap[:, 4:5], in1=lamw.ap[:, 3:4], op=ALU.subtract),
              reads=[lamw], writes=[lamw])
        fw.op("vector", lambda e: e.tensor_scalar(out=lamw.ap[:, 0:1], in0=lamw.ap[:, 0:1], scalar1=-lambda_init, scalar2=None,
                                                  op0=ALU.add), reads=[lamw], writes=[lamw])

        def attn_unit(u):
            isA = u < 4
            qoff = (0 if isA else 1536) + (u % 4) * 128
            koff = qoff + 512
            voff = qoff + 1024
            wq = loadw(abin_d[:, qoff:qoff + 128], 8)
            wk = loadw(abin_d[:, koff:koff + 128], 8)
            wv = loadw(abin_d[:, voff:voff + 128], 8)
            for (wb, dst) in ((wq, qT), (wk, kT)):
                for tg in range(4):
                    pb = bank()
                    for c in range(8):
                        mm(pb.ap, wb.ap[:, c, :], hnT.ap[:, c, tg * 512:(tg + 1) * 512], c == 0, c == 7, [wb, hnT], [pb])
                    if dst is qT:
                        evac(dst.ap[:, tg * 512:(tg + 1) * 512], pb.ap, [pb], [dst])
                    else:
                        evac(dst.ap[0:64, 0, tg * 512:(tg + 1) * 512], pb.ap[0:64, :], [pb], [dst])
                        evac(dst.ap[64:128, 1, tg * 512:(tg + 1) * 512], pb.ap[64:128, :], [pb], [dst])
            for t4 in range(4):
                pb = bank()
                for tt in range(4):
                    t = t4 * 4 + tt
                    for c in range(8):
                        mm(pb.ap[:, tt * 128:(tt + 1) * 128], hnT.ap[:, c, t * 128:(t + 1) * 128], wv.ap[:, c, :],
                           c == 0, c == 7, [wv, hnT], [pb])
                if isA:
                    evac(vA.ap[:, t4 * 4:(t4 + 1) * 4, :, 0:64], pb.ap.rearrange("p (t i d) -> p t i d", t=4, i=2), [pb], [vA])
                else:
                    evac(vB.ap[:, t4 * 4:(t4 + 1) * 4, 0:128], pb.ap.rearrange("p (t d) -> p t d", t=4), [pb], [vB])
            W = 65 if isA else 129
            if stage < 1:
                return
            steps = [(G, j) for G in range(ngroups) for j in range(2 * G + 2)]
            nst = len(steps)
            vbuf = vA if isA else vB

            def acc_of(G, i, b):
                return PB[2 * (G % 2) + i], b * W

            def emit_S(k):
                G, j = steps[k]
                sbk = PB[4 + (k % 4)]
                for i in range(2):
                    mm(sbk.ap[:, i * 256:(i + 1) * 256], kT.ap[:, i, j * 128:(j + 1) * 128],
                       qT.ap[:, G * 256:(G + 1) * 256], True, True, [kT, qT], [sbk])

            def finalize(G):
                for b in range(2):
                    qb = 2 * G + b
                    fs = fin_s[b]
                    if isA:
                        for i in range(2):
                            a, o = acc_of(G, i, b)
                            fw.op("vector", lambda e: e.reciprocal(out=fs.ap[:, i:i + 1], in_=a.ap[:, o + 64:o + 65]), reads=[a], writes=[fs])
                            fw.op("vector", lambda e: e.tensor_scalar(
                                out=ytm.ap[:, qb, u * 128 + 64 * i:u * 128 + 64 * i + 64], in0=a.ap[:, o:o + 64],
                                scalar1=fs.ap[:, i:i + 1], scalar2=None, op0=ALU.mult), reads=[a, fs], writes=[ytm])
                    else:
                        a0, o0 = acc_of(G, 0, b)
                        a1, o1 = acc_of(G, 1, b)
                        ft, fo = fin_t[b], fin_o[b]
                        fw.op("vector", lambda e: e.reciprocal(out=fs.ap[:, 0:1], in_=a0.ap[:, o0 + 128:o0 + 129]), reads=[a0], writes=[fs])
                        fw.op("vector", lambda e: e.reciprocal(out=fs.ap[:, 1:2], in_=a1.ap[:, o1 + 128:o1 + 129]), reads=[a1], writes=[fs])
                        fw.op("vector", lambda e: e.tensor_tensor(out=fs.ap[:, 2:3], in0=fs.ap[:, 1:2], in1=lamw.ap[:, 0:1], op=ALU.mult),
                              reads=[fs, lamw], writes=[fs])
                        fw.op("vector", lambda e: e.tensor_scalar(out=ft.ap, in0=a0.ap[:, o0:o0 + 128], scalar1=fs.ap[:, 0:1], scalar2=None,
                                                                  op0=ALU.mult), reads=[a0, fs], writes=[ft])
                        fw.op("vector", lambda e: e.scalar_tensor_tensor(out=fo.ap, in0=a1.ap[:, o1:o1 + 128], scalar=fs.ap[:, 2:3],
                                                                         in1=ft.ap, op0=ALU.mult, op1=ALU.add),
                              reads=[a1, fs, ft], writes=[fo])
                        fw.op("scalar", lambda e: e.activation(out=fin_j.ap, in_=fo.ap, func=AF.Square, accum_out=fs.ap[:, 3:4]),
                              reads=[fo], writes=[fin_j, fs])
                        fw.op("scalar", lambda e: e.activation(out=fs.ap[:, 4:5], in_=fs.ap[:, 3:4], func=AF.Ln, scale=1.0 / 128, bias=EPS),
                              reads=[fs], writes=[fs])
                        fw.op("scalar", lambda e: e.activation(out=fs.ap[:, 5:6], in_=fs.ap[:, 4:5], func=AF.Exp, scale=-0.5),
                              reads=[fs], writes=[fs])
                        fw.op("vector", lambda e: e.scalar_tensor_tensor(
                            out=ytm.ap[:, qb, u * 128:(u + 1) * 128], in0=fo.ap, scalar=fs.ap[:, 5:6], in1=sublnw.ap,
                            op0=ALU.mult, op1=ALU.mult), reads=[fo, fs, sublnw], writes=[ytm])

            LA = 2
            for k0 in range(min(LA, nst)):
                emit_S(k0)
            pending = None
            for k, (G, j) in enumerate(steps):
                if j == 0:
                    for i in range(2):
                        a, _ = acc_of(G, i, 0)
                        fw.op("vector", lambda e: e.memset(a.ap[:, 0:2 * W], 0.0), writes=[a])
                if k + LA < nst:
                    emit_S(k + LA)
                sbk = PB[4 + (k % 4)]
                pt = PT[k % 4]
                fw.op("scalar", lambda e: e.activation(out=pt.ap, in_=sbk.ap.rearrange("p (i n) -> p i n", i=2),
                                                       func=AF.Exp, scale=0.125), reads=[sbk], writes=[pt])
                d0 = 2 * G - j
                if isA:
                    m = cstb.ap[:, B_MA + 128 * (d0 + 1):B_MA + 128 * (d0 + 1) + 256]
                elif d0 <= 0:
                    m = cstb.ap[:, B_MB + 128 * (d0 + 1):B_MB + 128 * (d0 + 1) + 256]
                else:
                    m = None
                if m is not None:
                    mb_ = m.unsqueeze(1).broadcast_to([128, 2, 256])
                    fw.op("vector", lambda e: e.tensor_tensor(out=pt.ap, in0=pt.ap, in1=mb_, op=ALU.mult),
                          reads=[pt, cstb], writes=[pt])
                for b in range(2):
                    if 2 * G + b < j:
                        continue
                    for i in range(2):
                        rhs = vA.ap[:, j, i, :] if isA else vB.ap[:, j, :]
                        a, o = acc_of(G, i, b)
                        mm(a.ap[:, o:o + W], pt.ap[:, i, b * 128:(b + 1) * 128], rhs, False, False, [pt, vbuf], [a])
                if pending is not None and k >= pending[1]:
                    finalize(pending[0])
                    pending = None
                if j == 2 * G + 1:
                    if pending is not None:
                        finalize(pending[0])
                    pending = (G, k + 2)
            if pending is not None:
                finalize(pending[0])

        rmsnorm_to(hnT, 0)
        for u in units:
            attn_unit(u)
        transpose_ytm_to_hnT()
        out_proj(about_d)
        fw.barrier()
        scope.close()

    silu_t = sq

    def ffn(layer):
        rmsnorm_to(hnT, 1 + 2 * layer)
        for (b0, nb) in ((0, 8), (8, 7), (15, 7)):
            for hb in range(nb):
                col = (b0 + hb) * 128
                wg = loadw(wgu_d[layer, :, col:col + 128], 8)
                wu = loadw(wgu_d[layer, :, FFN_H + col:FFN_H + col + 128], 8)
                for tg in range(4):
                    pg = bank()
                    pu = bank()
                    for c in range(8):
                        mm(pg.ap, wg.ap[:, c, :], hnT.ap[:, c, tg * 512:(tg + 1) * 512], c == 0, c == 7, [wg, hnT], [pg])
                    for c in range(8):
                        mm(pu.ap, wu.ap[:, c, :], hnT.ap[:, c, tg * 512:(tg + 1) * 512], c == 0, c == 7, [wu, hnT], [pu])
                    st = silu_t[tg % 2]
                    fw.op("scalar", lambda e: e.activation(out=st.ap, in_=pg.ap, func=AF.Silu), reads=[pg], writes=[st])
                    fw.op("vector", lambda e: e.tensor_tensor(out=actT.ap[:, hb, tg * 512:(tg + 1) * 512], in0=st.ap, in1=pu.ap,
                                                              op=ALU.mult), reads=[st, pu], writes=[actT])
            for cb in range(8):
                wd = loadw(wdn_d[layer, b0 * 128:(b0 + nb) * 128, cb * 128:(cb + 1) * 128], nb)
                for tg in range(4):
                    pb = bank()
                    for hb in range(nb):
                        mm(pb.ap, wd.ap[:, hb, :], actT.ap[:, hb, tg * 512:(tg + 1) * 512], hb == 0, hb == nb - 1, [wd, actT], [pb])
                    fw.op("vector", lambda e: e.tensor_tensor(out=hT.ap[:, cb, tg * 512:(tg + 1) * 512],
                                                              in0=hT.ap[:, cb, tg * 512:(tg + 1) * 512], in1=pb.ap, op=ALU.add),
                          reads=[hT, pb], writes=[hT])

    def final_store(s):
        sc = ExitStack()
        sbx = scoped_sb(sc)
        onT = [sbx([128, 8, 128], F32, "onT") for _ in range(2)]
        ost = [sbx([128, D], F32, "ost") for _ in range(2)]
        for tg in range(4):
            rms_stats(tg)
            for tt in range(4):
                t = tg * 4 + tt
                o_n = onT[t % 2]
                fw.op("vector", lambda e: e.scalar_tensor_tensor(
                    out=o_n.ap, in0=hT.ap[:, :, t * 128:(t + 1) * 128], scalar=1.0,
                    in1=rstd.ap[:, tt * 128:(tt + 1) * 128].unsqueeze(1).broadcast_to([128, 8, 128]),
                    op0=ALU.mult, op1=ALU.mult), reads=[hT, rstd], writes=[o_n])
                fw.op("vector", lambda e: e.tensor_tensor(
                    out=o_n.ap, in0=o_n.ap, in1=gam.ap[:, 4, :].unsqueeze(2).broadcast_to([128, 8, 128]), op=ALU.mult),
                    reads=[o_n, gam], writes=[o_n])
                os_ = ost[t % 2]
                for g in range(2):
                    pb = bank()
                    for cc in range(4):
                        c = g * 4 + cc
                        mm(pb.ap[:, cc * 128:(cc + 1) * 128], o_n.ap[:, c, :], ident_f, True, True, [o_n, cst], [pb])
                    evac(os_.ap[:, g * 512:(g + 1) * 512], pb.ap, [pb], [os_])
                fw.dma("sync", out_d[s, t * 128:(t + 1) * 128, :], os_.ap, reads=[os_], sbuf=os_)
        fw.barrier()
        sc.close()

    U_f = cst.ap[:, C_U:C_U + 128]
    NEG_f = cst.ap[:, C_NEG:C_NEG + 128]

    def out_proj_tiled(w_dram):
        with ExitStack() as sc:
            sbl = scoped_sb(sc)
            yT = [sbl([128, 8, 512], BF16, "yT") for _ in range(2)]
            for tg in range(4):
                y_ = yT[tg % 2]
                for tt in range(4):
                    t = tg * 4 + tt
                    for g in range(2):
                        pb = bank()
                        for cc in range(4):
                            c = g * 4 + cc
                            mm(pb.ap[:, cc * 128:(cc + 1) * 128], ytm.ap[:, t, c * 128:(c + 1) * 128], ident_b, True, True,
                               [ytm, cstb], [pb])
                        evac(y_.ap[:, g * 4:(g + 1) * 4, tt * 128:(tt + 1) * 128],
                             pb.ap.rearrange("p (c n) -> p c n", c=4), [pb], [y_])
                for cb in range(8):
                    wb = loadw(w_dram[:, cb * 128:(cb + 1) * 128], 8)
                    pb = bank()
                    for c in range(8):
                        mm(pb.ap, wb.ap[:, c, :], y_.ap[:, c, :], c == 0, c == 7, [wb, y_], [pb])
                    fw.op("vector", lambda e: e.tensor_tensor(out=hT.ap[:, cb, tg * 512:(tg + 1) * 512],
                                                              in0=hT.ap[:, cb, tg * 512:(tg + 1) * 512], in1=pb.ap, op=ALU.add),
                          reads=[hT, pb], writes=[hT])
            fw.barrier()

    def layer1_mix():
        scope = ExitStack()
        sbl = scoped_sb(scope)
        evac_act_only[0] = True
        rmsnorm_to(hnT, 2)
        V = lambda fn, r, w: fw.op("vector", fn, reads=r, writes=w)
        A = lambda fn, r, w: fw.op("scalar", fn, reads=r, writes=w)
        par = sbl([128, 64], F32, "par")
        acum = sbl([128, NT, 20], F32, "acum")
        bias = sbl([128, NT, 20], F32, "bias")
        U_ = sbl([128, NT, 20], F32, "U_")
        Wt = sbl([128, NT, 20], F32, "Wt")
        Dc = sbl([128, NT, 20], F32, "Dc")
        sctmp = ExitStack()
        sbt = scoped_sb(sctmp)
        sm = sbt([128, NT, 24], F32, "sm")
        for (o, n, d_) in ((0, 16, cdtb_d), (16, 16, calog_d), (32, 16, cdskip_d), (48, 4, dib_d), (52, 4, dfb_d)):
            fw.dma("sync", par.ap[:, o:o + n], d_.partition_broadcast(128), writes=[par], sbuf=par)
        wdt = loadw(cdin_d[:, 2560:2576], 8, 16)
        wif = loadw(cdin_d[:, 5648:5656], 8, 8)
        pb = bank()
        for t in range(NT):
            for c in range(8):
                mm(pb.ap[:, t * 24:t * 24 + 16], hnT.ap[:, c, t * 128:(t + 1) * 128], wdt.ap[:, c, 0:16], c == 0, c == 7,
                   [wdt, hnT], [pb])
            for c in range(8):
                mm(pb.ap[:, t * 24 + 16:t * 24 + 24], hnT.ap[:, c, t * 128:(t + 1) * 128], wif.ap[:, c, 0:8], c == 0, c == 7,
                   [wif, hnT], [pb])
        evac(sm.ap, pb.ap[:, 0:NT * 24].rearrange("p (t n) -> p t n", t=NT), [pb], [sm])

        def bc_t(ap2d, n):
            return ap2d.unsqueeze(1).broadcast_to([128, NT, n])
        A(lambda e: e.activation(out=par.ap[:, 16:32], in_=par.ap[:, 16:32], func=AF.Exp), [par], [par])
        V(lambda e: e.tensor_scalar(out=par.ap[:, 16:32], in0=par.ap[:, 16:32], scalar1=-1.0, scalar2=None, op0=ALU.mult), [par], [par])
        V(lambda e: e.tensor_tensor(out=sm.ap[:, :, 0:16], in0=sm.ap[:, :, 0:16], in1=bc_t(par.ap[:, 0:16], 16), op=ALU.add), [sm, par], [sm])
        dtt = sbt([128, NT, 16], F32, "dtt")
        lndt = sbt([128, NT, 16], F32, "lndt")
        A(lambda e: e.activation(out=dtt.ap, in_=sm.ap[:, :, 0:16], func=AF.Exp), [sm], [dtt])
        A(lambda e: e.activation(out=dtt.ap, in_=dtt.ap, func=AF.Ln, bias=1.0), [dtt], [dtt])
        A(lambda e: e.activation(out=lndt.ap, in_=dtt.ap, func=AF.Ln), [dtt], [lndt])
        g20 = sbt([128, NT, 20], F32, "g20")
        V(lambda e: e.tensor_tensor(out=g20.ap[:, :, 0:16], in0=dtt.ap, in1=bc_t(par.ap[:, 16:32], 16), op=ALU.mult), [dtt, par], [g20])
        V(lambda e: e.tensor_tensor(out=sm.ap[:, :, 20:24], in0=sm.ap[:, :, 20:24], in1=bc_t(par.ap[:, 52:56], 4), op=ALU.add), [sm, par], [sm])
        A(lambda e: e.activation(out=g20.ap[:, :, 16:20], in_=sm.ap[:, :, 20:24], func=AF.Exp, scale=-1.0), [sm], [g20])
        A(lambda e: e.activation(out=g20.ap[:, :, 16:20], in_=g20.ap[:, :, 16:20], func=AF.Ln, bias=1.0), [g20], [g20])
        V(lambda e: e.tensor_scalar(out=g20.ap[:, :, 16:20], in0=g20.ap[:, :, 16:20], scalar1=-1.0, scalar2=None, op0=ALU.mult), [g20], [g20])
        V(lambda e: e.tensor_tensor(out=sm.ap[:, :, 16:20], in0=sm.ap[:, :, 16:20], in1=bc_t(par.ap[:, 48:52], 4), op=ALU.add), [sm, par], [sm])
        tot = sbt([128, NT, 20], F32, "tot")
        pb = bank()
        mm(pb.ap[:, 0:320], U_f, g20.ap.rearrange("p t n -> p (t n)"), True, True, [cst, g20], [pb])
        evac(acum.ap, pb.ap[:, 0:320].rearrange("p (t n) -> p t n", t=NT), [pb], [acum])
        pb = bank()
        mm(pb.ap[:, 0:320], ones_f, g20.ap.rearrange("p t n -> p (t n)"), True, True, [cst, g20], [pb])
        evac(tot.ap, pb.ap[:, 0:320].rearrange("p (t n) -> p t n", t=NT), [pb], [tot])
        for t in range(1, NT):
            V(lambda e: e.tensor_tensor(out=tot.ap[:, t, :], in0=tot.ap[:, t, :], in1=tot.ap[:, t - 1, :], op=ALU.add), [tot], [tot])
        V(lambda e: e.tensor_tensor(out=acum.ap[:, 1:NT, :], in0=acum.ap[:, 1:NT, :], in1=tot.ap[:, 0:NT - 1, :], op=ALU.add), [acum, tot], [acum])
        V(lambda e: e.tensor_tensor(out=bias.ap[:, :, 0:16], in0=lndt.ap, in1=acum.ap[:, :, 0:16], op=ALU.subtract), [lndt, acum], [bias])
        V(lambda e: e.tensor_tensor(out=bias.ap[:, :, 16:20], in0=sm.ap[:, :, 16:20], in1=acum.ap[:, :, 16:20], op=ALU.subtract), [sm, acum], [bias])

        V(lambda e: e.tensor_tensor(out=U_.ap, in0=bias.ap, in1=tot.ap, op=ALU.add), [bias, tot], [U_])
        A(lambda e: e.activation(out=U_.ap, in_=U_.ap, func=AF.Exp), [U_], [U_])
        V(lambda e: e.memset(Wt.ap[:, 0:1, :], 0.0), [], [Wt])
        V(lambda e: e.memset(Dc.ap[:, 0:1, :], 0.0), [], [Dc])
        V(lambda e: e.tensor_tensor(out=Wt.ap[:, 1:NT, :], in0=acum.ap[:, 1:NT, :], in1=tot.ap[:, 0:NT - 1, :], op=ALU.subtract),
          [acum, tot], [Wt])
        A(lambda e: e.activation(out=Wt.ap[:, 1:NT, :], in_=Wt.ap[:, 1:NT, :], func=AF.Exp), [Wt], [Wt])
        V(lambda e: e.tensor_tensor(out=Dc.ap[:, 1:NT, :], in0=tot.ap[:, 1:NT, :], in1=tot.ap[:, 0:NT - 1, :], op=ALU.subtract),
          [tot], [Dc])
        A(lambda e: e.activation(out=Dc.ap[:, 1:NT, :], in_=Dc.ap[:, 1:NT, :], func=AF.Exp), [Dc], [Dc])
        fw.barrier()
        sctmp.close()
        xpadL = [sbl([128, 516], F32, "xpad") for _ in range(2)]
        cvL = [sbl([128, 512], F32, "cv") for _ in range(2)]
        cw = sbl([128, 8], F32, "cw")
        fT = sbl([128, T], BF16, "fT")
        tmp = [sbl([128, 260], F32, "tmp") for _ in range(3)]
        fsm = sbl([128, 8], F32, "fsm")
        ssum = sbl([128, NT, 4], F32, "ssum")

        def proj_fm_conv(col0, convw_d, convb_d, ch0, dst):
            wb = loadw(cdin_d[:, col0:col0 + 128], 8)
            fw.dma("sync", cw.ap[:, 0:4], convw_d[:, ch0:ch0 + 128].rearrange("k c -> c k"), writes=[cw], sbuf=cw,
                   allow_slow_non_contiguous=True)
            fw.dma("sync", cw.ap[:, 4:5], convb_d[:, ch0:ch0 + 128].rearrange("o c -> c o"), writes=[cw], sbuf=cw,
                   allow_slow_non_contiguous=True)
            V(lambda e: e.memset(xpadL[0].ap[:, 0:3], 0.0), [], [xpadL[0]])
            for pair in range(2):
                for i in range(2):
                    tg = pair * 2 + i
                    xpad = xpadL[i]
                    pb = bank()
                    for c in range(8):
                        mm(pb.ap, wb.ap[:, c, :], hnT.ap[:, c, tg * 512:(tg + 1) * 512], c == 0, c == 7, [wb, hnT], [pb])
                    A(lambda e: e.activation(out=xpad.ap[:, 3:515], in_=pb.ap, func=AF.Copy), [pb], [xpad])
                    if i == 0:
                        V(lambda e: e.tensor_copy(out=xpadL[1].ap[:, 0:3], in_=xpad.ap[:, 512:515]), [xpad], [xpadL[1]])
                for i in range(2):
                    V(lambda e: e.tensor_scalar(out=cvL[i].ap, in0=xpadL[i].ap[:, 0:512], scalar1=cw.ap[:, 0:1], scalar2=None, op0=ALU.mult),
                      [xpadL[i], cw], [cvL[i]])
                for k_ in range(1, 4):
                    for i in range(2):
                        V(lambda e: e.scalar_tensor_tensor(out=cvL[i].ap, in0=xpadL[i].ap[:, k_:k_ + 512], scalar=cw.ap[:, k_:k_ + 1],
                                                           in1=cvL[i].ap, op0=ALU.mult, op1=ALU.add), [xpadL[i], cw, cvL[i]], [cvL[i]])
                if pair == 0:
                    V(lambda e: e.tensor_copy(out=xpadL[0].ap[:, 0:3], in_=xpadL[1].ap[:, 512:515]), [xpadL[1]], [xpadL[0]])
                for i in range(2):
                    tg = pair * 2 + i
                    A(lambda e: e.activation(out=dst.ap[:, tg * 512:(tg + 1) * 512], in_=cvL[i].ap, func=AF.Silu, bias=cw.ap[:, 4:5]),
                      [cvL[i], cw], [dst])

        def proj_tm(col0, dst, dcol0):
            wb = loadw(cdin_d[:, col0:col0 + 128], 8)
            for t4 in range(4):
                pb = bank()
                for tt in range(4):
                    t = t4 * 4 + tt
                    for c in range(8):
                        mm(pb.ap[:, tt * 128:(tt + 1) * 128], hnT.ap[:, c, t * 128:(t + 1) * 128], wb.ap[:, c, :], c == 0, c == 7,
                           [wb, hnT], [pb])
                evac(dst.ap[:, t4 * 4:(t4 + 1) * 4, dcol0:dcol0 + 128], pb.ap.rearrange("p (t d) -> p t d", t=4), [pb], [dst])

        def fm_to_tm(src, dst, dcol0):
            for t4 in range(4):
                pb = bank()
                for tt in range(4):
                    t = t4 * 4 + tt
                    mm(pb.ap[:, tt * 128:(tt + 1) * 128], src.ap[:, t * 128:(t + 1) * 128], ident_b, True, True, [src, cstb], [pb])
                evac(dst.ap[:, t4 * 4:(t4 + 1) * 4, dcol0:dcol0 + 128], pb.ap.rearrange("p (t d) -> p t d", t=4), [pb], [dst])

        def chunk_attn(sbu, h0, nh, W, isC, gT_fn, g_bufs, v_view, v_hi, v_buf, kfm_fn, k_buf, nkb, q_fn, q_buf, finalize):
            nhW = nh * W
            RbL = [sbu([128, nh, 128], F32, "Rb") for _ in range(2)]
            ArL = [sbu([128, nh, 128], F32, "Ar") for _ in range(2)]
            El = [sbu([128, nh, 128], BF16, "E") for _ in range(2)]
            Ml = [sbu([128, nh, 128], BF16, "M") for _ in range(2)]
            xsuL = [sbu([128, nhW], BF16, "xsu") for _ in range(2)]
            ktmL = [sbu([128, nkb * 128], BF16, "ktm") for _ in range(2)]
            S32 = sbu([128, nkb, nhW], F32, "S32")
            SbfL = [sbu([128, nkb, nhW], BF16, "Sbf") for _ in range(2)]
            tS = sbu([128, nhW], F32, "tS")

            dq = []

            def Vd(fn, r, w):
                if DEFER_FINALIZE:
                    dq.append(("vector", fn, r, w))
                else:
                    fw.op("vector", fn, reads=r, writes=w)

            def Ad(fn, r, w):
                if DEFER_FINALIZE:
                    dq.append(("scalar", fn, r, w))
                else:
                    fw.op("scalar", fn, reads=r, writes=w)

            def pop_deferred(n=1):
                for _ in range(n):
                    if dq:
                        e_, fn_, r_, w_ = dq.pop(0)
                        fw.op(e_, fn_, reads=r_, writes=w_)

            def banks(c):
                p = c % 2
                return PB[4 * p], PB[4 * p + 1], PB[4 * p + 2], PB[4 * p + 3]

            def pr_of(c):
                bA, bB, bC, bD = banks(c)
                return (bD, 0) if isC else (bB, nhW)

            def st_of(c, b):
                bA, bB, bC, bD = banks(c)
                bk = bC if (isC or b == 0) else bD
                return bk, 0, (256 if isC else 257)

            def setup1(c):
                Rb_, Ar_ = RbL[c % 2], ArL[c % 2]
                pr, po = pr_of(c)
                V(lambda e: e.tensor_tensor(out=Rb_.ap, in0=ident_f.unsqueeze(1).broadcast_to([128, nh, 128]),
                                            in1=acum.ap[:, c, h0:h0 + nh].unsqueeze(2).broadcast_to([128, nh, 128]), op=ALU.mult),
                  [cst, acum], [Rb_])
                pop_deferred(2)
                mm(pr.ap[:, po:po + nh * 128], ones_f, Rb_.ap.rearrange("p h l -> p (h l)"), True, True, [cst, Rb_], [pr])
                A(lambda e: e.activation(out=Ar_.ap, in_=pr.ap[:, po:po + nh * 128].rearrange("p (h l) -> p h l", h=nh), func=AF.Copy),
                  [pr], [Ar_])

            def setup2(c):
                Rb_, Ar_ = RbL[c % 2], ArL[c % 2]
                V(lambda e: e.tensor_tensor(out=Rb_.ap, in0=Ar_.ap, in1=NEG_f.unsqueeze(1).broadcast_to([128, nh, 128]), op=ALU.add),
                  [Ar_, cst], [Rb_])

            def prep(c):
                xsu, ktm = xsuL[c % 2], ktmL[c % 2]
                V(lambda e: e.tensor_tensor(out=xsu.ap.rearrange("p (h w) -> p h w", h=nh), in0=v_view(c),
                                            in1=U_.ap[:, c, h0:h0 + nh].unsqueeze(2).broadcast_to([128, nh, W]), op=ALU.mult),
                  [v_buf, U_], [xsu])
                pop_deferred(2)
                for b in range(nkb):
                    bk, so, ko = st_of(c, b)
                    mm(bk.ap[:, ko:ko + 128], kfm_fn(c, b), ident_b, True, True, [k_buf, cstb], [bk])
                    evac(ktm.ap[:, b * 128:(b + 1) * 128], bk.ap[:, ko:ko + 128], [bk], [ktm])
                for b in range(nkb):
                    bk, so, ko = st_of(c, b)
                    mm(bk.ap[:, so:so + nhW], ktm.ap[:, b * 128:(b + 1) * 128], xsu.ap, True, True, [ktm, xsu], [bk])

            setup1(0)
            setup2(0)
            prep(0)
            for c in range(NT):
                bA, bB, bC, bD = banks(c)
                if c + 1 < NT:
                    setup1(c + 1)
                gT_fn(c, bA.ap[:, nhW:nhW + 128], bA)
                if c + 1 < NT:
                    prep(c + 1)
                E_, M_, Rb_ = El[c % 2], Ml[c % 2], RbL[c % 2]
                for hi in range(nh):
                    A(lambda e: e.activation(out=E_.ap[:, hi, :], in_=Rb_.ap[:, hi, :], func=AF.Exp,
                                             bias=bias.ap[:, c, h0 + hi:h0 + hi + 1]), [Rb_, bias], [E_])
                V(lambda e: e.tensor_tensor(out=M_.ap, in0=bA.ap[:, nhW:nhW + 128].unsqueeze(1).broadcast_to([128, nh, 128]),
                                            in1=E_.ap, op=ALU.mult), [bA, E_], [M_])
                pop_deferred(2)
                for hi in range(nh):
                    mm(bA.ap[:, hi * W:(hi + 1) * W], M_.ap[:, hi, :], v_hi(c, hi), False, False, [M_, v_buf], [bA])
                if c > 0:
                    Sb = SbfL[c % 2]
                    for b in range(nkb):
                        mm(bB.ap[:, 0:nhW], q_fn(c, b), Sb.ap[:, b, :], b == 0, b == nkb - 1, [q_buf, Sb], [bB])
                if c + 1 < NT:
                    Sn = SbfL[(c + 1) % 2]
                    for b in range(nkb):
                        bk, so, ko = st_of(c, b)
                        if c == 0:
                            V(lambda e: e.tensor_copy(out=S32.ap[:, b, :], in_=bk.ap[:, so:so + nhW]), [bk], [S32])
                            A(lambda e: e.activation(out=Sn.ap[:, b, :], in_=bk.ap[:, so:so + nhW], func=AF.Copy), [bk], [Sn])
                        else:
                            V(lambda e: e.tensor_tensor(out=tS.ap.rearrange("p (h w) -> p h w", h=nh),
                                                        in0=S32.ap[:, b, :].rearrange("p (h w) -> p h w", h=nh),
                                                        in1=Dc.ap[:, c, h0:h0 + nh].unsqueeze(2).broadcast_to([128, nh, W]), op=ALU.mult),
                              [S32, Dc], [tS])
                            V(lambda e: e.tensor_tensor(out=S32.ap[:, b, :], in0=tS.ap, in1=bk.ap[:, so:so + nhW], op=ALU.add),
                              [tS, bk], [S32])
                            A(lambda e: e.activation(out=Sn.ap[:, b, :], in_=S32.ap[:, b, :], func=AF.Copy), [S32], [Sn])
                    pop_deferred(2)
                    setup2(c + 1)
                pop_deferred(len(dq))
                finalize(c, bA, (bB if c > 0 else None), Vd, Ad)
            pop_deferred(len(dq))

        with ExitStack() as scC:
            sbc = scoped_sb(scC)
            bmT = sbc([128, T], BF16, "bmT")
            cmT = sbc([128, T], BF16, "cmT")
            xs_tm = sbc([128, NT, 256], BF16, "xs_tm")
            for cu in range(4):
                g = cu // 2
                if cu % 2 == 0:
                    proj_fm_conv(1024 + 1024 + g * 128, cconvw_d, cconvb_d, 1024 + g * 128, bmT)
                    proj_fm_conv(1024 + 1280 + g * 128, cconvw_d, cconvb_d, 1280 + g * 128, cmT)
                for blk in range(2):
                    ch0 = cu * 256 + blk * 128
                    proj_fm_conv(1024 + ch0, cconvw_d, cconvb_d, ch0, fT)
                    fm_to_tm(fT, xs_tm, blk * 128)
                    proj_tm(ch0, ytm, cu * 256 + blk * 128)
                A(lambda e: e.activation(out=ytm.ap[:, :, cu * 256:(cu + 1) * 256], in_=ytm.ap[:, :, cu * 256:(cu + 1) * 256],
                                         func=AF.Silu), [ytm], [ytm])

                def gT_c(c, out_ap, bk):
                    mm(out_ap, bmT.ap[:, c * 128:(c + 1) * 128], cmT.ap[:, c * 128:(c + 1) * 128], True, True, [bmT, cmT], [bk])

                def fin_c(c, bA, bB, V, A, cu=cu):
                    y_, t1, sz = tmp
                    h0 = cu * 4
                    V(lambda e: e.tensor_tensor(out=y_.ap[:, 0:256].rearrange("p (h w) -> p h w", h=4),
                                                in0=xs_tm.ap[:, c, :].rearrange("p (h w) -> p h w", h=4),
                                                in1=par.ap[:, 32 + h0:32 + h0 + 4].unsqueeze(2).broadcast_to([128, 4, 64]), op=ALU.mult),
                      [xs_tm, par], [y_])
                    V(lambda e: e.tensor_tensor(out=y_.ap[:, 0:256], in0=y_.ap[:, 0:256], in1=bA.ap[:, 0:256], op=ALU.add), [y_, bA], [y_])
                    if bB is not None:
                        V(lambda e: e.tensor_tensor(out=t1.ap[:, 0:256].rearrange("p (h w) -> p h w", h=4),
                                                    in0=bB.ap[:, 0:256].rearrange("p (h w) -> p h w", h=4),
                                                    in1=Wt.ap[:, c, h0:h0 + 4].unsqueeze(2).broadcast_to([128, 4, 64]), op=ALU.mult),
                          [bB, Wt], [t1])
                        V(lambda e: e.tensor_tensor(out=y_.ap[:, 0:256], in0=y_.ap[:, 0:256], in1=t1.ap[:, 0:256], op=ALU.add), [y_, t1], [y_])
                    V(lambda e: e.tensor_tensor(out=ytm.ap[:, c, cu * 256:(cu + 1) * 256], in0=y_.ap[:, 0:256],
                                                in1=ytm.ap[:, c, cu * 256:(cu + 1) * 256], op=ALU.mult), [y_, ytm], [ytm])
                    A(lambda e: e.activation(out=sz.ap[:, 0:256], in_=ytm.ap[:, c, cu * 256:(cu + 1) * 256], func=AF.Square,
                                             accum_out=ssum.ap[:, c, cu:cu + 1]), [ytm], [sz, ssum])

                with ExitStack() as scu:
                    chunk_attn(scoped_sb(scu), cu * 4, 4, 64, True, gT_c, [bmT, cmT],
                               lambda c: xs_tm.ap[:, c, :].rearrange("p (h w) -> p h w", h=4),
                               lambda c, hi: xs_tm.ap[:, c, hi * 64:(hi + 1) * 64], xs_tm,
                               lambda c, b: bmT.ap[:, c * 128:(c + 1) * 128], bmT, 1,
                               lambda c, b: cmT.ap[:, c * 128:(c + 1) * 128], cmT, fin_c)
                    fw.barrier()
            fw.barrier()
        with ExitStack() as scN:
            sbn = scoped_sb(scN)
            cnw = sbn([128, D], F32, "cnw")
            rs = sbn([128, NT], F32, "rs")
            fw.dma("sync", cnw.ap, cnormw_d.partition_broadcast(128), writes=[cnw], sbuf=cnw)
            V(lambda e: e.reduce_sum(out=rs.ap, in_=ssum.ap, axis=mybir.AxisListType.X), [ssum], [rs])
            A(lambda e: e.activation(out=rs.ap, in_=rs.ap, func=AF.Ln, scale=1.0 / D, bias=EPS), [rs], [rs])
            A(lambda e: e.activation(out=rs.ap, in_=rs.ap, func=AF.Exp, scale=-0.5), [rs], [rs])
            for qb in range(NT):
                V(lambda e: e.scalar_tensor_tensor(out=ytm.ap[:, qb, :], in0=ytm.ap[:, qb, :], scalar=rs.ap[:, qb:qb + 1], in1=cnw.ap,
                                                   op0=ALU.mult, op1=ALU.mult), [ytm, rs, cnw], [ytm])
            fw.barrier()
        out_proj_tiled(cdout_d[0:D, :])

        with ExitStack() as scD:
            sbd = scoped_sb(scD)
            qT2 = sbd([128, 2, T], BF16, "qT2")
            kT2 = sbd([128, 2, T], BF16, "kT2")
            v_tm = sbd([128, NT, 258], BF16, "v_tm")
            dnw = sbd([128, 256], F32, "dnw")
            V(lambda e: e.memset(v_tm.ap[:, :, 256:258], 1.0), [], [v_tm])
            for h in range(4):
                fw.dma("sync", dnw.ap, dnormw_d[:, h * 256:(h + 1) * 256].partition_broadcast(128), writes=[dnw], sbuf=dnw)
                for blk in range(2):
                    ch0 = h * 256 + blk * 128
                    proj_fm_conv(2576 + ch0, dconvw_d, dconvb_d, ch0, fT)
                    wq_ = loadw(dwq_d[ch0 // 128], 1)
                    wk_ = loadw(dwk_d[ch0 // 128], 1)
                    for tg in range(4):
                        pb = bank()
                        mm(pb.ap, wq_.ap[:, 0, :], fT.ap[:, tg * 512:(tg + 1) * 512], True, True, [wq_, fT], [pb])
                        evac(qT2.ap[:, blk, tg * 512:(tg + 1) * 512], pb.ap, [pb], [qT2])
                        pb = bank()
                        mm(pb.ap, wk_.ap[:, 0, :], fT.ap[:, tg * 512:(tg + 1) * 512], True, True, [wk_, fT], [pb])
                        A(lambda e: e.activation(out=kT2.ap[:, blk, tg * 512:(tg + 1) * 512], in_=pb.ap, func=AF.Copy, scale=0.0625),
                          [pb], [kT2])
                    proj_tm(3600 + ch0, v_tm, blk * 128)
                    proj_tm(4624 + ch0, ytm, h * 256 + blk * 128)
                A(lambda e: e.activation(out=ytm.ap[:, :, h * 256:(h + 1) * 256], in_=ytm.ap[:, :, h * 256:(h + 1) * 256],
                                         func=AF.Sigmoid), [ytm], [ytm])

                def gT_d(c, out_ap, bk):
                    for blk in range(2):
                        mm(out_ap, kT2.ap[:, blk, c * 128:(c + 1) * 128], qT2.ap[:, blk, c * 128:(c + 1) * 128],
                           blk == 0, blk == 1, [kT2, qT2], [bk])

                def fin_d(c, bA, bB, V, A, h=h):
                    t_, hd, t1 = tmp
                    if bB is not None:
                        V(lambda e: e.tensor_scalar(out=t_.ap[:, 0:257], in0=bB.ap[:, 0:257], scalar1=Wt.ap[:, c, 16 + h:17 + h], scalar2=None,
                                                    op0=ALU.mult), [bB, Wt], [t_])
                        V(lambda e: e.tensor_tensor(out=t_.ap[:, 0:257], in0=t_.ap[:, 0:257], in1=bA.ap[:, 0:257], op=ALU.add), [t_, bA], [t_])
                    else:
                        V(lambda e: e.tensor_copy(out=t_.ap[:, 0:257], in_=bA.ap[:, 0:257]), [bA], [t_])
                    V(lambda e: e.tensor_scalar(out=fsm.ap[:, 5:6], in0=t_.ap[:, 256:257], scalar1=-1.0, scalar2=None, op0=ALU.mult),
                      [t_], [fsm])
                    V(lambda e: e.tensor_tensor(out=fsm.ap[:, 0:1], in0=t_.ap[:, 256:257], in1=fsm.ap[:, 5:6], op=ALU.max), [t_, fsm], [fsm])
                    V(lambda e: e.tensor_scalar(out=fsm.ap[:, 0:1], in0=fsm.ap[:, 0:1], scalar1=1.0, scalar2=None, op0=ALU.max), [fsm], [fsm])
                    V(lambda e: e.reciprocal(out=fsm.ap[:, 1:2], in_=fsm.ap[:, 0:1]), [fsm], [fsm])
                    V(lambda e: e.tensor_scalar(out=hd.ap[:, 0:256], in0=t_.ap[:, 0:256], scalar1=fsm.ap[:, 1:2], scalar2=None, op0=ALU.mult),
                      [t_, fsm], [hd])
                    A(lambda e: e.activation(out=t1.ap[:, 0:256], in_=hd.ap[:, 0:256], func=AF.Square, accum_out=fsm.ap[:, 2:3]), [hd], [t1, fsm])
                    A(lambda e: e.activation(out=fsm.ap[:, 3:4], in_=fsm.ap[:, 2:3], func=AF.Ln, scale=1.0 / 256, bias=EPS), [fsm], [fsm])
                    A(lambda e: e.activation(out=fsm.ap[:, 4:5], in_=fsm.ap[:, 3:4], func=AF.Exp, scale=-0.5), [fsm], [fsm])
                    V(lambda e: e.scalar_tensor_tensor(out=t1.ap[:, 0:256], in0=hd.ap[:, 0:256], scalar=fsm.ap[:, 4:5], in1=dnw.ap,
                                                       op0=ALU.mult, op1=ALU.mult), [hd, fsm, dnw], [t1])
                    V(lambda e: e.tensor_tensor(out=ytm.ap[:, c, h * 256:(h + 1) * 256], in0=t1.ap[:, 0:256],
                                                in1=ytm.ap[:, c, h * 256:(h + 1) * 256], op=ALU.mult), [t1, ytm], [ytm])

                with ExitStack() as scu:
                    chunk_attn(scoped_sb(scu), 16 + h, 1, 257, False, gT_d, [kT2, qT2],
                               lambda c: v_tm.ap[:, c:c + 1, 0:257],
                               lambda c, hi: v_tm.ap[:, c, 0:257], v_tm,
                               lambda c, b: kT2.ap[:, b, c * 128:(c + 1) * 128], kT2, 2,
                               lambda c, b: qT2.ap[:, b, c * 128:(c + 1) * 128], qT2, fin_d)
                    fw.barrier()
            fw.barrier()
        evac_act_only[0] = False
        out_proj_tiled(cdout_d[D:2 * D, :])
        fw.barrier()
        scope.close()

    for s in range(nseq):
        load_x(s)
        if do_l0:
            layer0_mix()
            if do_ffn:
                ffn(0)
        if do_l1:
            layer1_mix()
            if do_ffn:
                ffn(1)
        final_store(s)
    fw.barrier()
    print("program: ops=%d waits=%d" % (fw.nops, fw.nwaits))
    return nc


def block_diag(w):
    w = np.asarray(w, np.float32)
    o = np.zeros((8, 128, 128), np.float32)
    for n in range(256):
        b, r = divmod(n, 32)
        o[b, 4 * r:4 * r + 4, 4 * r:4 * r + 4] = w[n]
    return o


NSEQ = 4
_prog_cache = {}


def make_in_maps(inputs, nseq, ncores):
    f = lambda a: np.ascontiguousarray(np.asarray(a, np.float32))
    cf, cb = make_consts()
    shared = {
        "consts": cf, "constsb": cb,
        "mix_norm_w": f(inputs["mix_norm_w"]), "ffn_norm_w": f(inputs["ffn_norm_w"]),
        "final_norm_w": f(inputs["final_norm_w"]).reshape(1, D),
        "ab_w_in": f(inputs["ab_w_in"][0]), "ab_w_out": f(inputs["ab_w_out"][0]),
        "diff_lq1": f(inputs["diff_lq1"]), "diff_lk1": f(inputs["diff_lk1"]),
        "diff_lq2": f(inputs["diff_lq2"]), "diff_lk2": f(inputs["diff_lk2"]),
        "diff_subln_w": f(inputs["diff_subln_w"]),
        "cd_w_in": f(inputs["cd_w_in"][0]),
        "c_conv_w": f(inputs["c_conv_w"][0]), "c_conv_b": f(inputs["c_conv_b"]),
        "c_dt_bias": f(inputs["c_dt_bias"]), "c_a_log": f(inputs["c_a_log"]), "c_d_skip": f(inputs["c_d_skip"]),
        "c_norm_w": f(inputs["c_norm_w"]),
        "d_conv_w": f(inputs["d_conv_w"][0]), "d_conv_b": f(inputs["d_conv_b"]),
        "d_wq_bd": block_diag(inputs["d_wq"][0]), "d_wk_bd": block_diag(inputs["d_wk"][0]),
        "d_i_bias": f(inputs["d_i_bias"]), "d_f_bias": f(inputs["d_f_bias"]),
        "d_norm_w": f(inputs["d_norm_w"]),
        "cd_w_out": f(inputs["cd_w_out"][0]),
        "ffn_w_gate_up": f(inputs["ffn_w_gate_up"]), "ffn_w_down": f(inputs["ffn_w_down"]),
    }
    x = f(inputs["x"])
    maps = []
    for c in range(ncores):
        m = dict(shared)
        m["x"] = np.ascontiguousarray(x[c * nseq:(c + 1) * nseq])
        maps.append(m)
    return maps


def kernel(**inputs):
    ncores = 8
    nseq = NSEQ
    nc = build_program(nseq)
    in_maps = make_in_maps(inputs, nseq, ncores)
    res = run_bass_kernel_spmd(nc, in_maps, core_ids=list(range(ncores)))
    return np.concatenate([np.asarray(r["out"], np.float32) for r in res.results], axis=0)
```

```python
import math
from contextlib import ExitStack
import numpy as np
import concourse.bass as bass
import concourse.mybir as mybir
from concourse.bass_utils import run_bass_kernel_spmd

F32 = mybir.dt.float32
BF16 = mybir.dt.bfloat16
AF = mybir.ActivationFunctionType
ALU = mybir.AluOpType

T = 2048
D = 1024
NT = 16
FFN_H = 2816
EPS = 1e-6
AB_COLS = 3072
CD_COLS = 5656
NEGM = -30000.0
DEFER_FINALIZE = True


class _Dep:
    def __init__(self):
        self.w = {}
        self.r = {}
        self.dsem = None


class Buf:
    def __init__(self, ap, name="", owner=None, excl=False):
        self.ap = ap
        self.name = name
        self.excl = excl
        self.d = owner.d if owner is not None else _Dep()

    w = property(lambda self: self.d.w, lambda self, v: setattr(self.d, "w", v))
    r = property(lambda self: self.d.r, lambda self, v: setattr(self.d, "r", v))
    dsem = property(lambda self: self.d.dsem, lambda self, v: setattr(self.d, "dsem", v))


class Fw:
    ENG = ["tensor", "vector", "scalar", "gpsimd", "sync"]

    def __init__(self, nc):
        self.nc = nc
        self.sem = {}
        self.cnt = {}
        self.waited = {}
        self.semobj = {}
        for e in self.ENG:
            s = nc.alloc_semaphore(name="s_" + e)
            self.sem[e] = s
            self.cnt[e] = 0
            self.waited[e] = {}
        self.dma_total = {}
        self.nwaits = 0
        self.nops = 0

    def eng(self, e):
        return getattr(self.nc, e)

    def _wait(self, e, deps):
        w = self.waited[e]
        for k, v in deps.items():
            if k[0] == 'e':
                if k[1] == e and e == "tensor":
                    continue
                sem = self.sem[k[1]]
            else:
                sem = self.semobj[k[1]]
                v = max(v, self.dma_total[k[1]])
            if w.get(k, 0) >= v:
                continue
            self.eng(e).wait_ge(sem, v)
            self.nwaits += 1
            w[k] = v

    @staticmethod
    def _merge(d, k, v):
        if d.get(k, 0) < v:
            d[k] = v

    def _deps(self, reads, writes):
        deps = {}
        for b in reads:
            for k, v in b.w.items():
                self._merge(deps, k, v)
        for b in writes:
            for k, v in b.w.items():
                self._merge(deps, k, v)
            for k, v in b.r.items():
                self._merge(deps, k, v)
        return deps

    def _record(self, key, val, reads, writes):
        for b in reads:
            self._merge(b.r, key, val)
        for b in writes:
            self._merge(b.w, key, val)
            b.r = {}

    def op(self, e, fn, reads=(), writes=()):
        if any(b.excl for b in reads):
            writes = list(writes) + [b for b in reads if b.excl]
            reads = [b for b in reads if not b.excl]
        self._wait(e, self._deps(reads, writes))
        ins = fn(self.eng(e))
        self.cnt[e] += 1
        ins.then_inc(self.sem[e], 1)
        self.nops += 1
        self._record(('e', e), self.cnt[e], reads, writes)
        return ins

    def dma(self, q, out_ap, in_ap, reads=(), writes=(), sbuf=None, **kw):
        self._wait(q, self._deps(reads, writes))
        if sbuf.dsem is None:
            sbuf.dsem = self.nc.alloc_semaphore(name="d_" + sbuf.name)
            self.semobj[id(sbuf.dsem)] = sbuf.dsem
            self.dma_total[id(sbuf.dsem)] = 0
        ins = self.eng(q).dma_start(out=out_ap, in_=in_ap, **kw)
        ins.then_inc(sbuf.dsem, 16)
        self.dma_total[id(sbuf.dsem)] += 16
        self._record(('d', id(sbuf.dsem)), self.dma_total[id(sbuf.dsem)], reads, writes)
        return ins

    def barrier(self):
        deps = {('e', e): self.cnt[e] for e in self.ENG if self.cnt[e] > 0}
        for k, v in self.dma_total.items():
            if v > 0:
                deps[('d', k)] = v
        for e in self.ENG:
            self._wait(e, dict(deps))


C_ID = 0
C_U = 128
C_NEG = 256
C_ONES = 384
C_TOT = 512
B_ID = 0
B_MA = 128
B_MB = B_MA + 2304
B_TOT = B_MB + 384


def make_consts():
    c = np.zeros((128, C_TOT), np.float32)
    cb = np.zeros((128, B_TOT), np.float32)
    s = np.arange(128)[:, None]
    c[:, C_ID:C_ID + 128] = np.eye(128, dtype=np.float32)
    cb[:, B_ID:B_ID + 128] = np.eye(128, dtype=np.float32)
    cc = np.arange(2304)[None, :]
    d = cc - 128 - s
    mult = ((d >= 0) & (d <= 128)).astype(np.float32)
    mult += ((d >= 0) & (d % 4 == 0) & (d <= 512)).astype(np.float32)
    mult += ((d >= 0) & (d % 16 == 0) & (d <= 2048)).astype(np.float32)
    cb[:, B_MA:B_MA + 2304] = mult
    cc = np.arange(384)[None, :]
    cb[:, B_MB:B_MB + 384] = ((cc - 128 - s) >= 0).astype(np.float32)
    l = np.arange(128)[None, :]
    c[:, C_U:C_U + 128] = (s <= l).astype(np.float32)
    c[:, C_NEG:C_NEG + 128] = np.where(s > l, NEGM, 0.0).astype(np.float32)
    c[:, C_ONES:C_ONES + 128] = 1.0
    return c, cb


def build_program(nseq, do_l0=True, do_l1=True, do_ffn=True, units=tuple(range(8)), ngroups=8, stage=9):
    nc = bass.Bass("TRN2", target_bir_lowering=False)
    fw = Fw(nc)

    def din(name, shape):
        return nc.dram_tensor(name, list(shape), F32, kind="ExternalInput").ap()

    x_d = din("x", [nseq, T, D])
    consts_d = din("consts", [128, C_TOT])
    constsb_d = din("constsb", [128, B_TOT])
    mixg_d = din("mix_norm_w", [2, D])
    ffng_d = din("ffn_norm_w", [2, D])
    fing_d = din("final_norm_w", [1, D])
    abin_d = din("ab_w_in", [D, AB_COLS])
    about_d = din("ab_w_out", [D, D])
    lq1_d = din("diff_lq1", [1, 64]); lk1_d = din("diff_lk1", [1, 64])
    lq2_d = din("diff_lq2", [1, 64]); lk2_d = din("diff_lk2", [1, 64])
    subln_d = din("diff_subln_w", [1, 128])
    cdin_d = din("cd_w_in", [D, CD_COLS])
    cconvw_d = din("c_conv_w", [4, 1536]); cconvb_d = din("c_conv_b", [1, 1536])
    cdtb_d = din("c_dt_bias", [1, 16]); calog_d = din("c_a_log", [1, 16]); cdskip_d = din("c_d_skip", [1, 16])
    cnormw_d = din("c_norm_w", [1, D])
    dconvw_d = din("d_conv_w", [4, D]); dconvb_d = din("d_conv_b", [1, D])
    dwq_d = din("d_wq_bd", [8, 128, 128]); dwk_d = din("d_wk_bd", [8, 128, 128])
    dib_d = din("d_i_bias", [1, 4]); dfb_d = din("d_f_bias", [1, 4])
    dnormw_d = din("d_norm_w", [1, D])
    cdout_d = din("cd_w_out", [2 * D, D])
    wgu_d = din("ffn_w_gate_up", [2, D, 2 * FFN_H])
    wdn_d = din("ffn_w_down", [2, FFN_H, D])
    out_d = nc.dram_tensor("out", [nseq, T, D], F32, kind="ExternalOutput").ap()

    cnt = [0]

    def sb(shape, dt=F32, name=None):
        cnt[0] += 1
        nm = (name or "t") + "_%d" % cnt[0]
        return Buf(nc.alloc_sbuf_tensor(nm, list(shape), dt).ap(), nm)

    def scoped_sb(scope):
        def f(shape, dt=F32, name=None):
            cnt[0] += 1
            nm = (name or "t") + "_%d" % cnt[0]
            return Buf(scope.enter_context(nc.sbuf_tensor(nm, list(shape), dt)).ap(), nm)
        return f

    hT = sb([128, 8, T], F32, "hT")
    cst = sb([128, C_TOT], F32, "cst")
    cstb = sb([128, B_TOT], BF16, "cstb")
    gam = sb([128, 5, 8], F32, "gam")
    PB = []
    for i in range(8):
        PB.append(Buf(nc.alloc_psum_tensor("pb%d" % i, [128, 512], F32).ap(), "pb%d" % i, excl=True))
    pb_rr = [0]

    def bank():
        b = PB[pb_rr[0] % 8]
        pb_rr[0] += 1
        return b

    fw.dma("sync", cst.ap, consts_d, writes=[cst], sbuf=cst)
    fw.dma("gpsimd", cstb.ap, constsb_d, writes=[cstb], sbuf=cstb)
    for i, (g_d, row) in enumerate([(mixg_d, 0), (ffng_d, 0), (mixg_d, 1), (ffng_d, 1), (fing_d, 0)]):
        fw.dma("sync", gam.ap[:, i, :], g_d[row, :].rearrange("(c p) -> p c", p=128), writes=[gam], sbuf=gam,
               allow_slow_non_contiguous=True)
    ident_f = cst.ap[:, C_ID:C_ID + 128]
    ident_b = cstb.ap[:, B_ID:B_ID + 128]
    ones_f = cst.ap[:, C_ONES:C_ONES + 128]

    NW = 4
    wbf = [sb([128, 8, 128], BF16, "wbf") for _ in range(NW)]
    w_rr = [0]

    def loadw(dram_rows_cols, nchunk, ncols=128):
        i = w_rr[0] % NW
        w_rr[0] += 1
        bf = wbf[i]
        fw.dma("gpsimd", bf.ap[:, 0:nchunk, 0:ncols], dram_rows_cols.rearrange("(c p) n -> p c n", p=128),
               writes=[bf], sbuf=bf)
        return bf

    evac_rr = [0]
    evac_act_only = [False]

    def evac(out_ap, in_ap, reads, writes):
        evac_rr[0] += 1
        if evac_act_only[0] or evac_rr[0] % 2 == 0:
            fw.op("scalar", lambda e: e.activation(out=out_ap, in_=in_ap, func=AF.Copy), reads=reads, writes=writes)
        else:
            fw.op("vector", lambda e: e.tensor_copy(out=out_ap, in_=in_ap), reads=reads, writes=writes)

    def mm(out_ap, lhsT, rhs, start, stop, reads, writes):
        fw.op("tensor", lambda e: e.matmul(out_ap, lhsT=lhsT, rhs=rhs, start=start, stop=stop, skip_group_check=True),
              reads=reads, writes=writes)

    sq = [sb([128, 512], F32, "sq") for _ in range(2)]
    rstd = sb([128, 512], F32, "rstd")

    def rms_stats(tg):
        pb = bank()
        for c in range(8):
            s = sq[c % 2]
            fw.op("scalar", lambda e: e.activation(out=s.ap, in_=hT.ap[:, c, tg * 512:(tg + 1) * 512], func=AF.Square),
                  reads=[hT], writes=[s])
            mm(pb.ap, ones_f, s.ap, c == 0, c == 7, [s, cst], [pb])
        fw.op("scalar", lambda e: e.activation(out=rstd.ap, in_=pb.ap, func=AF.Ln, scale=1.0 / D, bias=EPS),
              reads=[pb], writes=[rstd])
        fw.op("scalar", lambda e: e.activation(out=rstd.ap, in_=rstd.ap, func=AF.Exp, scale=-0.5),
              reads=[rstd], writes=[rstd])

    def rmsnorm_to(dst, gi, dst_dtype_is_f32=False):
        for tg in range(4):
            rms_stats(tg)
            for c in range(8):
                fw.op("vector", lambda e: e.scalar_tensor_tensor(
                    out=dst.ap[:, c, tg * 512:(tg + 1) * 512], in0=hT.ap[:, c, tg * 512:(tg + 1) * 512],
                    scalar=gam.ap[:, gi, c:c + 1], in1=rstd.ap, op0=ALU.mult, op1=ALU.mult),
                    reads=[hT, gam, rstd], writes=[dst])

    hnT = sb([128, 8, T], BF16, "hnT")
    ytm = sb([128, NT, D], BF16, "ytm")
    actT = Buf(ytm.ap.rearrange("p t d -> p (t d)").rearrange("p (h t) -> p h t", h=8), "actT", owner=ytm)

    def load_x(s):
        sc = ExitStack()
        sbx = scoped_sb(sc)
        xin = [sbx([128, D], F32, "xin") for _ in range(2)]
        for t in range(NT):
            xt = xin[t % 2]
            fw.dma("sync", xt.ap, x_d[s, t * 128:(t + 1) * 128, :], writes=[xt], sbuf=xt)
            for g in range(2):
                pb = bank()
                for cc in range(4):
                    c = g * 4 + cc
                    mm(pb.ap[:, cc * 128:(cc + 1) * 128], xt.ap[:, c * 128:(c + 1) * 128], ident_f, True, True,
                       [xt, cst], [pb])
                evac(hT.ap[:, g * 4:(g + 1) * 4, t * 128:(t + 1) * 128],
                     pb.ap.rearrange("p (c n) -> p c n", c=4), [pb], [hT])
        fw.barrier()
        sc.close()

    def transpose_ytm_to_hnT():
        for t in range(NT):
            for g in range(2):
                pb = bank()
                for cc in range(4):
                    c = g * 4 + cc
                    mm(pb.ap[:, cc * 128:(cc + 1) * 128], ytm.ap[:, t, c * 128:(c + 1) * 128], ident_b, True, True,
                       [ytm, cstb], [pb])
                evac(hnT.ap[:, g * 4:(g + 1) * 4, t * 128:(t + 1) * 128],
                     pb.ap.rearrange("p (c n) -> p c n", c=4), [pb], [hnT])

    def out_proj(w_dram):
        for cb in range(8):
            wb = loadw(w_dram[:, cb * 128:(cb + 1) * 128], 8)
            for tg in range(4):
                pb = bank()
                for c in range(8):
                    mm(pb.ap, wb.ap[:, c, :], hnT.ap[:, c, tg * 512:(tg + 1) * 512], c == 0, c == 7, [wb, hnT], [pb])
                fw.op("vector", lambda e: e.tensor_tensor(out=hT.ap[:, cb, tg * 512:(tg + 1) * 512],
                                                          in0=hT.ap[:, cb, tg * 512:(tg + 1) * 512], in1=pb.ap, op=ALU.add),
                      reads=[hT, pb], writes=[hT])

    def layer0_mix():
        scope = ExitStack()
        sb = scoped_sb(scope)
        qT = sb([128, T], BF16, "qT")
        kT = sb([128, 2, T], BF16, "kT")
        vA = sb([128, NT, 2, 65], BF16, "vA")
        vB = sb([128, NT, 129], BF16, "vB")
        PT = [sb([128, 2, 256], BF16, "PT") for _ in range(4)]
        lamw = sb([128, 8], F32, "lamw")
        lqk = sb([128, 4, 64], F32, "lqk")
        sublnw = sb([128, 128], F32, "sublnw")
        fin_s = [sb([128, 8], F32, "fin_s") for _ in range(2)]
        fin_t = [sb([128, 128], F32, "fin_t") for _ in range(2)]
        fin_o = [sb([128, 128], F32, "fin_o") for _ in range(2)]
        fin_j = sb([128, 128], F32, "fin_j")

        fw.op("vector", lambda e: e.memset(kT.ap, 0.0), writes=[kT])
        fw.op("vector", lambda e: e.memset(vA.ap, 1.0), writes=[vA])
        fw.op("vector", lambda e: e.memset(vB.ap, 1.0), writes=[vB])
        for i, d_ in enumerate([lq1_d, lk1_d, lq2_d, lk2_d]):
            fw.dma("sync", lqk.ap[:, i, :], d_.partition_broadcast(128), writes=[lqk], sbuf=lqk)
        fw.dma("sync", sublnw.ap, subln_d.partition_broadcast(128), writes=[sublnw], sbuf=sublnw)
        lambda_init = 0.8 - 0.6 * math.exp(-0.3 * 0)
        fw.op("vector", lambda e: e.tensor_scalar(out=sublnw.ap, in0=sublnw.ap, scalar1=1.0 - lambda_init, scalar2=None,
                                                  op0=ALU.mult), reads=[sublnw], writes=[sublnw])
        fw.op("vector", lambda e: e.tensor_tensor(out=lqk.ap[:, 0, :], in0=lqk.ap[:, 0, :], in1=lqk.ap[:, 1, :], op=ALU.mult),
              reads=[lqk], writes=[lqk])
        fw.op("vector", lambda e: e.tensor_tensor(out=lqk.ap[:, 2, :], in0=lqk.ap[:, 2, :], in1=lqk.ap[:, 3, :], op=ALU.mult),
              reads=[lqk], writes=[lqk])
        fw.op("vector", lambda e: e.reduce_sum(out=lamw.ap[:, 1:2], in_=lqk.ap[:, 0, :], axis=mybir.AxisListType.X),
              reads=[lqk], writes=[lamw])
        fw.op("vector", lambda e: e.reduce_sum(out=lamw.ap[:, 2:3], in_=lqk.ap[:, 2, :], axis=mybir.AxisListType.X),
              reads=[lqk], writes=[lamw])
        fw.op("scalar", lambda e: e.activation(out=lamw.ap[:, 3:5], in_=lamw.ap[:, 1:3], func=AF.Exp), reads=[lamw], writes=[lamw])
        fw.op("vector", lambda e: e.tensor_tensor(out=lamw.ap[:, 0:1], in0=lamw.ap[:, 4:5], in1=lamw.ap[:, 3:4], op=ALU.subtract),
              reads=[lamw], writes=[lamw])
        fw.op("vector", lambda e: e.tensor_scalar(out=lamw.ap[:, 0:1], in0=lamw.ap[:, 0:1], scalar1=-lambda_init, scalar2=None,
                                                  op0=ALU.add), reads=[lamw], writes=[lamw])

        def attn_unit(u):
            isA = u < 4
            qoff = (0 if isA else 1536) + (u % 4) * 128
            koff = qoff + 512
            voff = qoff + 1024
            wq = loadw(abin_d[:, qoff:qoff + 128], 8)
            wk = loadw(abin_d[:, koff:koff + 128], 8)
            wv = loadw(abin_d[:, voff:voff + 128], 8)
            for (wb, dst) in ((wq, qT), (wk, kT)):
                for tg in range(4):
                    pb = bank()
                    for c in range(8):
                        mm(pb.ap, wb.ap[:, c, :], hnT.ap[:, c, tg * 512:(tg + 1) * 512], c == 0, c == 7, [wb, hnT], [pb])
                    if dst is qT:
                        evac(dst.ap[:, tg * 512:(tg + 1) * 512], pb.ap, [pb], [dst])
                    else:
                        evac(dst.ap[0:64, 0, tg * 512:(tg + 1) * 512], pb.ap[0:64, :], [pb], [dst])
                        evac(dst.ap[64:128, 1, tg * 512:(tg + 1) * 512], pb.ap[64:128, :], [pb], [dst])
            for t4 in range(4):
                pb = bank()
                for tt in range(4):
                    t = t4 * 4 + tt
                    for c in range(8):
                        mm(pb.ap[:, tt * 128:(tt + 1) * 128], hnT.ap[:, c, t * 128:(t + 1) * 128], wv.ap[:, c, :],
                           c == 0, c == 7, [wv, hnT], [pb])
                if isA:
                    evac(vA.ap[:, t4 * 4:(t4 + 1) * 4, :, 0:64], pb.ap.rearrange("p (t i d) -> p t i d", t=4, i=2), [pb], [vA])
                else:
                    evac(vB.ap[:, t4 * 4:(t4 + 1) * 4, 0:128], pb.ap.rearrange("p (t d) -> p t d", t=4), [pb], [vB])
            W = 65 if isA else 129
            if stage < 1:
                return
            steps = [(G, j) for G in range(ngroups) for j in range(2 * G + 2)]
            nst = len(steps)
            vbuf = vA if isA else vB

            def acc_of(G, i, b):
                return PB[2 * (G % 2) + i], b * W

            def emit_S(k):
                G, j = steps[k]
                sbk = PB[4 + (k % 4)]
                for i in range(2):
                    mm(sbk.ap[:, i * 256:(i + 1) * 256], kT.ap[:, i, j * 128:(j + 1) * 128],
                       qT.ap[:, G * 256:(G + 1) * 256], True, True, [kT, qT], [sbk])

            def finalize(G):
                for b in range(2):
                    qb = 2 * G + b
                    fs = fin_s[b]
                    if isA:
                        for i in range(2):
                            a, o = acc_of(G, i, b)
                            fw.op("vector", lambda e: e.reciprocal(out=fs.ap[:, i:i + 1], in_=a.ap[:, o + 64:o + 65]), reads=[a], writes=[fs])
                            fw.op("vector", lambda e: e.tensor_scalar(
                                out=ytm.ap[:, qb, u * 128 + 64 * i:u * 128 + 64 * i + 64], in0=a.ap[:, o:o + 64],
                                scalar1=fs.ap[:, i:i + 1], scalar2=None, op0=ALU.mult), reads=[a, fs], writes=[ytm])
                    else:
                        a0, o0 = acc_of(G, 0, b)
                        a1, o1 = acc_of(G, 1, b)
                        ft, fo = fin_t[b], fin_o[b]
                        fw.op("vector", lambda e: e.reciprocal(out=fs.ap[:, 0:1], in_=a0.ap[:, o0 + 128:o0 + 129]), reads=[a0], writes=[fs])
                        fw.op("vector", lambda e: e.reciprocal(out=fs.ap[:, 1:2], in_=a1.ap[:, o1 + 128:o1 + 129]), reads=[a1], writes=[fs])
                        fw.op("vector", lambda e: e.tensor_tensor(out=fs.ap[:, 2:3], in0=fs.ap[:, 1:2], in1=lamw.ap[:, 0:1], op=ALU.mult),
                              reads=[fs, lamw], writes=[fs])
                        fw.op("vector", lambda e: e.tensor_scalar(out=ft.ap, in0=a0.ap[:, o0:o0 + 128], scalar1=fs.ap[:, 0:1], scalar2=None,
                                                                  op0=ALU.mult), reads=[a0, fs], writes=[ft])
                        fw.op("vector", lambda e: e.scalar_tensor_tensor(out=fo.ap, in0=a1.ap[:, o1:o1 + 128], scalar=fs.ap[:, 2:3],
                                                                         in1=ft.ap, op0=ALU.mult, op1=ALU.add),
                              reads=[a1, fs, ft], writes=[fo])
                        fw.op("scalar", lambda e: e.activation(out=fin_j.ap, in_=fo.ap, func=AF.Square, accum_out=fs.ap[:, 3:4]),
                              reads=[fo], writes=[fin_j, fs])
                        fw.op("scalar", lambda e: e.activation(out=fs.ap[:, 4:5], in_=fs.ap[:, 3:4], func=AF.Ln, scale=1.0 / 128, bias=EPS),
                              reads=[fs], writes=[fs])
                        fw.op("scalar", lambda e: e.activation(out=fs.ap[:, 5:6], in_=fs.ap[:, 4:5], func=AF.Exp, scale=-0.5),
                              reads=[fs], writes=[fs])
                        fw.op("vector", lambda e: e.scalar_tensor_tensor(
                            out=ytm.ap[:, qb, u * 128:(u + 1) * 128], in0=fo.ap, scalar=fs.ap[:, 5:6], in1=sublnw.ap,
                            op0=ALU.mult, op1=ALU.mult), reads=[fo, fs, sublnw], writes=[ytm])

            LA = 2
            for k0 in range(min(LA, nst)):
                emit_S(k0)
            pending = None
            for k, (G, j) in enumerate(steps):
                if j == 0:
                    for i in range(2):
                        a, _ = acc_of(G, i, 0)
                        fw.op("vector", lambda e: e.memset(a.ap[:, 0:2 * W], 0.0), writes=[a])
                if k + LA < nst:
                    emit_S(k + LA)
                sbk = PB[4 + (k % 4)]
                pt = PT[k % 4]
                fw.op("scalar", lambda e: e.activation(out=pt.ap, in_=sbk.ap.rearrange("p (i n) -> p i n", i=2),
                                                       func=AF.Exp, scale=0.125), reads=[sbk], writes=[pt])
                d0 = 2 * G - j
                if isA:
                    m = cstb.ap[:, B_MA + 128 * (d0 + 1):B_MA + 128 * (d0 + 1) + 256]
                elif d0 <= 0:
                    m = cstb.ap[:, B_MB + 128 * (d0 + 1):B_MB + 128 * (d0 + 1) + 256]
                else:
                    m = None
                if m is not None:
                    mb_ = m.unsqueeze(1).broadcast_to([128, 2, 256])
                    fw.op("vector", lambda e: e.tensor_tensor(out=pt.ap, in0=pt.ap, in1=mb_, op=ALU.mult),
                          reads=[pt, cstb], writes=[pt])
                for b in range(2):
                    if 2 * G + b < j:
                        continue
                    for i in range(2):
                        rhs = vA.ap[:, j, i, :] if isA else vB.ap[:, j, :]
                        a, o = acc_of(G, i, b)
                        mm(a.ap[:, o:o + W], pt.ap[:, i, b * 128:(b + 1) * 128], rhs, False, False, [pt, vbuf], [a])
                if pending is not None and k >= pending[1]:
                    finalize(pending[0])
                    pending = None
                if j == 2 * G + 1:
                    if pending is not None:
                        finalize(pending[0])
                    pending = (G, k + 2)
            if pending is not None:
                finalize(pending[0])

        rmsnorm_to(hnT, 0)
        for u in units:
            attn_unit(u)
        transpose_ytm_to_hnT()
        out_proj(about_d)
        fw.barrier()
        scope.close()

    silu_t = sq

    def ffn(layer):
        rmsnorm_to(hnT, 1 + 2 * layer)
        for (b0, nb) in ((0, 8), (8, 7), (15, 7)):
            for hb in range(nb):
                col = (b0 + hb) * 128
                wg = loadw(wgu_d[layer, :, col:col + 128], 8)
                wu = loadw(wgu_d[layer, :, FFN_H + col:FFN_H + col + 128], 8)
                for tg in range(4):
                    pg = bank()
                    pu = bank()
                    for c in range(8):
                        mm(pg.ap, wg.ap[:, c, :], hnT.ap[:, c, tg * 512:(tg + 1) * 512], c == 0, c == 7, [wg, hnT], [pg])
                    for c in range(8):
                        mm(pu.ap, wu.ap[:, c, :], hnT.ap[:, c, tg * 512:(tg + 1) * 512], c == 0, c == 7, [wu, hnT], [pu])
                    st = silu_t[tg % 2]
                    fw.op("scalar", lambda e: e.activation(out=st.ap, in_=pg.ap, func=AF.Silu), reads=[pg], writes=[st])
                    fw.op("vector", lambda e: e.tensor_tensor(out=actT.ap[:, hb, tg * 512:(tg + 1) * 512], in0=st.ap, in1=pu.ap,
                                                              op=ALU.mult), reads=[st, pu], writes=[actT])
            for cb in range(8):
                wd = loadw(wdn_d[layer, b0 * 128:(b0 + nb) * 128, cb * 128:(cb + 1) * 128], nb)
                for tg in range(4):
                    pb = bank()
                    for hb in range(nb):
                        mm(pb.ap, wd.ap[:, hb, :], actT.ap[:, hb, tg * 512:(tg + 1) * 512], hb == 0, hb == nb - 1, [wd, actT], [pb])
                    fw.op("vector", lambda e: e.tensor_tensor(out=hT.ap[:, cb, tg * 512:(tg + 1) * 512],
                                                              in0=hT.ap[:, cb, tg * 512:(tg + 1) * 512], in1=pb.ap, op=ALU.add),
                          reads=[hT, pb], writes=[hT])

    def final_store(s):
        sc = ExitStack()
        sbx = scoped_sb(sc)
        onT = [sbx([128, 8, 128], F32, "onT") for _ in range(2)]
        ost = [sbx([128, D], F32, "ost") for _ in range(2)]
        for tg in range(4):
            rms_stats(tg)
            for tt in range(4):
                t = tg * 4 + tt
                o_n = onT[t % 2]
                fw.op("vector", lambda e: e.scalar_tensor_tensor(
                    out=o_n.ap, in0=hT.ap[:, :, t * 128:(t + 1) * 128], scalar=1.0,
                    in1=rstd.ap[:, tt * 128:(tt + 1) * 128].unsqueeze(1).broadcast_to([128, 8, 128]),
                    op0=ALU.mult, op1=ALU.mult), reads=[hT, rstd], writes=[o_n])
                fw.op("vector", lambda e: e.tensor_tensor(
                    out=o_n.ap, in0=o_n.ap, in1=gam.ap[:, 4, :].unsqueeze(2).broadcast_to([128, 8, 128]), op=ALU.mult),
                    reads=[o_n, gam], writes=[o_n])
                os_ = ost[t % 2]
                for g in range(2):
                    pb = bank()
                    for cc in range(4):
                        c = g * 4 + cc
                        mm(pb.ap[:, cc * 128:(cc + 1) * 128], o_n.ap[:, c, :], ident_f, True, True, [o_n, cst], [pb])
                    evac(os_.ap[:, g * 512:(g + 1) * 512], pb.ap, [pb], [os_])
                fw.dma("sync", out_d[s, t * 128:(t + 1) * 128, :], os_.ap, reads=[os_], sbuf=os_)
        fw.barrier()
        sc.close()

    U_f = cst.ap[:, C_U:C_U + 128]
    NEG_f = cst.ap[:, C_NEG:C_NEG + 128]

    def out_proj_tiled(w_dram):
        with ExitStack() as sc:
            sbl = scoped_sb(sc)
            yT = [sbl([128, 8, 512], BF16, "yT") for _ in range(2)]
            for tg in range(4):
                y_ = yT[tg % 2]
                for tt in range(4):
                    t = tg * 4 + tt
                    for g in range(2):
                        pb = bank()
                        for cc in range(4):
                            c = g * 4 + cc
                            mm(pb.ap[:, cc * 128:(cc + 1) * 128], ytm.ap[:, t, c * 128:(c + 1) * 128], ident_b, True, True,
                               [ytm, cstb], [pb])
                        evac(y_.ap[:, g * 4:(g + 1) * 4, tt * 128:(tt + 1) * 128],
                             pb.ap.rearrange("p (c n) -> p c n", c=4), [pb], [y_])
                for cb in range(8):
                    wb = loadw(w_dram[:, cb * 128:(cb + 1) * 128], 8)
                    pb = bank()
                    for c in range(8):
                        mm(pb.ap, wb.ap[:, c, :], y_.ap[:, c, :], c == 0, c == 7, [wb, y_], [pb])
                    fw.op("vector", lambda e: e.tensor_tensor(out=hT.ap[:, cb, tg * 512:(tg + 1) * 512],
                                                              in0=hT.ap[:, cb, tg * 512:(tg + 1) * 512], in1=pb.ap, op=ALU.add),
                          reads=[hT, pb], writes=[hT])
            fw.barrier()

    def layer1_mix():
        scope = ExitStack()
        sbl = scoped_sb(scope)
        evac_act_only[0] = True
        rmsnorm_to(hnT, 2)
        V = lambda fn, r, w: fw.op("vector", fn, reads=r, writes=w)
        A = lambda fn, r, w: fw.op("scalar", fn, reads=r, writes=w)
        par = sbl([128, 64], F32, "par")
        acum = sbl([128, NT, 20], F32, "acum")
        bias = sbl([128, NT, 20], F32, "bias")
        U_ = sbl([128, NT, 20], F32, "U_")
        Wt = sbl([128, NT, 20], F32, "Wt")
        Dc = sbl([128, NT, 20], F32, "Dc")
        sctmp = ExitStack()
        sbt = scoped_sb(sctmp)
        sm = sbt([128, NT, 24], F32, "sm")
        for (o, n, d_) in ((0, 16, cdtb_d), (16, 16, calog_d), (32, 16, cdskip_d), (48, 4, dib_d), (52, 4, dfb_d)):
            fw.dma("sync", par.ap[:, o:o + n], d_.partition_broadcast(128), writes=[par], sbuf=par)
        wdt = loadw(cdin_d[:, 2560:2576], 8, 16)
        wif = loadw(cdin_d[:, 5648:5656], 8, 8)
        pb = bank()
        for t in range(NT):
            for c in range(8):
                mm(pb.ap[:, t * 24:t * 24 + 16], hnT.ap[:, c, t * 128:(t + 1) * 128], wdt.ap[:, c, 0:16], c == 0, c == 7,
                   [wdt, hnT], [pb])
            for c in range(8):
                mm(pb.ap[:, t * 24 + 16:t * 24 + 24], hnT.ap[:, c, t * 128:(t + 1) * 128], wif.ap[:, c, 0:8], c == 0, c == 7,
                   [wif, hnT], [pb])
        evac(sm.ap, pb.ap[:, 0:NT * 24].rearrange("p (t n) -> p t n", t=NT), [pb], [sm])

        def bc_t(ap2d, n):
            return ap2d.unsqueeze(1).broadcast_to([128, NT, n])
        A(lambda e: e.activation(out=par.ap[:, 16:32], in_=par.ap[:, 16:32], func=AF.Exp), [par], [par])
        V(lambda e: e.tensor_scalar(out=par.ap[:, 16:32], in0=par.ap[:, 16:32], scalar1=-1.0, scalar2=None, op0=ALU.mult), [par], [par])
        V(lambda e: e.tensor_tensor(out=sm.ap[:, :, 0:16], in0=sm.ap[:, :, 0:16], in1=bc_t(par.ap[:, 0:16], 16), op=ALU.add), [sm, par], [sm])
        dtt = sbt([128, NT, 16], F32, "dtt")
        lndt = sbt([128, NT, 16], F32, "lndt")
        A(lambda e: e.activation(out=dtt.ap, in_=sm.ap[:, :, 0:16], func=AF.Exp), [sm], [dtt])
        A(lambda e: e.activation(out=dtt.ap, in_=dtt.ap, func=AF.Ln, bias=1.0), [dtt], [dtt])
        A(lambda e: e.activation(out=lndt.ap, in_=dtt.ap, func=AF.Ln), [dtt], [lndt])
        g20 = sbt([128, NT, 20], F32, "g20")
        V(lambda e: e.tensor_tensor(out=g20.ap[:, :, 0:16], in0=dtt.ap, in1=bc_t(par.ap[:, 16:32], 16), op=ALU.mult), [dtt, par], [g20])
        V(lambda e: e.tensor_tensor(out=sm.ap[:, :, 20:24], in0=sm.ap[:, :, 20:24], in1=bc_t(par.ap[:, 52:56], 4), op=ALU.add), [sm, par], [sm])
        A(lambda e: e.activation(out=g20.ap[:, :, 16:20], in_=sm.ap[:, :, 20:24], func=AF.Exp, scale=-1.0), [sm], [g20])
        A(lambda e: e.activation(out=g20.ap[:, :, 16:20], in_=g20.ap[:, :, 16:20], func=AF.Ln, bias=1.0), [g20], [g20])
        V(lambda e: e.tensor_scalar(out=g20.ap[:, :, 16:20], in0=g20.ap[:, :, 16:20], scalar1=-1.0, scalar2=None, op0=ALU.mult), [g20], [g20])
        V(lambda e: e.tensor_tensor(out=sm.ap[:, :, 16:20], in0=sm.ap[:, :, 16:20], in1=bc_t(par.ap[:, 48:52], 4), op=ALU.add), [sm, par], [sm])
        tot = sbt([128, NT, 20], F32, "tot")
        pb = bank()
        mm(pb.ap[:, 0:320], U_f, g20.ap.rearrange("p t n -> p (t n)"), True, True, [cst, g20], [pb])
        evac(acum.ap, pb.ap[:, 0:320].rearrange("p (t n) -> p t n", t=NT), [pb], [acum])
        pb = bank()
        mm(pb.ap[:, 0:320], ones_f, g20.ap.rearrange("p t n -> p (t n)"), True, True, [cst, g20], [pb])
        evac(tot.ap, pb.ap[:, 0:320].rearrange("p (t n) -> p t n", t=NT), [pb], [tot])
        for t in range(1, NT):
            V(lambda e: e.tensor_tensor(out=tot.ap[:, t, :], in0=tot.ap[:, t, :], in1=tot.ap[:, t - 1, :], op=ALU.add), [tot], [tot])
        V(lambda e: e.tensor_tensor(out=acum.ap[:, 1:NT, :], in0=acum.ap[:, 1:NT, :], in1=tot.ap[:, 0:NT - 1, :], op=ALU.add), [acum, tot], [acum])
        V(lambda e: e.tensor_tensor(out=bias.ap[:, :, 0:16], in0=lndt.ap, in1=acum.ap[:, :, 0:16], op=ALU.subtract), [lndt, acum], [bias])
        V(lambda e: e.tensor_tensor(out=bias.ap[:, :, 16:20], in0=sm.ap[:, :, 16:20], in1=acum.ap[:, :, 16:20], op=ALU.subtract), [sm, acum], [bias])

        V(lambda e: e.tensor_tensor(out=U_.ap, in0=bias.ap, in1=tot.ap, op=ALU.add), [bias, tot], [U_])
        A(lambda e: e.activation(out=U_.ap, in_=U_.ap, func=AF.Exp), [U_], [U_])
        V(lambda e: e.memset(Wt.ap[:, 0:1, :], 0.0), [], [Wt])
        V(lambda e: e.memset(Dc.ap[:, 0:1, :], 0.0), [], [Dc])
        V(lambda e: e.tensor_tensor(out=Wt.ap[:, 1:NT, :], in0=acum.ap[:, 1:NT, :], in1=tot.ap[:, 0:NT - 1, :], op=ALU.subtract),
          [acum, tot], [Wt])
        A(lambda e: e.activation(out=Wt.ap[:, 1:NT, :], in_=Wt.ap[:, 1:NT, :], func=AF.Exp), [Wt], [Wt])
        V(lambda e: e.tensor_tensor(out=Dc.ap[:, 1:NT, :], in0=tot.ap[:, 1:NT, :], in1=tot.ap[:, 0:NT - 1, :], op=ALU.subtract),
          [tot], [Dc])
        A(lambda e: e.activation(out=Dc.ap[:, 1:NT, :], in_=Dc.ap[:, 1:NT, :], func=AF.Exp), [Dc], [Dc])
        fw.barrier()
        sctmp.close()
        xpadL = [sbl([128, 516], F32, "xpad") for _ in range(2)]
        cvL = [sbl([128, 512], F32, "cv") for _ in range(2)]
        cw = sbl([128, 8], F32, "cw")
        fT = sbl([128, T], BF16, "fT")
        tmp = [sbl([128, 260], F32, "tmp") for _ in range(3)]
        fsm = sbl([128, 8], F32, "fsm")
        ssum = sbl([128, NT, 4], F32, "ssum")

        def proj_fm_conv(col0, convw_d, convb_d, ch0, dst):
            wb = loadw(cdin_d[:, col0:col0 + 128], 8)
            fw.dma("sync", cw.ap[:, 0:4], convw_d[:, ch0:ch0 + 128].rearrange("k c -> c k"), writes=[cw], sbuf=cw,
                   allow_slow_non_contiguous=True)
            fw.dma("sync", cw.ap[:, 4:5], convb_d[:, ch0:ch0 + 128].rearrange("o c -> c o"), writes=[cw], sbuf=cw,
                   allow_slow_non_contiguous=True)
            V(lambda e: e.memset(xpadL[0].ap[:, 0:3], 0.0), [], [xpadL[0]])
            for pair in range(2):
                for i in range(2):
                    tg = pair * 2 + i
                    xpad = xpadL[i]
                    pb = bank()
                    for c in range(8):
                        mm(pb.ap, wb.ap[:, c, :], hnT.ap[:, c, tg * 512:(tg + 1) * 512], c == 0, c == 7, [wb, hnT], [pb])
                    A(lambda e: e.activation(out=xpad.ap[:, 3:515], in_=pb.ap, func=AF.Copy), [pb], [xpad])
                    if i == 0:
                        V(lambda e: e.tensor_copy(out=xpadL[1].ap[:, 0:3], in_=xpad.ap[:, 512:515]), [xpad], [xpadL[1]])
                for i in range(2):
                    V(lambda e: e.tensor_scalar(out=cvL[i].ap, in0=xpadL[i].ap[:, 0:512], scalar1=cw.ap[:, 0:1], scalar2=None, op0=ALU.mult),
                      [xpadL[i], cw], [cvL[i]])
                for k_ in range(1, 4):
                    for i in range(2):
                        V(lambda e: e.scalar_tensor_tensor(out=cvL[i].ap, in0=xpadL[i].ap[:, k_:k_ + 512], scalar=cw.ap[:, k_:k_ + 1],
                                                           in1=cvL[i].ap, op0=ALU.mult, op1=ALU.add), [xpadL[i], cw, cvL[i]], [cvL[i]])
                if pair == 0:
                    V(lambda e: e.tensor_copy(out=xpadL[0].ap[:, 0:3], in_=xpadL[1].ap[:, 512:515]), [xpadL[1]], [xpadL[0]])
                for i in range(2):
                    tg = pair * 2 + i
                    A(lambda e: e.activation(out=dst.ap[:, tg * 512:(tg + 1) * 512], in_=cvL[i].ap, func=AF.Silu, bias=cw.ap[:, 4:5]),
                      [cvL[i], cw], [dst])

        def proj_tm(col0, dst, dcol0):
            wb = loadw(cdin_d[:, col0:col0 + 128], 8)
            for t4 in range(4):
                pb = bank()
                for tt in range(4):
                    t = t4 * 4 + tt
                    for c in range(8):
                        mm(pb.ap[:, tt * 128:(tt + 1) * 128], hnT.ap[:, c, t * 128:(t + 1) * 128], wb.ap[:, c, :], c == 0, c == 7,
                           [wb, hnT], [pb])
                evac(dst.ap[:, t4 * 4:(t4 + 1) * 4, dcol0:dcol0 + 128], pb.ap.rearrange("p (t d) -> p t d", t=4), [pb], [dst])

        def fm_to_tm(src, dst, dcol0):
            for t4 in range(4):
                pb = bank()
                for tt in range(4):
                    t = t4 * 4 + tt
                    mm(pb.ap[:, tt * 128:(tt + 1) * 128], src.ap[:, t * 128:(t + 1) * 128], ident_b, True, True, [src, cstb], [pb])
                evac(dst.ap[:, t4 * 4:(t4 + 1) * 4, dcol0:dcol0 + 128], pb.ap.rearrange("p (t d) -> p t d", t=4), [pb], [dst])

        def chunk_attn(sbu, h0, nh, W, isC, gT_fn, g_bufs, v_view, v_hi, v_buf, kfm_fn, k_buf, nkb, q_fn, q_buf, finalize):
            nhW = nh * W
            RbL = [sbu([128, nh, 128], F32, "Rb") for _ in range(2)]
            ArL = [sbu([128, nh, 128], F32, "Ar") for _ in range(2)]
            El = [sbu([128, nh, 128], BF16, "E") for _ in range(2)]
            Ml = [sbu([128, nh, 128], BF16, "M") for _ in range(2)]
            xsuL = [sbu([128, nhW], BF16, "xsu") for _ in range(2)]
            ktmL = [sbu([128, nkb * 128], BF16, "ktm") for _ in range(2)]
            S32 = sbu([128, nkb, nhW], F32, "S32")
            SbfL = [sbu([128, nkb, nhW], BF16, "Sbf") for _ in range(2)]
            tS = sbu([128, nhW], F32, "tS")

            dq = []

            def P(fn, r, w):
                fw.op("gpsimd", fn, reads=r, writes=w)

            def Vd(fn, r, w):
                if DEFER_FINALIZE:
                    dq.append(("vector", fn, r, w))
                else:
                    fw.op("vector", fn, reads=r, writes=w)

            def Ad(fn, r, w):
                if DEFER_FINALIZE:
                    dq.append(("scalar", fn, r, w))
                else:
                    fw.op("scalar", fn, reads=r, writes=w)

            def pop_deferred(n=1):
                for _ in range(n):
                    if dq:
                        e_, fn_, r_, w_ = dq.pop(0)
                        fw.op(e_, fn_, reads=r_, writes=w_)

            def banks(c):
                p = c % 2
                return PB[4 * p], PB[4 * p + 1], PB[4 * p + 2], PB[4 * p + 3]

            def pr_of(c):
                bA, bB, bC, bD = banks(c)
                return (bD, 0) if isC else (bB, nhW)

            def st_of(c, b):
                bA, bB, bC, bD = banks(c)
                bk = bC if (isC or b == 0) else bD
                return bk, 0, (256 if isC else 257)

            def setup1(c):
                Rb_, Ar_ = RbL[c % 2], ArL[c % 2]
                pr, po = pr_of(c)
                P(lambda e: e.tensor_tensor(out=Rb_.ap, in0=ident_f.unsqueeze(1).broadcast_to([128, nh, 128]),
                                            in1=acum.ap[:, c, h0:h0 + nh].unsqueeze(2).broadcast_to([128, nh, 128]), op=ALU.mult),
                  [cst, acum], [Rb_])
                pop_deferred(2)
                mm(pr.ap[:, po:po + nh * 128], ones_f, Rb_.ap.rearrange("p h l -> p (h l)"), True, True, [cst, Rb_], [pr])
                A(lambda e: e.activation(out=Ar_.ap, in_=pr.ap[:, po:po + nh * 128].rearrange("p (h l) -> p h l", h=nh), func=AF.Copy),
                  [pr], [Ar_])

            def setup2(c):
                Rb_, Ar_ = RbL[c % 2], ArL[c % 2]
                P(lambda e: e.tensor_tensor(out=Rb_.ap, in0=Ar_.ap, in1=NEG_f.unsqueeze(1).broadcast_to([128, nh, 128]), op=ALU.add),
                  [Ar_, cst], [Rb_])

            def prep(c):
                xsu, ktm = xsuL[c % 2], ktmL[c % 2]
                P(lambda e: e.tensor_tensor(out=xsu.ap.rearrange("p (h w) -> p h w", h=nh), in0=v_view(c),
                                            in1=U_.ap[:, c, h0:h0 + nh].unsqueeze(2).broadcast_to([128, nh, W]), op=ALU.mult),
                  [v_buf, U_], [xsu])
                pop_deferred(2)
                for b in range(nkb):
                    bk, so, ko = st_of(c, b)
                    mm(bk.ap[:, ko:ko + 128], kfm_fn(c, b), ident_b, True, True, [k_buf, cstb], [bk])
                    evac(ktm.ap[:, b * 128:(b + 1) * 128], bk.ap[:, ko:ko + 128], [bk], [ktm])
                for b in range(nkb):
                    bk, so, ko = st_of(c, b)
                    mm(bk.ap[:, so:so + nhW], ktm.ap[:, b * 128:(b + 1) * 128], xsu.ap, True, True, [ktm, xsu], [bk])

            setup1(0)
            setup2(0)
            prep(0)
            for c in range(NT):
                bA, bB, bC, bD = banks(c)
                if c + 1 < NT:
                    setup1(c + 1)
                gT_fn(c, bA.ap[:, nhW:nhW + 128], bA)
                if c + 1 < NT:
                    prep(c + 1)
                E_, M_, Rb_ = El[c % 2], Ml[c % 2], RbL[c % 2]
                for hi in range(nh):
                    A(lambda e: e.activation(out=E_.ap[:, hi, :], in_=Rb_.ap[:, hi, :], func=AF.Exp,
                                             bias=bias.ap[:, c, h0 + hi:h0 + hi + 1]), [Rb_, bias], [E_])
                V(lambda e: e.tensor_tensor(out=M_.ap, in0=bA.ap[:, nhW:nhW + 128].unsqueeze(1).broadcast_to([128, nh, 128]),
                                            in1=E_.ap, op=ALU.mult), [bA, E_], [M_])
                pop_deferred(2)
                for hi in range(nh):
                    mm(bA.ap[:, hi * W:(hi + 1) * W], M_.ap[:, hi, :], v_hi(c, hi), False, False, [M_, v_buf], [bA])
                if c > 0:
                    Sb = SbfL[c % 2]
                    for b in range(nkb):
                        mm(bB.ap[:, 0:nhW], q_fn(c, b), Sb.ap[:, b, :], b == 0, b == nkb - 1, [q_buf, Sb], [bB])
                if c + 1 < NT:
                    Sn = SbfL[(c + 1) % 2]
                    for b in range(nkb):
                        bk, so, ko = st_of(c, b)
                        if c == 0:
                            V(lambda e: e.tensor_copy(out=S32.ap[:, b, :], in_=bk.ap[:, so:so + nhW]), [bk], [S32])
                            A(lambda e: e.activation(out=Sn.ap[:, b, :], in_=bk.ap[:, so:so + nhW], func=AF.Copy), [bk], [Sn])
                        else:
                            P(lambda e: e.tensor_tensor(out=tS.ap.rearrange("p (h w) -> p h w", h=nh),
                                                        in0=S32.ap[:, b, :].rearrange("p (h w) -> p h w", h=nh),
                                                        in1=Dc.ap[:, c, h0:h0 + nh].unsqueeze(2).broadcast_to([128, nh, W]), op=ALU.mult),
                              [S32, Dc], [tS])
                            V(lambda e: e.tensor_tensor(out=S32.ap[:, b, :], in0=tS.ap, in1=bk.ap[:, so:so + nhW], op=ALU.add),
                              [tS, bk], [S32])
                            A(lambda e: e.activation(out=Sn.ap[:, b, :], in_=S32.ap[:, b, :], func=AF.Copy), [S32], [Sn])
                    pop_deferred(2)
                    setup2(c + 1)
                pop_deferred(len(dq))
                finalize(c, bA, (bB if c > 0 else None), Vd, Ad)
            pop_deferred(len(dq))

        with ExitStack() as scC:
            sbc = scoped_sb(scC)
            bmT = sbc([128, T], BF16, "bmT")
            cmT = sbc([128, T], BF16, "cmT")
            xs_tm = sbc([128, NT, 256], BF16, "xs_tm")
            for cu in range(4):
                g = cu // 2
                if cu % 2 == 0:
                    proj_fm_conv(1024 + 1024 + g * 128, cconvw_d, cconvb_d, 1024 + g * 128, bmT)
                    proj_fm_conv(1024 + 1280 + g * 128, cconvw_d, cconvb_d, 1280 + g * 128, cmT)
                for blk in range(2):
                    ch0 = cu * 256 + blk * 128
                    proj_fm_conv(1024 + ch0, cconvw_d, cconvb_d, ch0, fT)
                    fm_to_tm(fT, xs_tm, blk * 128)
                    proj_tm(ch0, ytm, cu * 256 + blk * 128)
                A(lambda e: e.activation(out=ytm.ap[:, :, cu * 256:(cu + 1) * 256], in_=ytm.ap[:, :, cu * 256:(cu + 1) * 256],
                                         func=AF.Silu), [ytm], [ytm])

                def gT_c(c, out_ap, bk):
                    mm(out_ap, bmT.ap[:, c * 128:(c + 1) * 128], cmT.ap[:, c * 128:(c + 1) * 128], True, True, [bmT, cmT], [bk])

                def fin_c(c, bA, bB, V, A, cu=cu):
                    y_, t1, sz = tmp
                    h0 = cu * 4
                    V(lambda e: e.tensor_tensor(out=y_.ap[:, 0:256].rearrange("p (h w) -> p h w", h=4),
                                                in0=xs_tm.ap[:, c, :].rearrange("p (h w) -> p h w", h=4),
                                                in1=par.ap[:, 32 + h0:32 + h0 + 4].unsqueeze(2).broadcast_to([128, 4, 64]), op=ALU.mult),
                      [xs_tm, par], [y_])
                    V(lambda e: e.tensor_tensor(out=y_.ap[:, 0:256], in0=y_.ap[:, 0:256], in1=bA.ap[:, 0:256], op=ALU.add), [y_, bA], [y_])
                    if bB is not None:
                        V(lambda e: e.tensor_tensor(out=t1.ap[:, 0:256].rearrange("p (h w) -> p h w", h=4),
                                                    in0=bB.ap[:, 0:256].rearrange("p (h w) -> p h w", h=4),
                                                    in1=Wt.ap[:, c, h0:h0 + 4].unsqueeze(2).broadcast_to([128, 4, 64]), op=ALU.mult),
                          [bB, Wt], [t1])
                        V(lambda e: e.tensor_tensor(out=y_.ap[:, 0:256], in0=y_.ap[:, 0:256], in1=t1.ap[:, 0:256], op=ALU.add), [y_, t1], [y_])
                    V(lambda e: e.tensor_tensor(out=ytm.ap[:, c, cu * 256:(cu + 1) * 256], in0=y_.ap[:, 0:256],
                                                in1=ytm.ap[:, c, cu * 256:(cu + 1) * 256], op=ALU.mult), [y_, ytm], [ytm])
                    A(lambda e: e.activation(out=sz.ap[:, 0:256], in_=ytm.ap[:, c, cu * 256:(cu + 1) * 256], func=AF.Square,
                                             accum_out=ssum.ap[:, c, cu:cu + 1]), [ytm], [sz, ssum])

                with ExitStack() as scu:
                    chunk_attn(scoped_sb(scu), cu * 4, 4, 64, True, gT_c, [bmT, cmT],
                               lambda c: xs_tm.ap[:, c, :].rearrange("p (h w) -> p h w", h=4),
                               lambda c, hi: xs_tm.ap[:, c, hi * 64:(hi + 1) * 64], xs_tm,
                               lambda c, b: bmT.ap[:, c * 128:(c + 1) * 128], bmT, 1,
                               lambda c, b: cmT.ap[:, c * 128:(c + 1) * 128], cmT, fin_c)
                    fw.barrier()
            fw.barrier()
        with ExitStack() as scN:
            sbn = scoped_sb(scN)
            cnw = sbn([128, D], F32, "cnw")
            rs = sbn([128, NT], F32, "rs")
            fw.dma("sync", cnw.ap, cnormw_d.partition_broadcast(128), writes=[cnw], sbuf=cnw)
            V(lambda e: e.reduce_sum(out=rs.ap, in_=ssum.ap, axis=mybir.AxisListType.X), [ssum], [rs])
            A(lambda e: e.activation(out=rs.ap, in_=rs.ap, func=AF.Ln, scale=1.0 / D, bias=EPS), [rs], [rs])
            A(lambda e: e.activation(out=rs.ap, in_=rs.ap, func=AF.Exp, scale=-0.5), [rs], [rs])
            for qb in range(NT):
                V(lambda e: e.scalar_tensor_tensor(out=ytm.ap[:, qb, :], in0=ytm.ap[:, qb, :], scalar=rs.ap[:, qb:qb + 1], in1=cnw.ap,
                                                   op0=ALU.mult, op1=ALU.mult), [ytm, rs, cnw], [ytm])
            fw.barrier()
        out_proj_tiled(cdout_d[0:D, :])

        with ExitStack() as scD:
            sbd = scoped_sb(scD)
            qT2 = sbd([128, 2, T], BF16, "qT2")
            kT2 = sbd([128, 2, T], BF16, "kT2")
            v_tm = sbd([128, NT, 258], BF16, "v_tm")
            dnw = sbd([128, 256], F32, "dnw")
            V(lambda e: e.memset(v_tm.ap[:, :, 256:258], 1.0), [], [v_tm])
            for h in range(4):
                fw.dma("sync", dnw.ap, dnormw_d[:, h * 256:(h + 1) * 256].partition_broadcast(128), writes=[dnw], sbuf=dnw)
                for blk in range(2):
                    ch0 = h * 256 + blk * 128
                    proj_fm_conv(2576 + ch0, dconvw_d, dconvb_d, ch0, fT)
                    wq_ = loadw(dwq_d[ch0 // 128], 1)
                    wk_ = loadw(dwk_d[ch0 // 128], 1)
                    for tg in range(4):
                        pb = bank()
                        mm(pb.ap, wq_.ap[:, 0, :], fT.ap[:, tg * 512:(tg + 1) * 512], True, True, [wq_, fT], [pb])
                        evac(qT2.ap[:, blk, tg * 512:(tg + 1) * 512], pb.ap, [pb], [qT2])
                        pb = bank()
                        mm(pb.ap, wk_.ap[:, 0, :], fT.ap[:, tg * 512:(tg + 1) * 512], True, True, [wk_, fT], [pb])
                        A(lambda e: e.activation(out=kT2.ap[:, blk, tg * 512:(tg + 1) * 512], in_=pb.ap, func=AF.Copy, scale=0.0625),
                          [pb], [kT2])
                    proj_tm(3600 + ch0, v_tm, blk * 128)
                    proj_tm(4624 + ch0, ytm, h * 256 + blk * 128)
                A(lambda e: e.activation(out=ytm.ap[:, :, h * 256:(h + 1) * 256], in_=ytm.ap[:, :, h * 256:(h + 1) * 256],
                                         func=AF.Sigmoid), [ytm], [ytm])

                def gT_d(c, out_ap, bk):
                    for blk in range(2):
                        mm(out_ap, kT2.ap[:, blk, c * 128:(c + 1) * 128], qT2.ap[:, blk, c * 128:(c + 1) * 128],
                           blk == 0, blk == 1, [kT2, qT2], [bk])

                def fin_d(c, bA, bB, V, A, h=h):
                    t_, hd, t1 = tmp
                    if bB is not None:
                        V(lambda e: e.tensor_scalar(out=t_.ap[:, 0:257], in0=bB.ap[:, 0:257], scalar1=Wt.ap[:, c, 16 + h:17 + h], scalar2=None,
                                                    op0=ALU.mult), [bB, Wt], [t_])
                        V(lambda e: e.tensor_tensor(out=t_.ap[:, 0:257], in0=t_.ap[:, 0:257], in1=bA.ap[:, 0:257], op=ALU.add), [t_, bA], [t_])
                    else:
                        V(lambda e: e.tensor_copy(out=t_.ap[:, 0:257], in_=bA.ap[:, 0:257]), [bA], [t_])
                    V(lambda e: e.tensor_scalar(out=fsm.ap[:, 5:6], in0=t_.ap[:, 256:257], scalar1=-1.0, scalar2=None, op0=ALU.mult),
                      [t_], [fsm])
                    V(lambda e: e.tensor_tensor(out=fsm.ap[:, 0:1], in0=t_.ap[:, 256:257], in1=fsm.ap[:, 5:6], op=ALU.max), [t_, fsm], [fsm])
                    V(lambda e: e.tensor_scalar(out=fsm.ap[:, 0:1], in0=fsm.ap[:, 0:1], scalar1=1.0, scalar2=None, op0=ALU.max), [fsm], [fsm])
                    V(lambda e: e.reciprocal(out=fsm.ap[:, 1:2], in_=fsm.ap[:, 0:1]), [fsm], [fsm])
                    V(lambda e: e.tensor_scalar(out=hd.ap[:, 0:256], in0=t_.ap[:, 0:256], scalar1=fsm.ap[:, 1:2], scalar2=None, op0=ALU.mult),
                      [t_, fsm], [hd])
                    A(lambda e: e.activation(out=t1.ap[:, 0:256], in_=hd.ap[:, 0:256], func=AF.Square, accum_out=fsm.ap[:, 2:3]), [hd], [t1, fsm])
                    A(lambda e: e.activation(out=fsm.ap[:, 3:4], in_=fsm.ap[:, 2:3], func=AF.Ln, scale=1.0 / 256, bias=EPS), [fsm], [fsm])
                    A(lambda e: e.activation(out=fsm.ap[:, 4:5], in_=fsm.ap[:, 3:4], func=AF.Exp, scale=-0.5), [fsm], [fsm])
                    V(lambda e: e.scalar_tensor_tensor(out=t1.ap[:, 0:256], in0=hd.ap[:, 0:256], scalar=fsm.ap[:, 4:5], in1=dnw.ap,
                                                       op0=ALU.mult, op1=ALU.mult), [hd, fsm, dnw], [t1])
                    V(lambda e: e.tensor_tensor(out=ytm.ap[:, c, h * 256:(h + 1) * 256], in0=t1.ap[:, 0:256],
                                                in1=ytm.ap[:, c, h * 256:(h + 1) * 256], op=ALU.mult), [t1, ytm], [ytm])

                with ExitStack() as scu:
                    chunk_attn(scoped_sb(scu), 16 + h, 1, 257, False, gT_d, [kT2, qT2],
                               lambda c: v_tm.ap[:, c:c + 1, 0:257],
                               lambda c, hi: v_tm.ap[:, c, 0:257], v_tm,
                               lambda c, b: kT2.ap[:, b, c * 128:(c + 1) * 128], kT2, 2,
                               lambda c, b: qT2.ap[:, b, c * 128:(c + 1) * 128], qT2, fin_d)
                    fw.barrier()
            fw.barrier()
        evac_act_only[0] = False
        out_proj_tiled(cdout_d[D:2 * D, :])
        fw.barrier()
        scope.close()

    for s in range(nseq):
        load_x(s)
        if do_l0:
            layer0_mix()
            if do_ffn:
                ffn(0)
        if do_l1:
            layer1_mix()
            if do_ffn:
                ffn(1)
        final_store(s)
    fw.barrier()
    print("program: ops=%d waits=%d" % (fw.nops, fw.nwaits))
    return nc


def block_diag(w):
    w = np.asarray(w, np.float32)
    o = np.zeros((8, 128, 128), np.float32)
    for n in range(256):
        b, r = divmod(n, 32)
        o[b, 4 * r:4 * r + 4, 4 * r:4 * r + 4] = w[n]
    return o


NSEQ = 4
_prog_cache = {}


def make_in_maps(inputs, nseq, ncores):
    f = lambda a: np.ascontiguousarray(np.asarray(a, np.float32))
    cf, cb = make_consts()
    shared = {
        "consts": cf, "constsb": cb,
        "mix_norm_w": f(inputs["mix_norm_w"]), "ffn_norm_w": f(inputs["ffn_norm_w"]),
        "final_norm_w": f(inputs["final_norm_w"]).reshape(1, D),
        "ab_w_in": f(inputs["ab_w_in"][0]), "ab_w_out": f(inputs["ab_w_out"][0]),
        "diff_lq1": f(inputs["diff_lq1"]), "diff_lk1": f(inputs["diff_lk1"]),
        "diff_lq2": f(inputs["diff_lq2"]), "diff_lk2": f(inputs["diff_lk2"]),
        "diff_subln_w": f(inputs["diff_subln_w"]),
        "cd_w_in": f(inputs["cd_w_in"][0]),
        "c_conv_w": f(inputs["c_conv_w"][0]), "c_conv_b": f(inputs["c_conv_b"]),
        "c_dt_bias": f(inputs["c_dt_bias"]), "c_a_log": f(inputs["c_a_log"]), "c_d_skip": f(inputs["c_d_skip"]),
        "c_norm_w": f(inputs["c_norm_w"]),
        "d_conv_w": f(inputs["d_conv_w"][0]), "d_conv_b": f(inputs["d_conv_b"]),
        "d_wq_bd": block_diag(inputs["d_wq"][0]), "d_wk_bd": block_diag(inputs["d_wk"][0]),
        "d_i_bias": f(inputs["d_i_bias"]), "d_f_bias": f(inputs["d_f_bias"]),
        "d_norm_w": f(inputs["d_norm_w"]),
        "cd_w_out": f(inputs["cd_w_out"][0]),
        "ffn_w_gate_up": f(inputs["ffn_w_gate_up"]), "ffn_w_down": f(inputs["ffn_w_down"]),
    }
    x = f(inputs["x"])
    maps = []
    for c in range(ncores):
        m = dict(shared)
        m["x"] = np.ascontiguousarray(x[c * nseq:(c + 1) * nseq])
        maps.append(m)
    return maps


def kernel(**inputs):
    ncores = 8
    nseq = NSEQ
    nc = build_program(nseq)
    in_maps = make_in_maps(inputs, nseq, ncores)
    res = run_bass_kernel_spmd(nc, in_maps, core_ids=list(range(ncores)))
    return np.concatenate([np.asarray(r["out"], np.float32) for r in res.results], axis=0)
```
